# Optimizing a Trainium2 kernel written in Bass

```python
import jax, jax.numpy as jnp
from jax import lax
import numpy as np

D_MODEL = 1024
BATCH = 32
SEQ = 2048
DEPTH = 1

D_MIX = D_MODEL
D_CONV = D_MIX // 2
D_ATTN = D_MIX - D_CONV
HEAD_DIM = 64
N_HEADS = D_ATTN // HEAD_DIM
CONV_WIDTH = 31
PATTERNS = ((128, 1), (512, 4), (2048, 16))
BLOCK_Q = 128
N_GROUPS = 4
EXPERTS_PER_GROUP = 8
N_EXPERTS = N_GROUPS * EXPERTS_PER_GROUP
TOP_K_EXPERT = 2
D_EXPERT = 256
EXPERT_BLOCK = 128
EPS = 1e-6
NEG_INF = -1e30

kernel_name = "hymba_conformer_dilated_hmoe_block"


def rms_norm(x, g):
    xf = x.astype(jnp.float32)
    r = lax.rsqrt(jnp.mean(xf * xf, axis=-1, keepdims=True) + EPS)
    return (xf * r * g.astype(jnp.float32)).astype(x.dtype)


def layer_norm(x, g, b):
    xf = x.astype(jnp.float32)
    mu = jnp.mean(xf, axis=-1, keepdims=True)
    var = jnp.mean(jnp.square(xf - mu), axis=-1, keepdims=True)
    y = (xf - mu) * lax.rsqrt(var + EPS) * g.astype(jnp.float32) + b.astype(jnp.float32)
    return y.astype(x.dtype)


def conformer_conv_group(a, gate, dw_w, dw_b, ln_g, ln_b):
    u = a * jax.nn.sigmoid(gate)
    c = u.shape[-1]
    y = lax.conv_general_dilated(
        u, dw_w.astype(u.dtype)[:, None, :], window_strides=(1,),
        padding=[(CONV_WIDTH - 1, 0)], dimension_numbers=("NWC", "WIO", "NWC"),
        feature_group_count=c)
    y = y + dw_b.astype(y.dtype)
    y = layer_norm(y, ln_g, ln_b)
    return jax.nn.silu(y)


def dilated_branch(q, k, v, window, dilation):
    b_, h_, s_, hd = q.shape
    d = dilation
    length = s_ // d
    w_sub = window // d
    nb = -(-length // BLOCK_Q)
    lp = nb * BLOCK_Q

    def to_strided(t):
        t = t.reshape(b_, h_, length, d, hd).transpose(0, 1, 3, 2, 4)
        t = jnp.pad(t, ((0, 0), (0, 0), (0, 0), (0, lp - length), (0, 0)))
        return t.reshape(b_, h_, d, nb, BLOCK_Q, hd)

    def with_prev(t):
        prev = jnp.pad(t[:, :, :, :-1], ((0, 0), (0, 0), (0, 0), (1, 0), (0, 0), (0, 0)))
        return jnp.concatenate([prev, t], axis=-2)

    qs = to_strided(q).astype(jnp.float32)
    kb = with_prev(to_strided(k)).astype(jnp.float32)
    vb = with_prev(to_strided(v)).astype(jnp.float32)

    s = jnp.einsum("bhrnqc,bhrnkc->bhrnqk", qs, kb) * (hd ** -0.5)
    qi = jnp.arange(BLOCK_Q)[:, None]
    ki = jnp.arange(2 * BLOCK_Q)[None, :]
    dist = BLOCK_Q + qi - ki
    band = (dist >= 0) & (dist <= w_sub)
    first = (jnp.arange(nb) == 0)[:, None, None]
    valid = band[None] & ~(first & (ki < BLOCK_Q)[None])
    s = jnp.where(valid[None, None, None], s, NEG_INF)
    m = jnp.max(s, axis=-1, keepdims=True)
    p = jnp.exp(s - m)
    den = jnp.sum(p, axis=-1, keepdims=True)
    o = jnp.einsum("bhrnqk,bhrnkc->bhrnqc", p, vb) / den
    lse = (m + jnp.log(den))[..., 0]

    o = o.reshape(b_, h_, d, lp, hd)[:, :, :, :length]
    o = o.transpose(0, 1, 3, 2, 4).reshape(b_, h_, s_, hd)
    lse = lse.reshape(b_, h_, d, lp)[..., :length].transpose(0, 1, 3, 2).reshape(b_, h_, s_)
    return o, lse


def dilated_attention_group(q, k, v):
    b_, s_, _ = q.shape
    split = lambda t: t.reshape(b_, s_, N_HEADS, HEAD_DIM).transpose(0, 2, 1, 3)
    qh, kh, vh = split(q), split(k), split(v)
    outs, lses = [], []
    for window, dilation in PATTERNS:
        o, l = dilated_branch(qh, kh, vh, window, dilation)
        outs.append(o)
        lses.append(l)
    wts = jax.nn.softmax(jnp.stack(lses, axis=0), axis=0)
    o = jnp.sum(wts[..., None] * jnp.stack(outs, axis=0), axis=0)
    return o.transpose(0, 2, 1, 3).reshape(b_, s_, D_ATTN).astype(q.dtype)


def hierarchical_moe(xn, router_group, router_expert, w_gate, w_up, w_down):
    t_ = xn.shape[0]
    g_prob = jax.nn.softmax((xn @ router_group).astype(jnp.float32), axis=-1)
    p_top, g_top = lax.top_k(g_prob, 1)
    e_logits = jnp.einsum("td,dge->tge", xn, router_expert).astype(jnp.float32)
    e_sel = jnp.take_along_axis(e_logits, g_top[:, :, None], axis=1)[:, 0]
    e_top, e_idx = lax.top_k(e_sel, TOP_K_EXPERT)
    weights = jax.nn.softmax(e_top, axis=-1) * p_top
    expert = (g_top * EXPERTS_PER_GROUP + e_idx).astype(jnp.int32)

    n_assign = t_ * TOP_K_EXPERT
    e_flat = expert.reshape(-1)
    w_flat = weights.reshape(-1)
    tok_flat = jnp.repeat(jnp.arange(t_, dtype=jnp.int32), TOP_K_EXPERT)
    order = jnp.argsort(e_flat)
    se, stok, sw = e_flat[order], tok_flat[order], w_flat[order]
    counts = jnp.zeros((N_EXPERTS,), jnp.int32).at[e_flat].add(1)
    starts = jnp.cumsum(counts) - counts
    pcounts = (counts + EXPERT_BLOCK - 1) // EXPERT_BLOCK * EXPERT_BLOCK
    pends = jnp.cumsum(pcounts)
    pstarts = pends - pcounts
    dest = pstarts[se] + (jnp.arange(n_assign, dtype=jnp.int32) - starts[se])
    n_blk = -(-n_assign // EXPERT_BLOCK) + N_EXPERTS
    n_rows = n_blk * EXPERT_BLOCK
    buf_tok = jnp.zeros((n_rows,), jnp.int32).at[dest].set(stok)
    buf_w = jnp.zeros((n_rows,), xn.dtype).at[dest].set(sw.astype(xn.dtype))
    blk_start = jnp.arange(n_blk, dtype=jnp.int32) * EXPERT_BLOCK
    blk_expert = jnp.minimum(jnp.searchsorted(pends, blk_start, side="right"),
                             N_EXPERTS - 1).astype(jnp.int32)

    def run_block(args):
        tok_b, w_b, e = args
        xb = xn[tok_b]
        hdn = jax.nn.silu(xb @ w_gate[e]) * (xb @ w_up[e])
        return (hdn @ w_down[e]) * w_b[:, None]

    out = lax.map(run_block, (buf_tok.reshape(n_blk, EXPERT_BLOCK),
                              buf_w.reshape(n_blk, EXPERT_BLOCK), blk_expert))
    return jnp.zeros_like(xn).at[buf_tok].add(out.reshape(n_rows, xn.shape[-1]))


def setup_inputs(seed: int = 0) -> dict:
    key = jax.random.key(seed)
    ks = jax.random.split(key, 16)
    f32 = jnp.float32
    nrm = lambda k, shape, scale: jax.random.normal(k, shape, f32) * scale
    d_in = 2 * D_CONV + 3 * D_ATTN
    return {
        "x": jax.random.normal(ks[0], (BATCH, SEQ, D_MODEL), f32),
        "norm_mix_g": 1.0 + nrm(ks[1], (DEPTH, D_MODEL), 0.02),
        "w_in": nrm(ks[2], (DEPTH, D_MODEL, d_in), D_MODEL ** -0.5),
        "conv_dw_w": nrm(ks[3], (DEPTH, CONV_WIDTH, D_CONV), CONV_WIDTH ** -0.5),
        "conv_dw_b": nrm(ks[4], (DEPTH, D_CONV), 0.02),
        "conv_ln_g": 1.0 + nrm(ks[5], (DEPTH, D_CONV), 0.02),
        "conv_ln_b": nrm(ks[6], (DEPTH, D_CONV), 0.02),
        "w_out": nrm(ks[7], (DEPTH, D_MIX, D_MODEL), D_MIX ** -0.5),
        "norm_ffn_g": 1.0 + nrm(ks[8], (DEPTH, D_MODEL), 0.02),
        "router_group": nrm(ks[9], (DEPTH, D_MODEL, N_GROUPS), D_MODEL ** -0.5),
        "router_expert": nrm(ks[10], (DEPTH, D_MODEL, N_GROUPS, EXPERTS_PER_GROUP), D_MODEL ** -0.5),
        "expert_w_gate": nrm(ks[11], (DEPTH, N_EXPERTS, D_MODEL, D_EXPERT), D_MODEL ** -0.5),
        "expert_w_up": nrm(ks[12], (DEPTH, N_EXPERTS, D_MODEL, D_EXPERT), D_MODEL ** -0.5),
        "expert_w_down": nrm(ks[13], (DEPTH, N_EXPERTS, D_EXPERT, D_MODEL), D_EXPERT ** -0.5),
        "norm_final_g": 1.0 + nrm(ks[14], (D_MODEL,), 0.02),
    }


def reference(x, norm_mix_g, w_in, conv_dw_w, conv_dw_b, conv_ln_g, conv_ln_b, w_out,
              norm_ffn_g, router_group, router_expert, expert_w_gate, expert_w_up,
              expert_w_down, norm_final_g):
    b_, s_, d_ = x.shape
    splits = [D_CONV, 2 * D_CONV, 2 * D_CONV + D_ATTN, 2 * D_CONV + 2 * D_ATTN]
    for layer in range(DEPTH):
        h = rms_norm(x, norm_mix_g[layer])
        proj = h @ w_in[layer]
        a, gate, q, k, v = jnp.split(proj, splits, axis=-1)
        conv_out = conformer_conv_group(a, gate, conv_dw_w[layer], conv_dw_b[layer],
                                        conv_ln_g[layer], conv_ln_b[layer])
        attn_out = dilated_attention_group(q, k, v)
        mixed = jnp.concatenate([conv_out, attn_out], axis=-1) @ w_out[layer]
        x = x + mixed
        hn = rms_norm(x, norm_ffn_g[layer]).reshape(b_ * s_, d_)
        ffn = hierarchical_moe(hn, router_group[layer], router_expert[layer],
                               expert_w_gate[layer], expert_w_up[layer], expert_w_down[layer])
        x = x + ffn.reshape(b_, s_, d_)
    return rms_norm(x, norm_final_g)
```

```python
import numpy as np
from contextlib import ExitStack
import concourse.bass as bass
import concourse.mybir as mybir
from concourse.bass_utils import run_bass_kernel_spmd

F32 = mybir.dt.float32
BF16 = mybir.dt.bfloat16
AF = mybir.ActivationFunctionType
ALU = mybir.AluOpType

S = 2048
D = 1024
NCORE = 8
SEQ_PER_CORE = 4
import os as _os
PATTERN_D = tuple(int(v) for v in _os.environ.get("KPD", "1,4,16").split(","))
BIG = 30000.0


class Eng:
    def __init__(self, name, h, sem, selfsync=True):
        self.name = name; self.h = h; self.sem = sem; self.count = 0
        self.seen = {}; self.selfsync = selfsync


class Buf:
    def __init__(self, name, mk=None, psum=False):
        self.name = name; self.w = None; self.r = {}
        self.mk = mk; self._d = None; self._p = None
        self.dcount = 0; self.psum = psum; self.pcount = 0

    @property
    def dsem(self):
        if self._d is None:
            self._d = self.mk("d_" + self.name)
        return self._d

    @property
    def psem(self):
        if self._p is None:
            self._p = self.mk("p_" + self.name)
        return self._p


class Trk:
    def wait(self, eng, deps):
        for d in deps:
            if d is None:
                continue
            key, sem, val, src = d
            if src is eng and not eng.selfsync:
                continue
            if eng.seen.get(key, 0) >= val:
                continue
            eng.h.wait_ge(sem, val)
            eng.seen[key] = val

    def deps(self, reads, writes):
        deps = []
        for b in reads:
            deps.append(b.w)
            if b.psum:
                deps.extend(b.r.values())
        for b in writes:
            deps.append(b.w)
            deps.extend(b.r.values())
        return deps

    def mark(self, d, reads, writes):
        for b in reads:
            o = b.r.get(d[0])
            if o is None or o[2] < d[2]:
                b.r[d[0]] = d
        for b in writes:
            b.w = d; b.r = {}

    skip = False

    def op(self, eng, fn, reads=(), writes=()):
        if self.skip:
            return
        self.wait(eng, self.deps(reads, writes))
        inst = fn()
        eng.count += 1
        inst.then_inc(eng.sem, 1)
        self.mark((eng.name, eng.sem, eng.count, eng), reads, writes)

    def dma(self, eng, sb, out, in_, reads=(), writes=(), **kw):
        if self.skip:
            return
        self.wait(eng, self.deps(reads, writes))
        inst = eng.h.dma_start(out=out, in_=in_, **kw)
        if eng.name == "pool":
            sb.pcount += 16
            inst.then_inc(sb.psem, 16)
            self.mark(("p" + sb.name, sb.psem, sb.pcount, None), reads, writes)
        else:
            sb.dcount += 16
            inst.then_inc(sb.dsem, 16)
            self.mark(("d" + sb.name, sb.dsem, sb.dcount, None), reads, writes)


def idma(T, eng, sb, out, out_off, in_, in_off, reads=(), writes=(), **kw):
    if T.skip:
        return
    T.wait(eng, T.deps(reads, writes))
    inst = eng.h.indirect_dma_start(out=out, out_offset=out_off, in_=in_, in_offset=in_off, **kw)
    sb.pcount += 16
    inst.then_inc(sb.psem, 16)
    T.mark(("p" + sb.name, sb.psem, sb.pcount, None), reads, writes)


def tile_base(d, tau):
    if d == 1:
        return 128 * tau, tau % 16
    if d == 4:
        r, n = tau // 4, tau % 4
        return r + 512 * n, n
    return tau, 0


def cols(d, tau):
    b, _ = tile_base(d, tau)
    return slice(b, b + 127 * d + 1, d)


def build(n_seq=SEQ_PER_CORE, moe=True, debug=False, G=2048, phases=("A1", "A2", "A3", "A4", "A6"), sparse=True):
    nc = bass.Bass("TRN2", target_bir_lowering=False)
    NT = n_seq * S
    dt_in = lambda name, shape: nc.dram_tensor(name, shape, F32, kind="ExternalInput").ap()
    x_d = dt_in("x", [NT, D])
    win_d = dt_in("w_in", [D, 2560])
    wout_d = dt_in("w_out", [D, D])
    cst_d = dt_in("cst", [128, 768])
    gmix_d = dt_in("gmix_b", [128, D])
    gffn_d = dt_in("gffn_b", [128, D])
    gfin_d = dt_in("gfin_b", [128, D])
    cw_d = dt_in("conv_w", [128, 4 * 31])
    cv_d = dt_in("conv_v", [128, 12])
    rt_d = dt_in("router", [D, 36])
    if sparse:
        eg_d = dt_in("e_gate", [32 * 128, 2048])
        eu_d = dt_in("e_up", [32 * 128, 2048])
        ed_d = dt_in("e_down", [32 * 128, 2048])
    else:
        eg_d = dt_in("e_gate", [32, D, 256])
        eu_d = dt_in("e_up", [32, D, 256])
        ed_d = dt_in("e_down", [32, 256, D])
    cst2_d = dt_in("cst2", [128, 450])
    NBLK = (2 * NT) // 128 + 32
    hn_d = nc.dram_tensor("hns", [NT, D], BF16, kind="Internal").ap()
    xs_d = nc.dram_tensor("xsort", [NBLK * 128, D], BF16, kind="Internal").ap()
    ys_d = nc.dram_tensor("ysort", [NBLK * 128, D], BF16, kind="Internal").ap()
    out_d = nc.dram_tensor("out", [NT, D], F32, kind="ExternalOutput").ap()
    skind = "ExternalOutput" if debug else "Internal"
    x2_d = nc.dram_tensor("x2s", [NT, D], F32, kind=skind).ap()
    hnT_d = nc.dram_tensor("hnTs", [128, 8, NT], BF16, kind=skind).ap()

    with ExitStack() as es:
        def sb(name, shape, dt=F32):
            return es.enter_context(nc.sbuf_tensor(name, shape, dt))

        def sem(name):
            return es.enter_context(nc.semaphore(name))

        PE = Eng("pe", nc.tensor, sem("s_pe"), selfsync=False)
        ACT = Eng("act", nc.scalar, sem("s_act"))
        DVE = Eng("dve", nc.vector, sem("s_dve"))
        POOL = Eng("pool", nc.gpsimd, sem("s_pool"))
        SP = Eng("sp", nc.sync, sem("s_sp"))
        engines = [PE, ACT, DVE, POOL, SP]
        T = Trk()
        nbuf = [0]

        def B(name, dma=False, psum=False):
            nbuf[0] += 1
            nm = "%s_%d" % (name, nbuf[0])
            return Buf(nm, sem if dma else None, psum)

        def mm(out, lhsT, rhs, start, stop, reads, writes):
            T.op(PE, lambda: nc.tensor.matmul(out, lhsT=lhsT, rhs=rhs, start=start, stop=stop), reads, writes)

        def barrier():
            deps = [(e.name, e.sem, e.count, None) for e in engines if e.count > 0]
            for e in engines:
                T.wait(e, deps)

        cstb = sb("cstb", [128, 768], BF16); b_cst = B("cst", True)
        identb = cstb[:, 0:128]
        onesH = [cstb[:, 128:256], cstb[:, 256:384]]
        onesM = cstb[:, 384:512]
        m2 = cstb[:, 512:768]
        gmix = sb("gmix", [128, D]); b_gmix = B("gmix", True)
        gffn = sb("gffn", [128, D]); b_gffn = B("gffn", True)
        cw = sb("cw", [128, 4, 31]); b_cw = B("cw", True)
        cv = sb("cv", [128, 12]); b_cv = B("cv", True)
        nh = sb("nh", [128, 1]); b_nh = B("nh")
        epsT = sb("epsT", [128, 1]); b_eps = B("eps")

        T.dma(POOL, b_cst, cstb[:], cst_d[:, :], writes=[b_cst])
        NTILE = NT // 128
        c2b = sb("c2b", [128, 256], BF16); b_c2b = B("c2b", True)
        c2f = sb("c2f", [128, 194]); b_c2f = B("c2f", True)
        ltri = c2b[:, 0:128]; onesF = c2b[:, 128:256]
        iota_e = c2f[:, 0:32]; iota_b = c2f[:, 32:192]; thr = c2f[:, 192:193]; pidx = c2f[:, 193:194]
        T.dma(POOL, b_c2b, c2b[:], cst2_d[:, 0:256], writes=[b_c2b])
        T.dma(SP, b_c2f, c2f[:], cst2_d[:, 256:450], writes=[b_c2f])
        zb = sb("zb", [128, D], BF16); b_zb = B("zb", True); b_xsd = B("xsd")
        T.op(POOL, lambda: nc.gpsimd.memset(zb[:], 0.0), (), [b_zb])
        zero_done = [0]

        def zero_fill(upto):
            for bz in range(zero_done[0], min(upto, NBLK)):
                T.dma(SP, b_zb, xs_d[bz * 128:(bz + 1) * 128, :], zb[:], reads=[b_zb], writes=[b_xsd])
            zero_done[0] = max(zero_done[0], min(upto, NBLK))
        rtb = sb("rtb", [128, 8, 36], BF16); b_rt = B("rt", True)
        T.dma(POOL, b_rt, rtb[:], rt_d.rearrange("(kc p) n -> p kc n", p=128), writes=[b_rt])
        eall = sb("eall", [128, 2 * NTILE]); posall = sb("posall", [128, 2 * NTILE]); wall = sb("wall", [128, 2 * NTILE])
        b_rall = B("rall")
        runb = sb("runb", [128, 32]); b_runb = B("runb")
        T.op(POOL, lambda: nc.gpsimd.memset(runb[:], 0.0), (), [b_runb])
        RS = [(sb("lg%d" % r_, [128, 36]), sb("lem%d" % r_, [128, 32]), sb("m8%d" % r_, [128, 8]), sb("sm%d" % r_, [128, 16]),
               sb("ohb%d" % r_, [128, 2, 32], BF16), sb("ohs%d" % r_, [128, 32], BF16), sb("posm%d" % r_, [128, 32]),
               sb("jk32%d" % r_, [128, 32]), sb("ppos%d" % r_, [128, 64])) for r_ in range(4)]
        b_rsl = [B("rscr") for _ in range(4)]
        T.dma(SP, b_gmix, gmix[:], gmix_d[:, :], writes=[b_gmix])
        T.dma(SP, b_gffn, gffn[:], gffn_d[:, :], writes=[b_gffn])
        T.dma(SP, b_cw, cw[:].rearrange("p a b -> p (a b)"), cw_d[:, :], writes=[b_cw])
        T.dma(SP, b_cv, cv[:], cv_d[:, :], writes=[b_cv])
        T.op(POOL, lambda: nc.gpsimd.memset(nh[:], -0.5), (), [b_nh])
        T.op(POOL, lambda: nc.gpsimd.memset(epsT[:], 1e-6), (), [b_eps])

        def rstd_from_ss(ss_ap, rstd_ap, b_ss, b_rstd):
            T.op(DVE, lambda: nc.vector.tensor_scalar(out=ss_ap, in0=ss_ap, scalar1=1.0 / D, scalar2=1e-6,
                                                     op0=ALU.mult, op1=ALU.add), [b_ss], [b_ss])
            T.op(POOL, lambda: nc.gpsimd.tensor_tensor(out=rstd_ap, in0=ss_ap, in1=nh[:], op=ALU.pow),
                 [b_ss, b_nh], [b_rstd])

        es_a = ExitStack()
        es.enter_context(es_a)

        def sba(name, shape, dt=F32):
            return es_a.enter_context(nc.sbuf_tensor(name, shape, dt))

        def psa(name, shape, dt=F32):
            return es_a.enter_context(nc.psum_tensor(name, shape, dt))

        winb = sba("winb", [128, 8, 2560], BF16); b_win = B("win", True)
        b_wout = B("wout", True)
        for kc in range(8):
            for hf in range(2):
                T.dma(POOL, b_win, winb[:, kc, hf * 1280:(hf + 1) * 1280],
                      win_d[kc * 128:(kc + 1) * 128, hf * 1280:(hf + 1) * 1280], writes=[b_win])

        b_wbf = B("wbf", True)
        PRECAST = False
        if sparse and moe and PRECAST:
            for wi, srcw in enumerate((eg_d, eu_d, ed_d)):
                for r4 in range(8):
                    T.dma(POOL, b_wbf, wbf_d[wi][r4 * 512:(r4 + 1) * 512, :], srcw[r4 * 512:(r4 + 1) * 512, :], writes=[b_wbf])

        NXS = 3
        x32 = [sba("x32_%d" % i, [128, D]) for i in range(NXS)]; b_x32 = [B("x32", True) for _ in range(NXS)]
        xn = [sba("xn_%d" % i, [128, D], BF16) for i in range(NXS)]; b_xn = [B("xn", True) for _ in range(NXS)]
        ss = [sba("ss_%d" % i, [128, 1]) for i in range(NXS)]; b_ss = [B("ss") for _ in range(NXS)]
        rstd = [sba("rstd_%d" % i, [128, 1]) for i in range(NXS)]; b_rstd = [B("rstd") for _ in range(NXS)]
        b_hnTs = []
        NHS = 6
        hnTr = [sba("hnTr_%d" % i, [128, 8, 128], BF16) for i in range(NHS)]; b_hnTr = [B("hnTr", True) for _ in range(NHS)]
        hT = sba("hT", [128, 8, S], BF16); b_hT = [B("hT") for _ in range(16)]
        woutb = hT[:, 0:4, :].rearrange("p a (b c) -> p (a b) c", c=D)
        catT = sba("catT", [128, 8, S], BF16); b_cat = [[B("cat") for _ in range(4)] for _ in range(8)]
        R1 = sba("R1", [128, 8192])
        R2 = sba("R2", [128, 3072])
        qTe = R2[:, 0:1024].bitcast(BF16); qTo = R2[:, 1024:2048].bitcast(BF16); kT = R2[:, 2048:3072].bitcast(BF16)
        b_qe = [B("qe") for _ in range(4)]; b_qo = [B("qo") for _ in range(4)]; b_k = [B("k") for _ in range(4)]
        vT = sba("vT", [128, S], BF16); b_vT = [B("vT") for _ in range(4)]
        NVS = 2
        Vz = [R1[:, 4096 + i * 2048:4096 + (i + 1) * 2048].bitcast(BF16).rearrange("p (t h c) -> p t h c", h=2, c=128)
              for i in range(NVS)]
        b_Vz = [[[B("Vz") for _ in range(2)] for _ in range(4)] for _ in range(NVS)]
        acc = R1[:, 0:4096].rearrange("p (a s) -> p a s", s=S); b_acc = B("acc")
        NPT = 8
        pt = [sba("pt_%d" % i, [128, 256], BF16) for i in range(NPT)]; b_pt = [B("pt") for _ in range(NPT)]
        u = R2[:, 0:1040].bitcast(BF16); b_u = [B("u") for _ in range(4)]; b_u0 = B("u0")
        Dg = R2[:, 1040:1040 + 1984].bitcast(BF16).rearrange("p (j c) -> p j c", c=128); b_Dg = B("Dg")
        ybf = R1[:, 0:4096].bitcast(BF16).rearrange("p (c s) -> p c s", s=S); b_y = [[B("y") for _ in range(4)] for _ in range(4)]
        sg = [R1[:, 4096 + i * 512:4096 + (i + 1) * 512] for i in range(2)]; b_sg = [B("sg") for _ in range(2)]
        mean = R1[:, 5120:5632]; b_mean = B("mean")
        var = R1[:, 5632:6144]; b_var = B("var")
        tt = [R1[:, 6144 + i * 512:6144 + (i + 1) * 512] for i in range(2)]; b_tt = [B("tt") for _ in range(2)]
        ysq = [R1[:, 7168 + i * 256:7168 + (i + 1) * 256].bitcast(BF16) for i in range(2)]; b_ysq = [B("ysq") for _ in range(2)]
        attn_bufs = [b_acc] + [b for s_ in b_Vz for g4 in s_ for b in g4] + b_qe + b_qo + b_k
        conv_bufs = [b for r_ in b_y for b in r_] + b_sg + b_ysq + [b_mean, b_var] + b_tt + b_u + [b_u0, b_Dg]

        def seed(dst, srcb):
            deps = []
            for b in srcb:
                if b.w is not None:
                    deps.append(b.w)
                deps.extend(b.r.values())
            for b in dst:
                for d_ in deps:
                    o = b.r.get(d_[0])
                    if o is None or o[2] < d_[2]:
                        b.r[d_[0]] = d_
        tp = psa("tp", [128, 8, 128], BF16); b_tp = B("tp", psum=True)
        NPJ = 3
        NSPS = 2
        pj = [psa("pj_%d" % i, [128, 512]) for i in range(NPJ)]; b_pj = [B("pj", psum=True) for _ in range(NPJ)]
        sps = [psa("sps_%d" % i, [128, 512]) for i in range(NSPS)]; b_sps = [B("sps", psum=True) for _ in range(NSPS)]
        aps = [psa("aps_%d" % i, [128, 512]) for i in range(2)]; b_aps = [B("aps", psum=True) for _ in range(2)]
        ctr = {"hs": 0, "pj": 0, "sps": 0, "aps": 0, "pt": 0, "x": 0, "sg": 0, "ysq": 0, "tt": 0, "vz": 0}

        def nxt(k, n):
            v = ctr[k] % n
            ctr[k] += 1
            return v

        def transpose_tile(src, b_src, dst, b_dst_list):
            for kc in range(8):
                T.op(PE, lambda: nc.tensor.transpose(out=tp[:, kc, :], in_=src[:, kc * 128:(kc + 1) * 128], identity=identb),
                     [b_src, b_cst], [b_tp])
            T.op(ACT, lambda: nc.scalar.copy(out=dst, in_=tp[:, :, :]), [b_tp], b_dst_list)

        def proj(fcol, tb, ps_ap, b_ps):
            for kc in range(8):
                mm(ps_ap, winb[:, kc, fcol:fcol + 128], hT[:, kc, tb * 512:(tb + 1) * 512], kc == 0, kc == 7,
                   [b_win] + b_hT[tb * 4:(tb + 1) * 4], [b_ps])

        for s in range(n_seq):
            r0 = s * S
            T.skip = "A1" not in phases
            seed(b_hT, [b_wout])
            for i in range(16):
                xs = nxt("x", NXS)
                T.dma(SP, b_x32[xs], x32[xs][:], x_d[r0 + i * 128:r0 + (i + 1) * 128, :], writes=[b_x32[xs]])
                T.op(ACT, lambda: nc.scalar.activation(out=xn[xs][:], in_=x32[xs][:], func=AF.Square, accum_out=ss[xs][:]),
                     [b_x32[xs]], [b_xn[xs], b_ss[xs]])
                rstd_from_ss(ss[xs][:], rstd[xs][:], b_ss[xs], b_rstd[xs])
                T.op(DVE, lambda: nc.vector.scalar_tensor_tensor(out=xn[xs][:], in0=x32[xs][:], scalar=rstd[xs][:], in1=gmix[:],
                                                                op0=ALU.mult, op1=ALU.mult),
                     [b_x32[xs], b_rstd[xs], b_gmix], [b_xn[xs]])
                transpose_tile(xn[xs], b_xn[xs], hT[:, :, i * 128:(i + 1) * 128], [b_hT[i]])

            T.skip = "A2" not in phases
            seed(attn_bufs, conv_bufs)
            zero_fill((NBLK * (s + 1) + n_seq - 1) // n_seq)
            for i in range(NVS):
                bl = [b for g4 in b_Vz[i] for b in g4]
                T.op(POOL, lambda: nc.gpsimd.memset(Vz[i], 0.0), (), bl)
            T.op(POOL, lambda: nc.gpsimd.memset(qTe[64:128, :], 0.0), (), b_qe)
            T.op(POOL, lambda: nc.gpsimd.memset(qTo[0:64, :], 0.0), (), b_qo)
            for hp in range(4):
                for tb in range(4):
                    p = nxt("pj", NPJ)
                    proj(1024 + hp * 128, tb, pj[p][:], b_pj[p])
                    T.op(ACT, lambda: nc.scalar.mul(out=qTe[0:64, tb * 512:(tb + 1) * 512], in_=pj[p][0:64, :], mul=0.125),
                         [b_pj[p]], [b_qe[tb]])
                    T.op(DVE, lambda: nc.vector.tensor_scalar(out=qTo[64:128, tb * 512:(tb + 1) * 512], in0=pj[p][64:128, :],
                                                             scalar1=0.125, scalar2=None, op0=ALU.mult),
                         [b_pj[p]], [b_qo[tb]])
                for tb in range(4):
                    p = nxt("pj", NPJ)
                    proj(1536 + hp * 128, tb, pj[p][:], b_pj[p])
                    T.op(DVE, lambda: nc.vector.tensor_copy(out=kT[:, tb * 512:(tb + 1) * 512], in_=pj[p][:]),
                         [b_pj[p]], [b_k[tb]])
                for tb in range(4):
                    p = nxt("pj", NPJ)
                    proj(2048 + hp * 128, tb, pj[p][:], b_pj[p])
                    T.op(ACT, lambda: nc.scalar.copy(out=vT[:, tb * 512:(tb + 1) * 512], in_=pj[p][:]), [b_pj[p]], [b_vT[tb]])
                for pi, d in enumerate(PATTERN_D):
                    vs = nxt("vz", NVS)
                    vz = Vz[vs]
                    T.skip = ("A2" not in phases) or ("noV" in phases)
                    for tg8 in range(2):
                        for t8 in range(8):
                            tau = tg8 * 8 + t8
                            T.op(PE, lambda: nc.tensor.transpose(out=tp[:, t8, :], in_=vT[:, cols(d, tau)], identity=identb),
                                 b_vT + [b_cst], [b_tp])
                        T.op(ACT, lambda: nc.scalar.copy(out=vz[:, tg8 * 8:(tg8 + 1) * 8, 0, 0:64], in_=tp[:, :, 0:64]),
                             [b_tp], [b_Vz[vs][2 * tg8][0], b_Vz[vs][2 * tg8 + 1][0]])
                        T.op(DVE, lambda: nc.vector.tensor_copy(out=vz[:, tg8 * 8:(tg8 + 1) * 8, 1, 64:128], in_=tp[:, :, 64:128]),
                             [b_tp], [b_Vz[vs][2 * tg8][1], b_Vz[vs][2 * tg8 + 1][1]])
                    T.skip = ("A2" not in phases) or ("noS" in phases)
                    items = [(tau, h) for tau in range(16) for h in range(2)]
                    st = {}
                    LOOK = 2

                    def stage_s(tau, h):
                        _, n = tile_base(d, tau)
                        has_prev = n > 0
                        cq = cols(d, tau)
                        qs, bq = (qTe, b_qe) if h == 0 else (qTo, b_qo)
                        sp_ = nxt("sps", NSPS)
                        lo = 0 if has_prev else 128
                        if has_prev:
                            mm(sps[sp_][:, 0:128], kT[:, cols(d, tau - 1)], qs[:, cq], True, True, b_k + bq, [b_sps[sp_]])
                        mm(sps[sp_][:, 128:256], kT[:, cq], qs[:, cq], True, True, b_k + bq, [b_sps[sp_]])
                        pp = nxt("pt", NPT)
                        T.op(ACT, lambda: nc.scalar.activation(out=pt[pp][:, lo:256], in_=sps[sp_][:, lo:256], func=AF.Exp),
                             [b_sps[sp_]], [b_pt[pp]])
                        T.op(POOL, lambda: nc.gpsimd.tensor_tensor(out=pt[pp][:, lo:256], in0=pt[pp][:, lo:256], in1=m2[:, lo:256],
                                                                  op=ALU.mult),
                             [b_pt[pp], b_cst], [b_pt[pp]])
                        st[(tau, h)] = (pp, has_prev)

                    def stage_pv(tau, h):
                        pp, has_prev = st.pop((tau, h))
                        if h == 0:
                            st["a"] = nxt("aps", 2)
                        a = st["a"]
                        cq = cols(d, tau)
                        kbs = ([(tau - 1, 0)] if has_prev else []) + [(tau, 1)]
                        first = (h == 0)
                        for (tk, kb) in kbs:
                            last = (h == 1 and kb == 1)
                            mm(aps[a][:, 0:128], vz[:, tk, h, :], pt[pp][:, kb * 128:(kb + 1) * 128], first, False,
                               [b_Vz[vs][tk // 4][h], b_pt[pp]], [b_aps[a]])
                            first = False
                            mm(aps[a][:, 128:256], onesH[h], pt[pp][:, kb * 128:(kb + 1) * 128], False, last,
                               [b_cst, b_pt[pp]], [b_aps[a]])
                        if h == 1:
                            av = aps[a][:, 0:256].rearrange("p (a b) -> p a b", b=128)
                            if pi == 0:
                                T.op(DVE, lambda: nc.vector.tensor_copy(out=acc[:, :, cq], in_=av), [b_aps[a]], [b_acc])
                            else:
                                T.op(DVE, lambda: nc.vector.tensor_tensor(out=acc[:, :, cq], in0=av, in1=acc[:, :, cq], op=ALU.add),
                                     [b_aps[a], b_acc], [b_acc])

                    for k in range(len(items) + LOOK):
                        if k < len(items):
                            stage_s(*items[k])
                        if k >= LOOK:
                            stage_pv(*items[k - LOOK])
                T.skip = "A2" not in phases
                T.op(DVE, lambda: nc.vector.reciprocal(out=acc[:, 1, :], in_=acc[:, 1, :]), [b_acc], [b_acc])
                T.op(DVE, lambda: nc.vector.tensor_tensor(out=catT[:, 4 + hp, :], in0=acc[:, 0, :], in1=acc[:, 1, :], op=ALU.mult),
                     [b_acc], b_cat[4 + hp])

            T.skip = "A3" not in phases
            seed(conv_bufs, attn_bufs)
            T.op(POOL, lambda: nc.gpsimd.memset(u[:, 0:30], 0.0), (), [b_u0])
            for cc in range(4):
                for j in range(31):
                    T.op(DVE, lambda: nc.vector.tensor_scalar(out=Dg[:, j, :], in0=identb, scalar1=cw[:, cc, j:j + 1], scalar2=None,
                                                             op0=ALU.mult),
                         [b_cst, b_cw], [b_Dg])
                for tb in range(4):
                    pa = nxt("pj", NPJ)
                    proj(cc * 128, tb, pj[pa][:], b_pj[pa])
                    pg = nxt("pj", NPJ)
                    proj(512 + cc * 128, tb, pj[pg][:], b_pj[pg])
                    sgi = nxt("sg", 2)
                    T.op(ACT, lambda: nc.scalar.activation(out=sg[sgi], in_=pj[pg][:], func=AF.Sigmoid), [b_pj[pg]], [b_sg[sgi]])
                    T.op(DVE, lambda: nc.vector.tensor_tensor(out=u[:, 30 + tb * 512:30 + (tb + 1) * 512], in0=pj[pa][:], in1=sg[sgi],
                                                             op=ALU.mult),
                         [b_pj[pa], b_sg[sgi]], [b_u[tb]])
                for tb in range(4):
                    pc = nxt("pj", NPJ)
                    rd = [b_Dg, b_u0] + ([b_u[tb - 1]] if tb > 0 else []) + [b_u[tb]]
                    for j in range(31):
                        mm(pj[pc][:], Dg[:, j, :], u[:, tb * 512 + j:tb * 512 + j + 512], j == 0, j == 30, rd, [b_pj[pc]])
                    T.op(ACT, lambda: nc.scalar.activation(out=ybf[:, cc, tb * 512:(tb + 1) * 512], in_=pj[pc][:], func=AF.Identity,
                                                           bias=cv[:, cc:cc + 1]),
                         [b_pj[pc], b_cv], [b_y[cc][tb]])
            T.skip = "A4" not in phases
            for tb in range(4):
                p1 = nxt("pj", NPJ)
                for cc in range(4):
                    mm(pj[p1][:], onesM, ybf[:, cc, tb * 512:(tb + 1) * 512], cc == 0, cc == 3, [b_cst, b_y[cc][tb]], [b_pj[p1]])
                p2 = nxt("pj", NPJ)
                for cc in range(4):
                    yi = nxt("ysq", 2)
                    T.op(ACT, lambda: nc.scalar.activation(out=ysq[yi], in_=ybf[:, cc, tb * 512:(tb + 1) * 512], func=AF.Square),
                         [b_y[cc][tb]], [b_ysq[yi]])
                    mm(pj[p2][:], onesM, ysq[yi], cc == 0, cc == 3, [b_cst, b_ysq[yi]], [b_pj[p2]])
                T.op(DVE, lambda: nc.vector.tensor_copy(out=mean, in_=pj[p1][:]), [b_pj[p1]], [b_mean])
                T.op(DVE, lambda: nc.vector.tensor_tensor(out=var, in0=mean, in1=mean, op=ALU.mult), [b_mean], [b_var])
                T.op(DVE, lambda: nc.vector.tensor_tensor(out=var, in0=pj[p2][:], in1=var, op=ALU.subtract),
                     [b_pj[p2], b_var], [b_var])
                T.op(ACT, lambda: nc.scalar.activation(out=var, in_=var, func=AF.Ln, bias=epsT[:]), [b_var, b_eps], [b_var])
                T.op(ACT, lambda: nc.scalar.activation(out=var, in_=var, func=AF.Exp, scale=-0.5), [b_var], [b_var])
                for cc in range(4):
                    ti = nxt("tt", 2)
                    T.op(DVE, lambda: nc.vector.tensor_tensor(out=tt[ti], in0=ybf[:, cc, tb * 512:(tb + 1) * 512], in1=mean,
                                                             op=ALU.subtract),
                         [b_y[cc][tb], b_mean], [b_tt[ti]])
                    T.op(DVE, lambda: nc.vector.tensor_tensor(out=tt[ti], in0=tt[ti], in1=var, op=ALU.mult),
                         [b_tt[ti], b_var], [b_tt[ti]])
                    T.op(ACT, lambda: nc.scalar.activation(out=catT[:, cc, tb * 512:(tb + 1) * 512], in_=tt[ti], func=AF.Silu,
                                                           scale=cv[:, 4 + cc:5 + cc], bias=cv[:, 8 + cc:9 + cc]),
                         [b_tt[ti], b_cv], [b_cat[cc][tb]])
            T.skip = "A6" not in phases
            seed([b_wout], b_hT)
            T.dma(POOL, b_wout, woutb, wout_d.rearrange("(kc p) n -> p kc n", p=128), writes=[b_wout])
            xslot = {}

            def a6_load(i):
                if i < 16 and i not in xslot:
                    xs_ = nxt("x", NXS)
                    xslot[i] = xs_
                    T.dma(SP, b_x32[xs_], x32[xs_][:], x_d[r0 + i * 128:r0 + (i + 1) * 128, :], writes=[b_x32[xs_]])

            def a6_main(i):
                a6_load(i)
                xs = xslot[i]
                hsl = nxt("hs", NHS)
                for hf in range(2):
                    p = nxt("pj", NPJ)
                    for kc in range(8):
                        mm(pj[p][:], catT[:, kc, i * 128:(i + 1) * 128], woutb[:, kc, hf * 512:(hf + 1) * 512], kc == 0, kc == 7,
                           [b_wout, b_cat[kc][i // 4]], [b_pj[p]])
                    T.op(DVE, lambda: nc.vector.tensor_tensor(out=x32[xs][:, hf * 512:(hf + 1) * 512], in0=pj[p][:],
                                                             in1=x32[xs][:, hf * 512:(hf + 1) * 512], op=ALU.add),
                         [b_pj[p], b_x32[xs]], [b_x32[xs]])
                a6_load(i + 1)
                T.dma(SP, b_x32[xs], x2_d[r0 + i * 128:r0 + (i + 1) * 128, :], x32[xs][:], reads=[b_x32[xs]])
                T.op(ACT, lambda: nc.scalar.activation(out=xn[xs][:], in_=x32[xs][:], func=AF.Square, accum_out=ss[xs][:]),
                     [b_x32[xs]], [b_xn[xs], b_ss[xs]])
                rstd_from_ss(ss[xs][:], rstd[xs][:], b_ss[xs], b_rstd[xs])
                T.op(DVE, lambda: nc.vector.scalar_tensor_tensor(out=xn[xs][:], in0=x32[xs][:], scalar=rstd[xs][:], in1=gffn[:],
                                                                op0=ALU.mult, op1=ALU.mult),
                     [b_x32[xs], b_rstd[xs], b_gffn], [b_xn[xs]])
                transpose_tile(xn[xs], b_xn[xs], hnTr[hsl][:], [b_hnTr[hsl]])
                if not sparse:
                    T.dma(SP, b_hnTr[hsl], hnT_d[:, :, r0 + i * 128:r0 + (i + 1) * 128], hnTr[hsl][:], reads=[b_hnTr[hsl]])
                else:
                    T.dma(SP, b_xn[xs], hn_d[r0 + i * 128:r0 + (i + 1) * 128, :], xn[xs][:], reads=[b_xn[xs]])
                return hsl

            def route(i, hsl, ri):
                jg = s * 16 + i
                V = nc.vector
                lg, lem, m8, sm, ohb, ohs, posm, jk32, ppos = RS[ri]
                R_ = [b_rsl[ri]]
                RA = [b_rsl[ri], b_rall]
                p = nxt("pj", NPJ)
                for kc in range(8):
                    mm(pj[p][:, 0:36], hnTr[hsl][:, kc, :], rtb[:, kc, :], kc == 0, kc == 7, [b_hnTr[hsl], b_rt], [b_pj[p]])
                T.op(DVE, lambda: V.tensor_copy(out=lg[:], in_=pj[p][:, 0:36]), [b_pj[p]], R_)
                yield
                ops = [
                    (DVE, lambda: V.reduce_max(out=sm[:, 0:1], in_=lg[:, 0:4], axis=mybir.AxisListType.X), R_, R_),
                    (DVE, lambda: V.tensor_scalar(out=sm[:, 1:2], in0=sm[:, 0:1], scalar1=-1.0, scalar2=None, op0=ALU.mult), R_, R_),
                    (ACT, lambda: nc.scalar.activation(out=sm[:, 12:16], in_=lg[:, 0:4], func=AF.Exp, bias=sm[:, 1:2], accum_out=sm[:, 2:3]),
                     R_, R_),
                    (DVE, lambda: V.reciprocal(out=sm[:, 3:4], in_=sm[:, 2:3]), R_, R_),
                    (DVE, lambda: V.tensor_scalar(out=sm[:, 8:12], in0=lg[:, 0:4], scalar1=sm[:, 0:1], scalar2=None, op0=ALU.is_equal), R_, R_),
                    (DVE, lambda: V.tensor_scalar(out=sm[:, 8:12], in0=sm[:, 8:12], scalar1=BIG, scalar2=-BIG, op0=ALU.mult, op1=ALU.add),
                     R_, R_),
                ]
                for gi in range(4):
                    ops.append((DVE, lambda gi=gi: V.tensor_scalar(out=lem[:, gi * 8:(gi + 1) * 8], in0=lg[:, 4 + gi * 8:12 + gi * 8],
                                                                  scalar1=sm[:, 8 + gi:9 + gi], scalar2=None, op0=ALU.add), R_, R_))
                ops += [
                    (DVE, lambda: V.max(out=m8[:], in_=lem[:]), R_, R_),
                    (DVE, lambda: V.tensor_tensor(out=sm[:, 4:5], in0=m8[:, 1:2], in1=m8[:, 0:1], op=ALU.subtract), R_, R_),
                    (ACT, lambda: nc.scalar.activation(out=sm[:, 5:6], in_=sm[:, 4:5], func=AF.Exp), R_, R_),
                    (DVE, lambda: V.tensor_scalar(out=sm[:, 5:6], in0=sm[:, 5:6], scalar1=1.0, scalar2=None, op0=ALU.add), R_, R_),
                    (DVE, lambda: V.reciprocal(out=sm[:, 5:6], in_=sm[:, 5:6]), R_, R_),
                    (DVE, lambda: V.tensor_tensor(out=wall[:, 2 * jg:2 * jg + 1], in0=sm[:, 5:6], in1=sm[:, 3:4], op=ALU.mult), R_, RA),
                    (DVE, lambda: V.tensor_tensor(out=wall[:, 2 * jg + 1:2 * jg + 2], in0=sm[:, 3:4], in1=wall[:, 2 * jg:2 * jg + 1],
                                                  op=ALU.subtract), RA, RA),
                ]
                for a_ in range(2):
                    ops.append((DVE, lambda a_=a_: V.tensor_scalar(out=ohb[:, a_, :], in0=lem[:], scalar1=m8[:, a_:a_ + 1], scalar2=None,
                                                                  op0=ALU.is_equal), R_, R_))
                    ops.append((DVE, lambda a_=a_: V.scalar_tensor_tensor(out=jk32[:], in0=ohb[:, a_, :], scalar=1.0, in1=iota_e,
                                                                         op0=ALU.mult, op1=ALU.mult,
                                                                         accum_out=eall[:, 2 * jg + a_:2 * jg + a_ + 1]),
                                [b_rsl[ri], b_c2f], RA))
                ops.append((DVE, lambda: V.tensor_tensor(out=ohs[:], in0=ohb[:, 0, :], in1=ohb[:, 1, :], op=ALU.add), R_, R_))
                for (e_, f_, rd_, wr_) in ops:
                    T.op(e_, f_, rd_, wr_)
                    yield
                p2 = nxt("pj", NPJ)
                mm(pj[p2][:, 0:32], ltri, ohs[:], True, True, [b_c2b, b_rsl[ri]], [b_pj[p2]])
                mm(pj[p2][:, 32:64], onesF, ohs[:], True, True, [b_c2b, b_rsl[ri]], [b_pj[p2]])
                T.op(DVE, lambda: V.tensor_copy(out=ppos[:], in_=pj[p2][:, 0:64]), [b_pj[p2]], R_)
                tails[i] = (p2, ri, jg)

            def route_tail_a(i):
                p2, ri, jg = tails[i]
                posm = RS[ri][6]
                ppos = RS[ri][8]
                T.op(DVE, lambda: nc.vector.tensor_tensor(out=posm[:], in0=ppos[:, 0:32], in1=runb[:], op=ALU.add),
                     [b_rsl[ri], b_runb], [b_rsl[ri]])
                T.op(DVE, lambda: nc.vector.tensor_tensor(out=runb[:], in0=ppos[:, 32:64], in1=runb[:], op=ALU.add),
                     [b_rsl[ri], b_runb], [b_runb])

            def route_tail_b(i, a_):
                p2, ri, jg = tails[i]
                ohb, posm, jk32 = RS[ri][4], RS[ri][6], RS[ri][7]
                T.op(DVE, lambda: nc.vector.scalar_tensor_tensor(out=jk32[:], in0=ohb[:, a_, :], scalar=1.0, in1=posm[:], op0=ALU.mult,
                                                                op1=ALU.mult, accum_out=posall[:, 2 * jg + a_:2 * jg + a_ + 1]),
                     [b_rsl[ri]], [b_rsl[ri], b_rall])

            tails = {}
            GRP = int(_os.environ.get("GRP", "4"))
            for i0 in range(0, 16, GRP):
                hsl_ = [a6_main(i) for i in range(i0, i0 + GRP)]
                if not sparse:
                    continue
                gens = [route(i0 + k_, hsl_[k_], k_) for k_ in range(GRP)]
                alive = list(gens)
                while alive:
                    nxt_alive = []
                    for g_ in alive:
                        try:
                            next(g_)
                            nxt_alive.append(g_)
                        except StopIteration:
                            pass
                    alive = nxt_alive
                for k_ in range(GRP):
                    route_tail_a(i0 + k_)
                for a_ in range(2):
                    for k_ in range(GRP):
                        route_tail_b(i0 + k_, a_)

        T.skip = False
        zero_fill(NBLK)
        last_store = [list(b.r.values()) for b in b_x32 + b_hnTr + b_xn]
        for e in engines:
            for l in last_store:
                T.wait(e, l)
        barrier()
        es_a.close()

        if moe and sparse:
            stage_b_sparse(nc, T, es, engines, B, mm, barrier, NT, NBLK, x2_d, hn_d, xs_d, ys_d, out_d, gfin_d, eg_d, eu_d, ed_d,
                           nh, b_nh, rstd_from_ss, cstb, b_cst, c2b, b_c2b, iota_e, iota_b, thr, pidx, b_c2f,
                           eall, posall, wall, b_rall, runb, b_runb, b_xsd)
        elif moe:
            stage_b(nc, T, es, engines, B, mm, barrier, n_seq, G, x2_d, hnT_d, out_d, gfin_d, rt_d, eg_d, eu_d, ed_d,
                    nh, b_nh, rstd_from_ss)
        else:
            T.wait(SP, [])
    return nc


def stage_b(nc, T, es, engines, B, mm, barrier, n_seq, G, x2_d, hnT_d, out_d, gfin_d, rt_d, eg_d, eu_d, ed_d,
            nh, b_nh, rstd_from_ss):
    PE, ACT, DVE, POOL, SP = engines
    NT = n_seq * S
    NTG = G // 128

    def sb(name, shape, dt=F32):
        return es.enter_context(nc.sbuf_tensor(name, shape, dt))

    def ps(name, shape, dt=F32):
        return es.enter_context(nc.psum_tensor(name, shape, dt))

    gfin = sb("gfin", [128, D]); b_gfin = B("gfin", True)
    T.dma(SP, b_gfin, gfin[:], gfin_d[:, :], writes=[b_gfin])
    rtb = sb("rtb", [128, 8, 36], BF16); b_rt = B("rt", True)
    T.dma(POOL, b_rt, rtb[:], rt_d.rearrange("(kc p) n -> p kc n", p=128), writes=[b_rt])
    hnT = sb("hnT", [128, 8, G], BF16); b_hnT = B("hnT", True)
    accb = sb("accb", [128, NTG, D]); b_accb = [B("accb", True) for _ in range(NTG)]
    NW = 2
    wg = [sb("wg_%d" % i, [128, 8, 256], BF16) for i in range(NW)]; b_wg = [B("wg", True) for _ in range(NW)]
    wu = [sb("wu_%d" % i, [128, 8, 256], BF16) for i in range(NW)]; b_wu = [B("wu", True) for _ in range(NW)]
    wd = [sb("wd_%d" % i, [128, 2, D], BF16) for i in range(NW)]; b_wd = [B("wd", True) for _ in range(NW)]
    hE = [sb("hE_%d" % i, [128, 2, G], BF16) for i in range(2)]
    b_hE = [[[B("hE") for _ in range(G // 512)] for _ in range(2)] for _ in range(2)]
    sgl = [sb("sgl_%d" % i, [128, 512], BF16) for i in range(2)]; b_sgl = [B("sgl") for _ in range(2)]
    call = sb("call", [128, NTG, 32]); b_call = [B("call") for _ in range(NTG)]
    lg = sb("lg", [128, 36]); b_lg = B("lg")
    lem = sb("lem", [128, 32]); b_lem = B("lem")
    oh = sb("oh", [128, 32]); b_oh = B("oh")
    m8 = sb("m8", [128, 8]); b_m8 = B("m8")
    sm = sb("sm", [128, 16]); b_sm = B("sm")
    junk = sb("junkb", [128, D], BF16); b_junk = B("junk")
    ssb = sb("ssb", [128, 1]); b_ssb = B("ssb")
    rsb = sb("rsb", [128, 1]); b_rsb = B("rsb")
    ob = [sb("ob_%d" % i, [128, D]) for i in range(2)]; b_ob = [B("ob", True) for _ in range(2)]
    pgu = [ps("pgu_%d" % i, [128, 512]) for i in range(4)]; b_pgu = [B("pgu", psum=True) for _ in range(4)]
    pdn = [ps("pdn_%d" % i, [128, 512]) for i in range(4)]; b_pdn = [B("pdn", psum=True) for _ in range(4)]
    ctr = {"gu": 0, "dn": 0, "sgl": 0, "ob": 0}

    def nxt(k, n):
        v = ctr[k] % n
        ctr[k] += 1
        return v

    def load_w(e, slot):
        T.dma(POOL, b_wg[slot], wg[slot][:], eg_d[e].rearrange("(kc p) n -> p kc n", p=128), writes=[b_wg[slot]])
        T.dma(POOL, b_wu[slot], wu[slot][:], eu_d[e].rearrange("(kc p) n -> p kc n", p=128), writes=[b_wu[slot]])
        T.dma(POOL, b_wd[slot], wd[slot][:], ed_d[e].rearrange("(kc p) n -> p kc n", p=128), writes=[b_wd[slot]])

    for g in range(NT // G):
        t0 = g * G
        load_w(0, 0)
        T.dma(SP, b_hnT, hnT[:], hnT_d[:, :, t0:t0 + G], writes=[b_hnT])
        for j in range(NTG):
            T.dma(SP, b_accb[j], accb[:, j, :], x2_d[t0 + j * 128:t0 + (j + 1) * 128, :], writes=[b_accb[j]])
        for j in range(NTG):
            p = nxt("gu", 4)
            for kc in range(8):
                mm(pgu[p][:, 0:36], hnT[:, kc, j * 128:(j + 1) * 128], rtb[:, kc, :], kc == 0, kc == 7, [b_hnT, b_rt], [b_pgu[p]])
            V = nc.vector
            T.op(DVE, lambda: V.tensor_copy(out=lg[:], in_=pgu[p][:, 0:36]), [b_pgu[p]], [b_lg])
            T.op(DVE, lambda: V.reduce_max(out=sm[:, 0:1], in_=lg[:, 0:4], axis=mybir.AxisListType.X), [b_lg], [b_sm])
            T.op(DVE, lambda: V.tensor_scalar(out=sm[:, 1:2], in0=sm[:, 0:1], scalar1=-1.0, scalar2=None, op0=ALU.mult), [b_sm], [b_sm])
            T.op(ACT, lambda: nc.scalar.activation(out=sm[:, 12:16], in_=lg[:, 0:4], func=AF.Exp, bias=sm[:, 1:2], accum_out=sm[:, 2:3]),
                 [b_lg, b_sm], [b_sm])
            T.op(DVE, lambda: V.reciprocal(out=sm[:, 3:4], in_=sm[:, 2:3]), [b_sm], [b_sm])
            T.op(DVE, lambda: V.tensor_scalar(out=sm[:, 8:12], in0=lg[:, 0:4], scalar1=sm[:, 0:1], scalar2=None, op0=ALU.is_equal),
                 [b_lg, b_sm], [b_sm])
            T.op(DVE, lambda: V.tensor_scalar(out=sm[:, 8:12], in0=sm[:, 8:12], scalar1=BIG, scalar2=-BIG, op0=ALU.mult, op1=ALU.add),
                 [b_sm], [b_sm])
            for gi in range(4):
                T.op(DVE, lambda: V.tensor_scalar(out=lem[:, gi * 8:(gi + 1) * 8], in0=lg[:, 4 + gi * 8:12 + gi * 8],
                                                 scalar1=sm[:, 8 + gi:9 + gi], scalar2=None, op0=ALU.add),
                     [b_lg, b_sm], [b_lem])
            T.op(DVE, lambda: V.max(out=m8[:], in_=lem[:]), [b_lem], [b_m8])
            T.op(DVE, lambda: V.tensor_tensor(out=sm[:, 4:5], in0=m8[:, 1:2], in1=m8[:, 0:1], op=ALU.subtract), [b_m8, b_sm], [b_sm])
            T.op(ACT, lambda: nc.scalar.activation(out=sm[:, 5:6], in_=sm[:, 4:5], func=AF.Exp), [b_sm], [b_sm])
            T.op(DVE, lambda: V.tensor_scalar(out=sm[:, 5:6], in0=sm[:, 5:6], scalar1=1.0, scalar2=None, op0=ALU.add), [b_sm], [b_sm])
            T.op(DVE, lambda: V.reciprocal(out=sm[:, 5:6], in_=sm[:, 5:6]), [b_sm], [b_sm])
            T.op(DVE, lambda: V.tensor_tensor(out=sm[:, 6:7], in0=sm[:, 5:6], in1=sm[:, 3:4], op=ALU.mult), [b_sm], [b_sm])
            T.op(DVE, lambda: V.tensor_tensor(out=sm[:, 7:8], in0=sm[:, 3:4], in1=sm[:, 6:7], op=ALU.subtract), [b_sm], [b_sm])
            T.op(DVE, lambda: V.tensor_scalar(out=oh[:], in0=lem[:], scalar1=m8[:, 0:1], scalar2=sm[:, 6:7], op0=ALU.is_equal, op1=ALU.mult),
                 [b_lem, b_m8, b_sm], [b_oh])
            T.op(DVE, lambda: V.tensor_scalar(out=call[:, j, :], in0=lem[:], scalar1=m8[:, 1:2], scalar2=sm[:, 7:8], op0=ALU.is_equal,
                                             op1=ALU.mult),
                 [b_lem, b_m8, b_sm], [b_call[j]])
            T.op(DVE, lambda: V.tensor_tensor(out=call[:, j, :], in0=call[:, j, :], in1=oh[:], op=ALU.add), [b_call[j], b_oh], [b_call[j]])
        for e in range(32):
            sl = e % NW
            if e + 1 < 32:
                load_w(e + 1, (e + 1) % NW)
            hs = e % 2
            for dc in range(2):
                for tb in range(G // 512):
                    pg = nxt("gu", 4)
                    for kc in range(8):
                        mm(pgu[pg][:], wg[sl][:, kc, dc * 128:(dc + 1) * 128], hnT[:, kc, tb * 512:(tb + 1) * 512], kc == 0, kc == 7,
                           [b_wg[sl], b_hnT], [b_pgu[pg]])
                    pu = nxt("gu", 4)
                    for kc in range(8):
                        mm(pgu[pu][:], wu[sl][:, kc, dc * 128:(dc + 1) * 128], hnT[:, kc, tb * 512:(tb + 1) * 512], kc == 0, kc == 7,
                           [b_wu[sl], b_hnT], [b_pgu[pu]])
                    si = nxt("sgl", 2)
                    T.op(ACT, lambda: nc.scalar.activation(out=sgl[si][:], in_=pgu[pg][:], func=AF.Silu), [b_pgu[pg]], [b_sgl[si]])
                    T.op(DVE, lambda: nc.vector.tensor_tensor(out=hE[hs][:, dc, tb * 512:(tb + 1) * 512], in0=pgu[pu][:], in1=sgl[si][:],
                                                             op=ALU.mult),
                         [b_pgu[pu], b_sgl[si]], [b_hE[hs][dc][tb]])
            for j in range(NTG):
                for hf in range(2):
                    pd = nxt("dn", 4)
                    for dc in range(2):
                        mm(pdn[pd][:], hE[hs][:, dc, j * 128:(j + 1) * 128], wd[sl][:, dc, hf * 512:(hf + 1) * 512], dc == 0, dc == 1,
                           [b_hE[hs][dc][j // 4], b_wd[sl]], [b_pdn[pd]])
                    T.op(DVE, lambda: nc.vector.scalar_tensor_tensor(out=accb[:, j, hf * 512:(hf + 1) * 512], in0=pdn[pd][:],
                                                                    scalar=call[:, j, e:e + 1],
                                                                    in1=accb[:, j, hf * 512:(hf + 1) * 512], op0=ALU.mult, op1=ALU.add),
                         [b_pdn[pd], b_call[j], b_accb[j]], [b_accb[j]])
        for j in range(NTG):
            T.op(ACT, lambda: nc.scalar.activation(out=junk[:], in_=accb[:, j, :], func=AF.Square, accum_out=ssb[:]),
                 [b_accb[j]], [b_junk, b_ssb])
            rstd_from_ss(ssb[:], rsb[:], b_ssb, b_rsb)
            oi = nxt("ob", 2)
            T.op(DVE, lambda: nc.vector.scalar_tensor_tensor(out=ob[oi][:], in0=accb[:, j, :], scalar=rsb[:], in1=gfin[:],
                                                            op0=ALU.mult, op1=ALU.mult),
                 [b_accb[j], b_rsb, b_gfin], [b_ob[oi]])
            T.dma(SP, b_ob[oi], out_d[t0 + j * 128:t0 + (j + 1) * 128, :], ob[oi][:], reads=[b_ob[oi]])
    fin = [list(b.r.values()) for b in b_ob]
    for l in fin:
        T.wait(SP, l)
    barrier()


def stage_b_sparse(nc, T, es, engines, B, mm, barrier, NT, NBLK, x2_d, hn_d, xs_d, ys_d, out_d, gfin_d, eg_d, eu_d, ed_d,
                   nh, b_nh, rstd_from_ss, cstb, b_cst, c2b, b_c2b, iota_e, iota_b, thr, pidx, b_c2f,
                   eall, posall, wall, b_rall, runb, b_runb, b_xsd):
    PE, ACT, DVE, POOL, SP = engines
    I32 = mybir.dt.int32
    NTILE = NT // 128
    V = nc.vector
    identb = cstb[:, 0:128]
    onesF = c2b[:, 128:256]

    def sb(name, shape, dt=F32):
        return es.enter_context(nc.sbuf_tensor(name, shape, dt))

    def ps(name, shape, dt=F32):
        return es.enter_context(nc.psum_tensor(name, shape, dt))

    IOff = bass.IndirectOffsetOnAxis
    gfin = sb("gfin", [128, D]); b_gfin = B("gfin", True)
    T.dma(SP, b_gfin, gfin[:], gfin_d[:, :], writes=[b_gfin])
    cmpb = sb("cmpb", [128, 32], BF16); nblk = sb("nblk", [128, 32]); incl = sb("incl", [128, 32]); pstart = sb("pstart", [128, 32])
    one32 = sb("one32", [128, 32]); ebf = sb("ebf", [128, 160]); idxw = sb("idxw", [128, 160], I32)
    destf = sb("destf", [128, 2 * NTILE]); desti = sb("desti", [128, 2 * NTILE], I32)
    oht = sb("oht", [128, 32]); jk = sb("jkb", [128, 32])
    b_t = B("btab")
    ptab = ps("ptab", [128, 512]); b_ptab = B("ptab", psum=True)
    TB = [b_t]
    T.op(DVE, lambda: V.tensor_scalar(out=cmpb[:], in0=runb[:], scalar1=thr, scalar2=None, op0=ALU.is_gt), [b_runb, b_c2f], TB)
    mm(ptab[:, 0:32], onesF, cmpb[:], True, True, [b_c2b, b_t], [b_ptab])
    T.op(DVE, lambda: V.tensor_copy(out=nblk[:], in_=ptab[:, 0:32]), [b_ptab], TB)
    T.op(POOL, lambda: nc.gpsimd.memset(one32[:], 1.0), (), TB)
    T.op(DVE, lambda: V.tensor_tensor_scan(out=incl[:], data0=one32[:], data1=nblk[:], initial=0.0, op0=ALU.mult, op1=ALU.add), TB, TB)
    T.op(DVE, lambda: V.tensor_tensor(out=pstart[:], in0=incl[:], in1=nblk[:], op=ALU.subtract), TB, TB)
    T.op(DVE, lambda: V.tensor_scalar(out=pstart[:], in0=pstart[:], scalar1=128.0, scalar2=None, op0=ALU.mult), TB, TB)
    T.op(POOL, lambda: nc.gpsimd.memset(ebf[:], 0.0), (), TB)
    for e in range(32):
        T.op(DVE, lambda: V.scalar_tensor_tensor(out=ebf[:], in0=iota_b, scalar=incl[:, e:e + 1], in1=ebf[:], op0=ALU.is_ge, op1=ALU.add),
             [b_c2f, b_t], TB)
    T.op(DVE, lambda: V.tensor_scalar(out=ebf[:], in0=ebf[:], scalar1=31.0, scalar2=None, op0=ALU.min), TB, TB)
    eqf = sb("eqf", [128, 160])
    T.op(POOL, lambda: nc.gpsimd.memset(eqf[:], 0.0), (), TB)
    T.op(DVE, lambda: V.tensor_tensor(out=eqf[:, 2:160], in0=ebf[:, 2:160], in1=ebf[:, 0:158], op=ALU.is_equal), TB, TB)
    T.op(DVE, lambda: V.tensor_scalar(out=ebf[:], in0=ebf[:], scalar1=128.0, scalar2=pidx, op0=ALU.mult, op1=ALU.add), [b_t, b_c2f], TB)
    T.op(DVE, lambda: V.scalar_tensor_tensor(out=ebf[:], in0=eqf[:], scalar=8192.0, in1=ebf[:], op0=ALU.mult, op1=ALU.add), TB, TB)
    T.op(DVE, lambda: V.tensor_copy(out=idxw[:], in_=ebf[:]), TB, TB)
    NH = 3
    hnb = [sb("hnb_%d" % i, [128, D], BF16) for i in range(NH)]; b_hnb = [B("hnb", True) for _ in range(NH)]
    b_dst = [B("dst") for _ in range(NTILE)]
    for j in range(NTILE):
        hs = j % NH
        T.dma(SP, b_hnb[hs], hnb[hs][:], hn_d[j * 128:(j + 1) * 128, :], writes=[b_hnb[hs]])
        for k in (2 * j, 2 * j + 1):
            T.op(DVE, lambda: V.tensor_scalar(out=oht[:], in0=iota_e, scalar1=eall[:, k:k + 1], scalar2=None, op0=ALU.is_equal),
                 [b_c2f, b_rall], TB)
            T.op(DVE, lambda: V.scalar_tensor_tensor(out=jk[:], in0=oht[:], scalar=1.0, in1=pstart[:], op0=ALU.mult, op1=ALU.mult,
                                                    accum_out=destf[:, k:k + 1]), TB, TB)
        T.op(DVE, lambda: V.tensor_tensor(out=destf[:, 2 * j:2 * j + 2], in0=destf[:, 2 * j:2 * j + 2], in1=posall[:, 2 * j:2 * j + 2],
                                         op=ALU.add), [b_t, b_rall], TB)
        T.op(DVE, lambda: V.tensor_copy(out=desti[:, 2 * j:2 * j + 2], in_=destf[:, 2 * j:2 * j + 2]), TB, [b_dst[j]])
        for a_ in range(2):
            idma(T, POOL, b_hnb[hs], xs_d[:, :], IOff(ap=desti[:, 2 * j + a_:2 * j + a_ + 1], axis=0), hnb[hs][:], None,
                 reads=[b_hnb[hs], b_dst[j], b_xsd], writes=[])
    T.wait(SP, [d_ for b in b_hnb for d_ in b.r.values()])
    NW = 2
    wgu = [[sb("wgu_%d_%d" % (i, q), [128, 8, 256], BF16) for q in range(2)] for i in range(NW)]
    b_wgu = [[B("wgu", True) for _ in range(2)] for _ in range(NW)]
    wd = [sb("wd_%d" % i, [128, 2, D], BF16) for i in range(NW)]; b_wd = [B("wd", True) for _ in range(NW)]
    NX = 4
    xb = [sb("xb_%d" % i, [128, D], BF16) for i in range(NX)]; b_xb = [B("xb", True) for _ in range(NX)]
    sgl = [sb("sgl_%d" % i, [128, 256]) for i in range(2)]; b_sgl = [B("sgl") for _ in range(2)]
    hTok = [sb("hTok_%d" % i, [128, 256], BF16) for i in range(2)]; b_hTok = [B("hTok") for _ in range(2)]
    hE = [sb("hE_%d" % i, [128, 2, 128], BF16) for i in range(2)]; b_hE = [B("hE") for _ in range(2)]
    yb = [sb("yb_%d" % i, [128, D], BF16) for i in range(2)]; b_yb = [B("yb", True) for _ in range(2)]
    tpb = ps("tpb", [128, 8, 128], BF16); b_tpb = B("tpb", psum=True)
    tp2 = ptab[:, 0:128].bitcast(BF16).rearrange("p (a b) -> p a b", b=128)
    gu = [ps("gu_%d" % i, [128, 512]) for i in range(2)]; b_gu = [B("gu", psum=True) for _ in range(2)]
    yp = [[ps("yp_%d_%d" % (i, h), [128, 512]) for h in range(2)] for i in range(2)]
    b_yp = [[B("yp", psum=True) for h in range(2)] for i in range(2)]

    bc_reg = nc.gpsimd.to_reg(32 * 128 - 1)
    kw = dict(bounds_check=bc_reg, oob_is_err=False)
    eg3 = eg_d.rearrange("r (k n) -> r k n", n=256)
    eu3 = eu_d.rearrange("r (k n) -> r k n", n=256)

    def load_gu(b):
        if b >= NBLK:
            return
        sl = b % NW
        io = IOff(ap=idxw[:, b:b + 1], axis=0)
        idma(T, POOL, b_wgu[sl][0], wgu[sl][0][:].rearrange("p a b -> p (a b)"), None, eg_d[:, :], io, reads=[b_t],
             writes=[b_wgu[sl][0]], **kw)
        idma(T, POOL, b_wgu[sl][1], wgu[sl][1][:].rearrange("p a b -> p (a b)"), None, eu_d[:, :], io, reads=[b_t],
             writes=[b_wgu[sl][1]], **kw)

    def load_d(b):
        if b >= NBLK:
            return
        sl = b % NW
        io = IOff(ap=idxw[:, b:b + 1], axis=0)
        idma(T, POOL, b_wd[sl], wd[sl][:].rearrange("p a b -> p (a b)"), None, ed_d[:, :], io, reads=[b_t], writes=[b_wd[sl]], **kw)

    def load_x(b):
        if b >= NBLK:
            return
        xi = b % NX
        T.dma(SP, b_xb[xi], xb[xi][:], xs_d[b * 128:(b + 1) * 128, :], writes=[b_xb[xi]])

    NXT = 3
    xT = [sb("xTp_%d" % i, [128, 8, 128], BF16) for i in range(NXT)]; b_xT = [B("xT") for _ in range(NXT)]

    def st_T(k):
        xi = k % NX; ti = k % NXT
        for kc in range(8):
            T.op(PE, lambda: nc.tensor.transpose(out=tpb[:, kc, :], in_=xb[xi][:, kc * 128:(kc + 1) * 128], identity=identb),
                 [b_xb[xi], b_cst], [b_tpb])
        T.op(ACT, lambda: nc.scalar.copy(out=xT[ti][:], in_=tpb[:, :, :]), [b_tpb], [b_xT[ti]])

    def st_GU(k):
        tx = k % NXT; ti = k % 2; sl = k % NW
        for q in range(2):
            for kc in range(8):
                mm(gu[ti][:, q * 256:(q + 1) * 256], xT[tx][:, kc, :], wgu[sl][q][:, kc, :], kc == 0, kc == 7,
                   [b_xT[tx], b_wgu[sl][q]], [b_gu[ti]])
        T.op(ACT, lambda: nc.scalar.activation(out=sgl[ti][:], in_=gu[ti][:, 0:256], func=AF.Silu), [b_gu[ti]], [b_sgl[ti]])
        T.op(DVE, lambda: V.tensor_tensor(out=hTok[ti][:], in0=gu[ti][:, 256:512], in1=sgl[ti][:], op=ALU.mult),
             [b_gu[ti], b_sgl[ti]], [b_hTok[ti]])

    def st_T2(k):
        ti = k % 2
        for dc in range(2):
            T.op(PE, lambda: nc.tensor.transpose(out=tp2[:, dc, :], in_=hTok[ti][:, dc * 128:(dc + 1) * 128], identity=identb),
                 [b_hTok[ti], b_cst], [b_ptab])
        T.op(DVE, lambda: V.tensor_copy(out=hE[ti][:], in_=tp2), [b_ptab], [b_hE[ti]])

    def st_DN(k):
        ti = k % 2; sl = k % NW
        for hf in range(2):
            for dc in range(2):
                mm(yp[ti][hf][:], hE[ti][:, dc, :], wd[sl][:, dc, hf * 512:(hf + 1) * 512], dc == 0, dc == 1,
                   [b_hE[ti], b_wd[sl]], [b_yp[ti][hf]])
        T.op(ACT, lambda: nc.scalar.copy(out=yb[ti][:, 0:512], in_=yp[ti][0][:]), [b_yp[ti][0]], [b_yb[ti]])
        T.op(DVE, lambda: V.tensor_copy(out=yb[ti][:, 512:1024], in_=yp[ti][1][:]), [b_yp[ti][1], b_yb[ti]], [b_yb[ti]])
        T.dma(SP, b_yb[ti], ys_d[k * 128:(k + 1) * 128, :], yb[ti][:], reads=[b_yb[ti]])

    for b0 in range(2):
        load_gu(b0); load_d(b0)
    for b0 in range(3):
        load_x(b0)
    for it in range(NBLK + 3):
        load_x(it + 3)
        if it < NBLK:
            st_T(it)
        if 0 <= it - 1 < NBLK:
            st_GU(it - 1)
            load_gu(it + 1)
        if 0 <= it - 2 < NBLK:
            st_T2(it - 2)
        if 0 <= it - 3 < NBLK:
            st_DN(it - 3)
            load_d(it - 1)
    T.wait(POOL, [d_ for b in b_yb for d_ in b.r.values()])
    NC_ = 6
    xa = [sb("xa_%d" % i, [128, D]) for i in range(NC_)]; b_xa = [B("xa", True) for _ in range(NC_)]
    y1 = [sb("y1_%d" % i, [128, D], BF16) for i in range(NC_)]; b_y1 = [B("y1", True) for _ in range(NC_)]
    y2 = [sb("y2_%d" % i, [128, D], BF16) for i in range(NC_)]; b_y2 = [B("y2", True) for _ in range(NC_)]
    ob = [sb("ob_%d" % i, [128, D]) for i in range(NC_)]; b_ob = [B("ob", True) for _ in range(NC_)]
    junk = sb("junkb", [128, D], BF16); b_junk = B("junk")
    ssb = [sb("ssb_%d" % i, [128, 1]) for i in range(NC_)]; b_ssb = [B("ssb") for _ in range(NC_)]
    rsb = [sb("rsb_%d" % i, [128, 1]) for i in range(NC_)]; b_rsb = [B("rsb") for _ in range(NC_)]
    for j in range(NTILE):
        c = j % NC_
        T.dma(SP, b_xa[c], xa[c][:], x2_d[j * 128:(j + 1) * 128, :], writes=[b_xa[c]])
        idma(T, POOL, b_y1[c], y1[c][:], None, ys_d[:, :], IOff(ap=desti[:, 2 * j:2 * j + 1], axis=0), reads=[b_dst[j]], writes=[b_y1[c]])
        idma(T, POOL, b_y2[c], y2[c][:], None, ys_d[:, :], IOff(ap=desti[:, 2 * j + 1:2 * j + 2], axis=0), reads=[b_dst[j]], writes=[b_y2[c]])
        T.op(DVE, lambda: V.scalar_tensor_tensor(out=xa[c][:], in0=y1[c][:], scalar=wall[:, 2 * j:2 * j + 1], in1=xa[c][:],
                                                op0=ALU.mult, op1=ALU.add), [b_y1[c], b_rall, b_xa[c]], [b_xa[c]])
        T.op(DVE, lambda: V.scalar_tensor_tensor(out=xa[c][:], in0=y2[c][:], scalar=wall[:, 2 * j + 1:2 * j + 2], in1=xa[c][:],
                                                op0=ALU.mult, op1=ALU.add), [b_y2[c], b_rall, b_xa[c]], [b_xa[c]])
        T.op(ACT, lambda: nc.scalar.activation(out=junk[:], in_=xa[c][:], func=AF.Square, accum_out=ssb[c][:]),
             [b_xa[c]], [b_junk, b_ssb[c]])
        rstd_from_ss(ssb[c][:], rsb[c][:], b_ssb[c], b_rsb[c])
        T.op(DVE, lambda: V.scalar_tensor_tensor(out=ob[c][:], in0=xa[c][:], scalar=rsb[c][:], in1=gfin[:], op0=ALU.mult, op1=ALU.mult),
             [b_xa[c], b_rsb[c], b_gfin], [b_ob[c]])
        T.dma(ACT, b_ob[c], out_d[j * 128:(j + 1) * 128, :], ob[c][:], reads=[b_ob[c]])
    for b in b_ob:
        T.wait(SP, list(b.r.values()))
        T.wait(ACT, list(b.r.values()))
    barrier()


def make_consts():
    c = np.zeros((128, 768), np.float32)
    c[:, 0:128] = np.eye(128, dtype=np.float32)
    c[:, 128:192] = 1.0
    c[:, 256 + 64:384] = 1.0
    c[:, 384:512] = 1.0 / 512.0
    kj = np.arange(128)[:, None]
    qi = np.arange(128)[None, :]
    c[:, 512:640] = (kj >= qi)
    c[:, 640:768] = (kj <= qi)
    return c


def make_consts2():
    c = np.zeros((128, 450), np.float32)
    tp_ = np.arange(128)[:, None]
    t_ = np.arange(128)[None, :]
    c[:, 0:128] = (tp_ < t_)
    c[:, 128:256] = 1.0
    c[:, 256:288] = np.arange(32)[None, :]
    c[:, 288:448] = np.arange(160)[None, :]
    c[:, 448] = 128.0 * np.arange(128)
    c[:, 449] = np.arange(128)
    return c


def host_layout(inp, n_seq=SEQ_PER_CORE, sparse=True):
    f = lambda a: np.ascontiguousarray(np.asarray(a, dtype=np.float32))
    bc = lambda v: f(np.broadcast_to(np.asarray(v, np.float32).reshape(1, D), (128, D)))
    cwt = np.asarray(inp["conv_dw_w"], np.float32)[0]
    conv_w = f(cwt.T.reshape(4, 128, 31).transpose(1, 0, 2).reshape(128, 124))
    pc = lambda v: np.asarray(v, np.float32).reshape(4, 128).T
    conv_v = f(np.concatenate([pc(inp["conv_dw_b"]), pc(inp["conv_ln_g"]), pc(inp["conv_ln_b"])], axis=1))
    router = f(np.concatenate([np.asarray(inp["router_group"], np.float32)[0],
                               np.asarray(inp["router_expert"], np.float32)[0].reshape(D, 32)], axis=1))
    shared = {
        "w_in": f(inp["w_in"][0]), "w_out": f(inp["w_out"][0]), "cst": make_consts(),
        "gmix_b": bc(inp["norm_mix_g"]), "gffn_b": bc(inp["norm_ffn_g"]), "gfin_b": bc(inp["norm_final_g"]),
        "conv_w": conv_w, "conv_v": conv_v, "router": router,
        "cst2": make_consts2(),
    }
    if sparse:
        lay = lambda w, kcn: f(np.asarray(w, np.float32)[0].reshape(32, kcn, 128, -1).transpose(0, 2, 1, 3).reshape(32 * 128, 2048))
        shared.update({"e_gate": lay(inp["expert_w_gate"], 8), "e_up": lay(inp["expert_w_up"], 8), "e_down": lay(inp["expert_w_down"], 2)})
    else:
        shared.update({"e_gate": f(inp["expert_w_gate"][0]), "e_up": f(inp["expert_w_up"][0]), "e_down": f(inp["expert_w_down"][0])})
    return shared


def kernel(x, norm_mix_g, w_in, conv_dw_w, conv_dw_b, conv_ln_g, conv_ln_b, w_out, norm_ffn_g, router_group,
           router_expert, expert_w_gate, expert_w_up, expert_w_down, norm_final_g):
    inp = dict(norm_mix_g=norm_mix_g, w_in=w_in, conv_dw_w=conv_dw_w, conv_dw_b=conv_dw_b, conv_ln_g=conv_ln_g,
               conv_ln_b=conv_ln_b, w_out=w_out, norm_ffn_g=norm_ffn_g, router_group=router_group,
               router_expert=router_expert, expert_w_gate=expert_w_gate, expert_w_up=expert_w_up,
               expert_w_down=expert_w_down, norm_final_g=norm_final_g)
    shared = host_layout(inp)
    xf = np.asarray(x, dtype=np.float32)
    nc = build()
    in_maps = []
    for c in range(NCORE):
        m = dict(shared)
        m["x"] = np.ascontiguousarray(xf[c * SEQ_PER_CORE:(c + 1) * SEQ_PER_CORE].reshape(SEQ_PER_CORE * S, D))
        in_maps.append(m)
    res = run_bass_kernel_spmd(nc, in_maps, core_ids=list(range(NCORE)))
    out = np.concatenate([np.asarray(r["out"], dtype=np.float32).reshape(SEQ_PER_CORE, S, D) for r in res.results], axis=0)
    return out
```

```python
import numpy as np
from contextlib import ExitStack
import concourse.bass as bass
import concourse.mybir as mybir
from concourse.bass_utils import run_bass_kernel_spmd

F32 = mybir.dt.float32
BF16 = mybir.dt.bfloat16
AF = mybir.ActivationFunctionType
ALU = mybir.AluOpType

S = 2048
D = 1024
NCORE = 8
SEQ_PER_CORE = 4
import os as _os
PATTERN_D = tuple(int(v) for v in _os.environ.get("KPD", "1,4,16").split(","))
BIG = 30000.0


class Eng:
    def __init__(self, name, h, sem, selfsync=True):
        self.name = name; self.h = h; self.sem = sem; self.count = 0
        self.seen = {}; self.selfsync = selfsync


class Buf:
    def __init__(self, name, mk=None, psum=False):
        self.name = name; self.w = None; self.r = {}
        self.mk = mk; self._d = None; self._p = None
        self.dcount = 0; self.psum = psum; self.pcount = 0

    @property
    def dsem(self):
        if self._d is None:
            self._d = self.mk("d_" + self.name)
        return self._d

    @property
    def psem(self):
        if self._p is None:
            self._p = self.mk("p_" + self.name)
        return self._p


class Trk:
    def wait(self, eng, deps):
        for d in deps:
            if d is None:
                continue
            key, sem, val, src = d
            if src is eng and not eng.selfsync:
                continue
            if eng.seen.get(key, 0) >= val:
                continue
            eng.h.wait_ge(sem, val)
            eng.seen[key] = val

    def deps(self, reads, writes):
        deps = []
        for b in reads:
            deps.append(b.w)
            if b.psum:
                deps.extend(b.r.values())
        for b in writes:
            deps.append(b.w)
            deps.extend(b.r.values())
        return deps

    def mark(self, d, reads, writes):
        for b in reads:
            o = b.r.get(d[0])
            if o is None or o[2] < d[2]:
                b.r[d[0]] = d
        for b in writes:
            b.w = d; b.r = {}

    skip = False

    def op(self, eng, fn, reads=(), writes=()):
        if self.skip:
            return
        self.wait(eng, self.deps(reads, writes))
        inst = fn()
        eng.count += 1
        inst.then_inc(eng.sem, 1)
        self.mark((eng.name, eng.sem, eng.count, eng), reads, writes)

    def dma(self, eng, sb, out, in_, reads=(), writes=(), **kw):
        if self.skip:
            return
        self.wait(eng, self.deps(reads, writes))
        inst = eng.h.dma_start(out=out, in_=in_, **kw)
        if eng.name == "pool":
            sb.pcount += 16
            inst.then_inc(sb.psem, 16)
            self.mark(("p" + sb.name, sb.psem, sb.pcount, None), reads, writes)
        else:
            sb.dcount += 16
            inst.then_inc(sb.dsem, 16)
            self.mark(("d" + sb.name, sb.dsem, sb.dcount, None), reads, writes)


def idma(T, eng, sb, out, out_off, in_, in_off, reads=(), writes=(), **kw):
    if T.skip:
        return
    T.wait(eng, T.deps(reads, writes))
    inst = eng.h.indirect_dma_start(out=out, out_offset=out_off, in_=in_, in_offset=in_off, **kw)
    sb.pcount += 16
    inst.then_inc(sb.psem, 16)
    T.mark(("p" + sb.name, sb.psem, sb.pcount, None), reads, writes)


def tile_base(d, tau):
    if d == 1:
        return 128 * tau, tau % 16
    if d == 4:
        r, n = tau // 4, tau % 4
        return r + 512 * n, n
    return tau, 0


def cols(d, tau):
    b, _ = tile_base(d, tau)
    return slice(b, b + 127 * d + 1, d)


def build(n_seq=SEQ_PER_CORE, moe=True, debug=False, G=2048, phases=("A1", "A2", "A3", "A4", "A6"), sparse=True):
    nc = bass.Bass("TRN2", target_bir_lowering=False)
    NT = n_seq * S
    dt_in = lambda name, shape: nc.dram_tensor(name, shape, F32, kind="ExternalInput").ap()
    x_d = dt_in("x", [NT, D])
    win_d = dt_in("w_in", [D, 2560])
    wout_d = dt_in("w_out", [D, D])
    cst_d = dt_in("cst", [128, 768])
    gmix_d = dt_in("gmix_b", [128, D])
    gffn_d = dt_in("gffn_b", [128, D])
    gfin_d = dt_in("gfin_b", [128, D])
    cw_d = dt_in("conv_w", [128, 4 * 31])
    cv_d = dt_in("conv_v", [128, 12])
    rt_d = dt_in("router", [D, 36])
    if sparse:
        eg_d = dt_in("e_gate", [32 * 128, 2048])
        eu_d = dt_in("e_up", [32 * 128, 2048])
        ed_d = dt_in("e_down", [32 * 128, 2048])
    else:
        eg_d = dt_in("e_gate", [32, D, 256])
        eu_d = dt_in("e_up", [32, D, 256])
        ed_d = dt_in("e_down", [32, 256, D])
    cst2_d = dt_in("cst2", [128, 450])
    NBLK = (2 * NT) // 128 + 32
    hn_d = nc.dram_tensor("hns", [NT, D], BF16, kind="Internal").ap()
    xs_d = nc.dram_tensor("xsort", [NBLK * 128, D], BF16, kind="Internal").ap()
    ys_d = nc.dram_tensor("ysort", [NBLK * 128, D], BF16, kind="Internal").ap()
    out_d = nc.dram_tensor("out", [NT, D], F32, kind="ExternalOutput").ap()
    skind = "ExternalOutput" if debug else "Internal"
    x2_d = nc.dram_tensor("x2s", [NT, D], F32, kind=skind).ap()
    hnT_d = nc.dram_tensor("hnTs", [128, 8, NT], BF16, kind=skind).ap()

    with ExitStack() as es:
        def sb(name, shape, dt=F32):
            return es.enter_context(nc.sbuf_tensor(name, shape, dt))

        def sem(name):
            return es.enter_context(nc.semaphore(name))

        PE = Eng("pe", nc.tensor, sem("s_pe"), selfsync=False)
        ACT = Eng("act", nc.scalar, sem("s_act"))
        DVE = Eng("dve", nc.vector, sem("s_dve"))
        POOL = Eng("pool", nc.gpsimd, sem("s_pool"))
        SP = Eng("sp", nc.sync, sem("s_sp"))
        engines = [PE, ACT, DVE, POOL, SP]
        T = Trk()
        nbuf = [0]

        def B(name, dma=False, psum=False):
            nbuf[0] += 1
            nm = "%s_%d" % (name, nbuf[0])
            return Buf(nm, sem if dma else None, psum)

        def mm(out, lhsT, rhs, start, stop, reads, writes):
            T.op(PE, lambda: nc.tensor.matmul(out, lhsT=lhsT, rhs=rhs, start=start, stop=stop), reads, writes)

        def barrier():
            deps = [(e.name, e.sem, e.count, None) for e in engines if e.count > 0]
            for e in engines:
                T.wait(e, deps)

        cstb = sb("cstb", [128, 768], BF16); b_cst = B("cst", True)
        identb = cstb[:, 0:128]
        onesH = [cstb[:, 128:256], cstb[:, 256:384]]
        onesM = cstb[:, 384:512]
        m2 = cstb[:, 512:768]
        gmix = sb("gmix", [128, D]); b_gmix = B("gmix", True)
        gffn = sb("gffn", [128, D]); b_gffn = B("gffn", True)
        cw = sb("cw", [128, 4, 31]); b_cw = B("cw", True)
        cv = sb("cv", [128, 12]); b_cv = B("cv", True)
        nh = sb("nh", [128, 1]); b_nh = B("nh")
        epsT = sb("epsT", [128, 1]); b_eps = B("eps")

        T.dma(POOL, b_cst, cstb[:], cst_d[:, :], writes=[b_cst])
        NTILE = NT // 128
        c2b = sb("c2b", [128, 256], BF16); b_c2b = B("c2b", True)
        c2f = sb("c2f", [128, 194]); b_c2f = B("c2f", True)
        ltri = c2b[:, 0:128]; onesF = c2b[:, 128:256]
        iota_e = c2f[:, 0:32]; iota_b = c2f[:, 32:192]; thr = c2f[:, 192:193]; pidx = c2f[:, 193:194]
        T.dma(POOL, b_c2b, c2b[:], cst2_d[:, 0:256], writes=[b_c2b])
        T.dma(SP, b_c2f, c2f[:], cst2_d[:, 256:450], writes=[b_c2f])
        zb = sb("zb", [128, D], BF16); b_zb = B("zb", True); b_xsd = B("xsd")
        T.op(POOL, lambda: nc.gpsimd.memset(zb[:], 0.0), (), [b_zb])
        zero_done = [0]

        def zero_fill(upto):
            for bz in range(zero_done[0], min(upto, NBLK)):
                T.dma(SP, b_zb, xs_d[bz * 128:(bz + 1) * 128, :], zb[:], reads=[b_zb], writes=[b_xsd])
            zero_done[0] = max(zero_done[0], min(upto, NBLK))
        rtb = sb("rtb", [128, 8, 36], BF16); b_rt = B("rt", True)
        T.dma(POOL, b_rt, rtb[:], rt_d.rearrange("(kc p) n -> p kc n", p=128), writes=[b_rt])
        eall = sb("eall", [128, 2 * NTILE]); posall = sb("posall", [128, 2 * NTILE]); wall = sb("wall", [128, 2 * NTILE])
        b_rall = B("rall")
        runb = sb("runb", [128, 32]); b_runb = B("runb")
        T.op(POOL, lambda: nc.gpsimd.memset(runb[:], 0.0), (), [b_runb])
        RS = [(sb("lg%d" % r_, [128, 36]), sb("lem%d" % r_, [128, 32]), sb("m8%d" % r_, [128, 8]), sb("sm%d" % r_, [128, 16]),
               sb("ohb%d" % r_, [128, 2, 32], BF16), sb("ohs%d" % r_, [128, 32], BF16), sb("posm%d" % r_, [128, 32]),
               sb("jk32%d" % r_, [128, 32]), sb("ppos%d" % r_, [128, 64]), sb("idx8%d" % r_, [128, 8], mybir.dt.uint32)) for r_ in range(4)]
        b_rsl = [B("rscr") for _ in range(4)]
        T.dma(SP, b_gmix, gmix[:], gmix_d[:, :], writes=[b_gmix])
        T.dma(SP, b_gffn, gffn[:], gffn_d[:, :], writes=[b_gffn])
        T.dma(SP, b_cw, cw[:].rearrange("p a b -> p (a b)"), cw_d[:, :], writes=[b_cw])
        T.dma(SP, b_cv, cv[:], cv_d[:, :], writes=[b_cv])
        T.op(POOL, lambda: nc.gpsimd.memset(nh[:], -0.5), (), [b_nh])
        T.op(POOL, lambda: nc.gpsimd.memset(epsT[:], 1e-6), (), [b_eps])

        def rstd_from_ss(ss_ap, rstd_ap, b_ss, b_rstd):
            T.op(DVE, lambda: nc.vector.tensor_scalar(out=ss_ap, in0=ss_ap, scalar1=1.0 / D, scalar2=1e-6,
                                                     op0=ALU.mult, op1=ALU.add), [b_ss], [b_ss])
            T.op(POOL, lambda: nc.gpsimd.tensor_tensor(out=rstd_ap, in0=ss_ap, in1=nh[:], op=ALU.pow),
                 [b_ss, b_nh], [b_rstd])

        es_a = ExitStack()
        es.enter_context(es_a)

        def sba(name, shape, dt=F32):
            return es_a.enter_context(nc.sbuf_tensor(name, shape, dt))

        def psa(name, shape, dt=F32):
            return es_a.enter_context(nc.psum_tensor(name, shape, dt))

        winb = sba("winb", [128, 8, 2560], BF16); b_win = B("win", True)
        b_wout = B("wout", True)
        for kc in range(8):
            for hf in range(2):
                T.dma(POOL, b_win, winb[:, kc, hf * 1280:(hf + 1) * 1280],
                      win_d[kc * 128:(kc + 1) * 128, hf * 1280:(hf + 1) * 1280], writes=[b_win])

        b_wbf = B("wbf", True)
        PRECAST = False
        if sparse and moe and PRECAST:
            for wi, srcw in enumerate((eg_d, eu_d, ed_d)):
                for r4 in range(8):
                    T.dma(POOL, b_wbf, wbf_d[wi][r4 * 512:(r4 + 1) * 512, :], srcw[r4 * 512:(r4 + 1) * 512, :], writes=[b_wbf])

        NXS = 3
        x32 = [sba("x32_%d" % i, [128, D]) for i in range(NXS)]; b_x32 = [B("x32", True) for _ in range(NXS)]
        xn = [sba("xn_%d" % i, [128, D], BF16) for i in range(NXS)]; b_xn = [B("xn", True) for _ in range(NXS)]
        ss = [sba("ss_%d" % i, [128, 1]) for i in range(NXS)]; b_ss = [B("ss") for _ in range(NXS)]
        rstd = [sba("rstd_%d" % i, [128, 1]) for i in range(NXS)]; b_rstd = [B("rstd") for _ in range(NXS)]
        b_hnTs = []
        NHS = 6
        hnTr = [sba("hnTr_%d" % i, [128, 8, 128], BF16) for i in range(NHS)]; b_hnTr = [B("hnTr", True) for _ in range(NHS)]
        hT = sba("hT", [128, 8, S], BF16); b_hT = [B("hT") for _ in range(16)]
        woutb = hT[:, 0:4, :].rearrange("p a (b c) -> p (a b) c", c=D)
        catT = sba("catT", [128, 8, S], BF16); b_cat = [[B("cat") for _ in range(4)] for _ in range(8)]
        R1 = sba("R1", [128, 8192])
        R2 = sba("R2", [128, 3072])
        qTe = R2[:, 0:1024].bitcast(BF16); qTo = R2[:, 1024:2048].bitcast(BF16); kT = R2[:, 2048:3072].bitcast(BF16)
        b_qe = [B("qe") for _ in range(4)]; b_qo = [B("qo") for _ in range(4)]; b_k = [B("k") for _ in range(4)]
        vT = sba("vT", [128, S], BF16); b_vT = [B("vT") for _ in range(4)]
        NVS = 2
        Vz = [R1[:, 4096 + i * 2048:4096 + (i + 1) * 2048].bitcast(BF16).rearrange("p (t h c) -> p t h c", h=2, c=128)
              for i in range(NVS)]
        b_Vz = [[[B("Vz") for _ in range(2)] for _ in range(4)] for _ in range(NVS)]
        acc = R1[:, 0:4096].rearrange("p (a s) -> p a s", s=S); b_acc = B("acc")
        NPT = 8
        pt = [sba("pt_%d" % i, [128, 256], BF16) for i in range(NPT)]; b_pt = [B("pt") for _ in range(NPT)]
        u = R2[:, 0:1040].bitcast(BF16); b_u = [B("u") for _ in range(4)]; b_u0 = B("u0")
        Dg = R2[:, 1040:1040 + 1984].bitcast(BF16).rearrange("p (j c) -> p j c", c=128); b_Dg = B("Dg")
        ybf = R1[:, 0:4096].bitcast(BF16).rearrange("p (c s) -> p c s", s=S); b_y = [[B("y") for _ in range(4)] for _ in range(4)]
        sg = [R1[:, 4096 + i * 512:4096 + (i + 1) * 512] for i in range(2)]; b_sg = [B("sg") for _ in range(2)]
        mean = R1[:, 5120:5632]; b_mean = B("mean")
        var = R1[:, 5632:6144]; b_var = B("var")
        tt = [R1[:, 6144 + i * 512:6144 + (i + 1) * 512] for i in range(2)]; b_tt = [B("tt") for _ in range(2)]
        ysq = [R1[:, 7168 + i * 256:7168 + (i + 1) * 256].bitcast(BF16) for i in range(2)]; b_ysq = [B("ysq") for _ in range(2)]
        attn_bufs = [b_acc] + [b for s_ in b_Vz for g4 in s_ for b in g4] + b_qe + b_qo + b_k
        conv_bufs = [b for r_ in b_y for b in r_] + b_sg + b_ysq + [b_mean, b_var] + b_tt + b_u + [b_u0, b_Dg]

        def seed(dst, srcb):
            deps = []
            for b in srcb:
                if b.w is not None:
                    deps.append(b.w)
                deps.extend(b.r.values())
            for b in dst:
                for d_ in deps:
                    o = b.r.get(d_[0])
                    if o is None or o[2] < d_[2]:
                        b.r[d_[0]] = d_
        tp = psa("tp", [128, 8, 128], BF16); b_tp = B("tp", psum=True)
        NPJ = 3
        NSPS = 2
        pj = [psa("pj_%d" % i, [128, 512]) for i in range(NPJ)]; b_pj = [B("pj", psum=True) for _ in range(NPJ)]
        sps = [psa("sps_%d" % i, [128, 512]) for i in range(NSPS)]; b_sps = [B("sps", psum=True) for _ in range(NSPS)]
        aps = [psa("aps_%d" % i, [128, 512]) for i in range(2)]; b_aps = [B("aps", psum=True) for _ in range(2)]
        ctr = {"hs": 0, "pj": 0, "sps": 0, "aps": 0, "pt": 0, "x": 0, "sg": 0, "ysq": 0, "tt": 0, "vz": 0}

        def nxt(k, n):
            v = ctr[k] % n
            ctr[k] += 1
            return v

        def transpose_tile(src, b_src, dst, b_dst_list):
            for kc in range(8):
                T.op(PE, lambda: nc.tensor.transpose(out=tp[:, kc, :], in_=src[:, kc * 128:(kc + 1) * 128], identity=identb),
                     [b_src, b_cst], [b_tp])
            T.op(ACT, lambda: nc.scalar.copy(out=dst, in_=tp[:, :, :]), [b_tp], b_dst_list)

        def proj(fcol, tb, ps_ap, b_ps):
            for kc in range(8):
                mm(ps_ap, winb[:, kc, fcol:fcol + 128], hT[:, kc, tb * 512:(tb + 1) * 512], kc == 0, kc == 7,
                   [b_win] + b_hT[tb * 4:(tb + 1) * 4], [b_ps])

        for s in range(n_seq):
            r0 = s * S
            T.skip = "A1" not in phases
            seed(b_hT, [b_wout])
            for i in range(16):
                xs = nxt("x", NXS)
                T.dma(SP, b_x32[xs], x32[xs][:], x_d[r0 + i * 128:r0 + (i + 1) * 128, :], writes=[b_x32[xs]])
                T.op(ACT, lambda: nc.scalar.activation(out=xn[xs][:], in_=x32[xs][:], func=AF.Square, accum_out=ss[xs][:]),
                     [b_x32[xs]], [b_xn[xs], b_ss[xs]])
                rstd_from_ss(ss[xs][:], rstd[xs][:], b_ss[xs], b_rstd[xs])
                T.op(DVE, lambda: nc.vector.scalar_tensor_tensor(out=xn[xs][:], in0=x32[xs][:], scalar=rstd[xs][:], in1=gmix[:],
                                                                op0=ALU.mult, op1=ALU.mult),
                     [b_x32[xs], b_rstd[xs], b_gmix], [b_xn[xs]])
                transpose_tile(xn[xs], b_xn[xs], hT[:, :, i * 128:(i + 1) * 128], [b_hT[i]])

            T.skip = "A2" not in phases
            seed(attn_bufs, conv_bufs)
            zero_fill((NBLK * (s + 1) + n_seq - 1) // n_seq)
            for i in range(NVS):
                bl = [b for g4 in b_Vz[i] for b in g4]
                T.op(POOL, lambda: nc.gpsimd.memset(Vz[i], 0.0), (), bl)
            T.op(POOL, lambda: nc.gpsimd.memset(qTe[64:128, :], 0.0), (), b_qe)
            T.op(POOL, lambda: nc.gpsimd.memset(qTo[0:64, :], 0.0), (), b_qo)
            for hp in range(4):
                for tb in range(4):
                    p = nxt("pj", NPJ)
                    proj(1024 + hp * 128, tb, pj[p][:], b_pj[p])
                    T.op(ACT, lambda: nc.scalar.mul(out=qTe[0:64, tb * 512:(tb + 1) * 512], in_=pj[p][0:64, :], mul=0.125),
                         [b_pj[p]], [b_qe[tb]])
                    T.op(DVE, lambda: nc.vector.tensor_scalar(out=qTo[64:128, tb * 512:(tb + 1) * 512], in0=pj[p][64:128, :],
                                                             scalar1=0.125, scalar2=None, op0=ALU.mult),
                         [b_pj[p]], [b_qo[tb]])
                for tb in range(4):
                    p = nxt("pj", NPJ)
                    proj(1536 + hp * 128, tb, pj[p][:], b_pj[p])
                    T.op(DVE, lambda: nc.vector.tensor_copy(out=kT[:, tb * 512:(tb + 1) * 512], in_=pj[p][:]),
                         [b_pj[p]], [b_k[tb]])
                for tb in range(4):
                    p = nxt("pj", NPJ)
                    proj(2048 + hp * 128, tb, pj[p][:], b_pj[p])
                    T.op(ACT, lambda: nc.scalar.copy(out=vT[:, tb * 512:(tb + 1) * 512], in_=pj[p][:]), [b_pj[p]], [b_vT[tb]])
                for pi, d in enumerate(PATTERN_D):
                    vs = nxt("vz", NVS)
                    vz = Vz[vs]
                    T.skip = ("A2" not in phases) or ("noV" in phases)
                    for tg8 in range(2):
                        for t8 in range(8):
                            tau = tg8 * 8 + t8
                            T.op(PE, lambda: nc.tensor.transpose(out=tp[:, t8, :], in_=vT[:, cols(d, tau)], identity=identb),
                                 b_vT + [b_cst], [b_tp])
                        T.op(ACT, lambda: nc.scalar.copy(out=vz[:, tg8 * 8:(tg8 + 1) * 8, 0, 0:64], in_=tp[:, :, 0:64]),
                             [b_tp], [b_Vz[vs][2 * tg8][0], b_Vz[vs][2 * tg8 + 1][0]])
                        T.op(DVE, lambda: nc.vector.tensor_copy(out=vz[:, tg8 * 8:(tg8 + 1) * 8, 1, 64:128], in_=tp[:, :, 64:128]),
                             [b_tp], [b_Vz[vs][2 * tg8][1], b_Vz[vs][2 * tg8 + 1][1]])
                    T.skip = ("A2" not in phases) or ("noS" in phases)
                    items = [(tau, h) for tau in range(16) for h in range(2)]
                    st = {}
                    LOOK = 2

                    def stage_s(tau, h):
                        _, n = tile_base(d, tau)
                        has_prev = n > 0
                        cq = cols(d, tau)
                        qs, bq = (qTe, b_qe) if h == 0 else (qTo, b_qo)
                        sp_ = nxt("sps", NSPS)
                        lo = 0 if has_prev else 128
                        if has_prev:
                            mm(sps[sp_][:, 0:128], kT[:, cols(d, tau - 1)], qs[:, cq], True, True, b_k + bq, [b_sps[sp_]])
                        mm(sps[sp_][:, 128:256], kT[:, cq], qs[:, cq], True, True, b_k + bq, [b_sps[sp_]])
                        pp = nxt("pt", NPT)
                        T.op(ACT, lambda: nc.scalar.activation(out=pt[pp][:, lo:256], in_=sps[sp_][:, lo:256], func=AF.Exp),
                             [b_sps[sp_]], [b_pt[pp]])
                        T.op(POOL, lambda: nc.gpsimd.tensor_tensor(out=pt[pp][:, lo:256], in0=pt[pp][:, lo:256], in1=m2[:, lo:256],
                                                                  op=ALU.mult),
                             [b_pt[pp], b_cst], [b_pt[pp]])
                        st[(tau, h)] = (pp, has_prev)

                    def stage_pv(tau, h):
                        pp, has_prev = st.pop((tau, h))
                        if h == 0:
                            st["a"] = nxt("aps", 2)
                        a = st["a"]
                        cq = cols(d, tau)
                        kbs = ([(tau - 1, 0)] if has_prev else []) + [(tau, 1)]
                        first = (h == 0)
                        for (tk, kb) in kbs:
                            last = (h == 1 and kb == 1)
                            mm(aps[a][:, 0:128], vz[:, tk, h, :], pt[pp][:, kb * 128:(kb + 1) * 128], first, False,
                               [b_Vz[vs][tk // 4][h], b_pt[pp]], [b_aps[a]])
                            first = False
                            mm(aps[a][:, 128:256], onesH[h], pt[pp][:, kb * 128:(kb + 1) * 128], False, last,
                               [b_cst, b_pt[pp]], [b_aps[a]])
                        if h == 1:
                            av = aps[a][:, 0:256].rearrange("p (a b) -> p a b", b=128)
                            if pi == 0:
                                T.op(DVE, lambda: nc.vector.tensor_copy(out=acc[:, :, cq], in_=av), [b_aps[a]], [b_acc])
                            else:
                                T.op(DVE, lambda: nc.vector.tensor_tensor(out=acc[:, :, cq], in0=av, in1=acc[:, :, cq], op=ALU.add),
                                     [b_aps[a], b_acc], [b_acc])

                    for k in range(len(items) + LOOK):
                        if k < len(items):
                            stage_s(*items[k])
                        if k >= LOOK:
                            stage_pv(*items[k - LOOK])
                T.skip = "A2" not in phases
                T.op(DVE, lambda: nc.vector.reciprocal(out=acc[:, 1, :], in_=acc[:, 1, :]), [b_acc], [b_acc])
                T.op(DVE, lambda: nc.vector.tensor_tensor(out=catT[:, 4 + hp, :], in0=acc[:, 0, :], in1=acc[:, 1, :], op=ALU.mult),
                     [b_acc], b_cat[4 + hp])

            T.skip = "A3" not in phases
            seed(conv_bufs, attn_bufs)
            T.op(POOL, lambda: nc.gpsimd.memset(u[:, 0:30], 0.0), (), [b_u0])
            for cc in range(4):
                for j in range(31):
                    T.op(DVE, lambda: nc.vector.tensor_scalar(out=Dg[:, j, :], in0=identb, scalar1=cw[:, cc, j:j + 1], scalar2=None,
                                                             op0=ALU.mult),
                         [b_cst, b_cw], [b_Dg])
                for tb in range(4):
                    pa = nxt("pj", NPJ)
                    proj(cc * 128, tb, pj[pa][:], b_pj[pa])
                    pg = nxt("pj", NPJ)
                    proj(512 + cc * 128, tb, pj[pg][:], b_pj[pg])
                    sgi = nxt("sg", 2)
                    T.op(ACT, lambda: nc.scalar.activation(out=sg[sgi], in_=pj[pg][:], func=AF.Sigmoid), [b_pj[pg]], [b_sg[sgi]])
                    T.op(DVE, lambda: nc.vector.tensor_tensor(out=u[:, 30 + tb * 512:30 + (tb + 1) * 512], in0=pj[pa][:], in1=sg[sgi],
                                                             op=ALU.mult),
                         [b_pj[pa], b_sg[sgi]], [b_u[tb]])
                for tb in range(4):
                    pc = nxt("pj", NPJ)
                    rd = [b_Dg, b_u0] + ([b_u[tb - 1]] if tb > 0 else []) + [b_u[tb]]
                    for j in range(31):
                        mm(pj[pc][:], Dg[:, j, :], u[:, tb * 512 + j:tb * 512 + j + 512], j == 0, j == 30, rd, [b_pj[pc]])
                    T.op(ACT, lambda: nc.scalar.activation(out=ybf[:, cc, tb * 512:(tb + 1) * 512], in_=pj[pc][:], func=AF.Identity,
                                                           bias=cv[:, cc:cc + 1]),
                         [b_pj[pc], b_cv], [b_y[cc][tb]])
            T.skip = "A4" not in phases
            for tb in range(4):
                p1 = nxt("pj", NPJ)
                for cc in range(4):
                    mm(pj[p1][:], onesM, ybf[:, cc, tb * 512:(tb + 1) * 512], cc == 0, cc == 3, [b_cst, b_y[cc][tb]], [b_pj[p1]])
                p2 = nxt("pj", NPJ)
                for cc in range(4):
                    yi = nxt("ysq", 2)
                    T.op(ACT, lambda: nc.scalar.activation(out=ysq[yi], in_=ybf[:, cc, tb * 512:(tb + 1) * 512], func=AF.Square),
                         [b_y[cc][tb]], [b_ysq[yi]])
                    mm(pj[p2][:], onesM, ysq[yi], cc == 0, cc == 3, [b_cst, b_ysq[yi]], [b_pj[p2]])
                T.op(DVE, lambda: nc.vector.tensor_copy(out=mean, in_=pj[p1][:]), [b_pj[p1]], [b_mean])
                T.op(DVE, lambda: nc.vector.tensor_tensor(out=var, in0=mean, in1=mean, op=ALU.mult), [b_mean], [b_var])
                T.op(DVE, lambda: nc.vector.tensor_tensor(out=var, in0=pj[p2][:], in1=var, op=ALU.subtract),
                     [b_pj[p2], b_var], [b_var])
                T.op(ACT, lambda: nc.scalar.activation(out=var, in_=var, func=AF.Ln, bias=epsT[:]), [b_var, b_eps], [b_var])
                T.op(ACT, lambda: nc.scalar.activation(out=var, in_=var, func=AF.Exp, scale=-0.5), [b_var], [b_var])
                for cc in range(4):
                    ti = nxt("tt", 2)
                    T.op(DVE, lambda: nc.vector.tensor_tensor(out=tt[ti], in0=ybf[:, cc, tb * 512:(tb + 1) * 512], in1=mean,
                                                             op=ALU.subtract),
                         [b_y[cc][tb], b_mean], [b_tt[ti]])
                    T.op(DVE, lambda: nc.vector.tensor_tensor(out=tt[ti], in0=tt[ti], in1=var, op=ALU.mult),
                         [b_tt[ti], b_var], [b_tt[ti]])
                    T.op(ACT, lambda: nc.scalar.activation(out=catT[:, cc, tb * 512:(tb + 1) * 512], in_=tt[ti], func=AF.Silu,
                                                           scale=cv[:, 4 + cc:5 + cc], bias=cv[:, 8 + cc:9 + cc]),
                         [b_tt[ti], b_cv], [b_cat[cc][tb]])
            T.skip = "A6" not in phases
            seed([b_wout], b_hT)
            T.dma(POOL, b_wout, woutb, wout_d.rearrange("(kc p) n -> p kc n", p=128), writes=[b_wout])
            xslot = {}

            def a6_load(i):
                if i < 16 and i not in xslot:
                    xs_ = nxt("x", NXS)
                    xslot[i] = xs_
                    T.dma(SP, b_x32[xs_], x32[xs_][:], x_d[r0 + i * 128:r0 + (i + 1) * 128, :], writes=[b_x32[xs_]])

            def a6_main(i):
                a6_load(i)
                xs = xslot[i]
                hsl = nxt("hs", NHS)
                for hf in range(2):
                    p = nxt("pj", NPJ)
                    for kc in range(8):
                        mm(pj[p][:], catT[:, kc, i * 128:(i + 1) * 128], woutb[:, kc, hf * 512:(hf + 1) * 512], kc == 0, kc == 7,
                           [b_wout, b_cat[kc][i // 4]], [b_pj[p]])
                    T.op(DVE, lambda: nc.vector.tensor_tensor(out=x32[xs][:, hf * 512:(hf + 1) * 512], in0=pj[p][:],
                                                             in1=x32[xs][:, hf * 512:(hf + 1) * 512], op=ALU.add),
                         [b_pj[p], b_x32[xs]], [b_x32[xs]])
                a6_load(i + 1)
                T.dma(SP, b_x32[xs], x2_d[r0 + i * 128:r0 + (i + 1) * 128, :], x32[xs][:], reads=[b_x32[xs]])
                T.op(ACT, lambda: nc.scalar.activation(out=xn[xs][:], in_=x32[xs][:], func=AF.Square, accum_out=ss[xs][:]),
                     [b_x32[xs]], [b_xn[xs], b_ss[xs]])
                rstd_from_ss(ss[xs][:], rstd[xs][:], b_ss[xs], b_rstd[xs])
                T.op(DVE, lambda: nc.vector.scalar_tensor_tensor(out=xn[xs][:], in0=x32[xs][:], scalar=rstd[xs][:], in1=gffn[:],
                                                                op0=ALU.mult, op1=ALU.mult),
                     [b_x32[xs], b_rstd[xs], b_gffn], [b_xn[xs]])
                transpose_tile(xn[xs], b_xn[xs], hnTr[hsl][:], [b_hnTr[hsl]])
                if not sparse:
                    T.dma(SP, b_hnTr[hsl], hnT_d[:, :, r0 + i * 128:r0 + (i + 1) * 128], hnTr[hsl][:], reads=[b_hnTr[hsl]])
                else:
                    T.dma(SP, b_xn[xs], hn_d[r0 + i * 128:r0 + (i + 1) * 128, :], xn[xs][:], reads=[b_xn[xs]])
                return hsl

            def route(i, hsl, ri):
                jg = s * 16 + i
                V = nc.vector
                lg, lem, m8, sm, ohb, ohs, posm, jk32, ppos, idx8 = RS[ri]
                R_ = [b_rsl[ri]]
                RA = [b_rsl[ri], b_rall]
                p = nxt("pj", NPJ)
                for kc in range(8):
                    mm(pj[p][:, 0:36], hnTr[hsl][:, kc, :], rtb[:, kc, :], kc == 0, kc == 7, [b_hnTr[hsl], b_rt], [b_pj[p]])
                T.op(DVE, lambda: V.tensor_copy(out=lg[:], in_=pj[p][:, 0:36]), [b_pj[p]], R_)
                yield
                ops = [
                    (DVE, lambda: V.reduce_max(out=sm[:, 0:1], in_=lg[:, 0:4], axis=mybir.AxisListType.X), R_, R_),
                    (DVE, lambda: V.tensor_scalar(out=sm[:, 1:2], in0=sm[:, 0:1], scalar1=-1.0, scalar2=None, op0=ALU.mult), R_, R_),
                    (ACT, lambda: nc.scalar.activation(out=sm[:, 12:16], in_=lg[:, 0:4], func=AF.Exp, bias=sm[:, 1:2], accum_out=sm[:, 2:3]),
                     R_, R_),
                    (DVE, lambda: V.reciprocal(out=sm[:, 3:4], in_=sm[:, 2:3]), R_, R_),
                    (DVE, lambda: V.tensor_scalar(out=sm[:, 8:12], in0=lg[:, 0:4], scalar1=sm[:, 0:1], scalar2=None, op0=ALU.is_equal), R_, R_),
                    (DVE, lambda: V.tensor_scalar(out=sm[:, 8:12], in0=sm[:, 8:12], scalar1=BIG, scalar2=-BIG, op0=ALU.mult, op1=ALU.add),
                     R_, R_),
                ]
                for gi in range(4):
                    ops.append((DVE, lambda gi=gi: V.tensor_scalar(out=lem[:, gi * 8:(gi + 1) * 8], in0=lg[:, 4 + gi * 8:12 + gi * 8],
                                                                  scalar1=sm[:, 8 + gi:9 + gi], scalar2=None, op0=ALU.add), R_, R_))
                ops += [
                    (DVE, lambda: V.max(out=m8[:], in_=lem[:]), R_, R_),
                    (DVE, lambda: V.tensor_tensor(out=sm[:, 4:5], in0=m8[:, 1:2], in1=m8[:, 0:1], op=ALU.subtract), R_, R_),
                    (ACT, lambda: nc.scalar.activation(out=sm[:, 5:6], in_=sm[:, 4:5], func=AF.Exp), R_, R_),
                    (DVE, lambda: V.tensor_scalar(out=sm[:, 5:6], in0=sm[:, 5:6], scalar1=1.0, scalar2=None, op0=ALU.add), R_, R_),
                    (DVE, lambda: V.reciprocal(out=sm[:, 5:6], in_=sm[:, 5:6]), R_, R_),
                    (DVE, lambda: V.tensor_tensor(out=wall[:, 2 * jg:2 * jg + 1], in0=sm[:, 5:6], in1=sm[:, 3:4], op=ALU.mult), R_, RA),
                    (DVE, lambda: V.tensor_tensor(out=wall[:, 2 * jg + 1:2 * jg + 2], in0=sm[:, 3:4], in1=wall[:, 2 * jg:2 * jg + 1],
                                                  op=ALU.subtract), RA, RA),
                ]
                ops.append((DVE, lambda: V.max_index(out=idx8[:], in_max=m8[:], in_values=lem[:]), R_, R_))
                ops.append((DVE, lambda: V.tensor_copy(out=eall[:, 2 * jg:2 * jg + 2], in_=idx8[:, 0:2]), R_, RA))
                for a_ in range(2):
                    ops.append((DVE, lambda a_=a_: V.tensor_scalar(out=ohb[:, a_, :], in0=iota_e, scalar1=eall[:, 2 * jg + a_:2 * jg + a_ + 1],
                                                                  scalar2=None, op0=ALU.is_equal), [b_rsl[ri], b_c2f, b_rall], R_))
                ops.append((DVE, lambda: V.tensor_tensor(out=ohs[:], in0=ohb[:, 0, :], in1=ohb[:, 1, :], op=ALU.add), R_, R_))
                for (e_, f_, rd_, wr_) in ops:
                    T.op(e_, f_, rd_, wr_)
                    yield
                p2 = nxt("pj", NPJ)
                mm(pj[p2][:, 0:32], ltri, ohs[:], True, True, [b_c2b, b_rsl[ri]], [b_pj[p2]])
                mm(pj[p2][:, 32:64], onesF, ohs[:], True, True, [b_c2b, b_rsl[ri]], [b_pj[p2]])
                T.op(DVE, lambda: V.tensor_copy(out=ppos[:], in_=pj[p2][:, 0:64]), [b_pj[p2]], R_)
                tails[i] = (p2, ri, jg)

            def route_tail_a(i):
                p2, ri, jg = tails[i]
                posm = RS[ri][6]
                ppos = RS[ri][8]
                T.op(DVE, lambda: nc.vector.tensor_tensor(out=posm[:], in0=ppos[:, 0:32], in1=runb[:], op=ALU.add),
                     [b_rsl[ri], b_runb], [b_rsl[ri]])
                T.op(DVE, lambda: nc.vector.tensor_tensor(out=runb[:], in0=ppos[:, 32:64], in1=runb[:], op=ALU.add),
                     [b_rsl[ri], b_runb], [b_runb])

            def route_tail_b(i, a_):
                p2, ri, jg = tails[i]
                ohb, posm, jk32 = RS[ri][4], RS[ri][6], RS[ri][7]
                T.op(DVE, lambda: nc.vector.scalar_tensor_tensor(out=jk32[:], in0=ohb[:, a_, :], scalar=1.0, in1=posm[:], op0=ALU.mult,
                                                                op1=ALU.mult, accum_out=posall[:, 2 * jg + a_:2 * jg + a_ + 1]),
                     [b_rsl[ri]], [b_rsl[ri], b_rall])

            tails = {}
            GRP = int(_os.environ.get("GRP", "4"))
            for i0 in range(0, 16, GRP):
                hsl_ = [a6_main(i) for i in range(i0, i0 + GRP)]
                if not sparse:
                    continue
                gens = [route(i0 + k_, hsl_[k_], k_) for k_ in range(GRP)]
                alive = list(gens)
                while alive:
                    nxt_alive = []
                    for g_ in alive:
                        try:
                            next(g_)
                            nxt_alive.append(g_)
                        except StopIteration:
                            pass
                    alive = nxt_alive
                for k_ in range(GRP):
                    route_tail_a(i0 + k_)
                for a_ in range(2):
                    for k_ in range(GRP):
                        route_tail_b(i0 + k_, a_)

        T.skip = False
        zero_fill(NBLK)
        last_store = [list(b.r.values()) for b in b_x32 + b_hnTr + b_xn]
        for e in engines:
            for l in last_store:
                T.wait(e, l)
        barrier()
        es_a.close()

        if moe and sparse:
            stage_b_sparse(nc, T, es, engines, B, mm, barrier, NT, NBLK, x2_d, hn_d, xs_d, ys_d, out_d, gfin_d, eg_d, eu_d, ed_d,
                           nh, b_nh, rstd_from_ss, cstb, b_cst, c2b, b_c2b, iota_e, iota_b, thr, pidx, b_c2f,
                           eall, posall, wall, b_rall, runb, b_runb, b_xsd)
        elif moe:
            stage_b(nc, T, es, engines, B, mm, barrier, n_seq, G, x2_d, hnT_d, out_d, gfin_d, rt_d, eg_d, eu_d, ed_d,
                    nh, b_nh, rstd_from_ss)
        else:
            T.wait(SP, [])
    return nc


def stage_b(nc, T, es, engines, B, mm, barrier, n_seq, G, x2_d, hnT_d, out_d, gfin_d, rt_d, eg_d, eu_d, ed_d,
            nh, b_nh, rstd_from_ss):
    PE, ACT, DVE, POOL, SP = engines
    NT = n_seq * S
    NTG = G // 128

    def sb(name, shape, dt=F32):
        return es.enter_context(nc.sbuf_tensor(name, shape, dt))

    def ps(name, shape, dt=F32):
        return es.enter_context(nc.psum_tensor(name, shape, dt))

    gfin = sb("gfin", [128, D]); b_gfin = B("gfin", True)
    T.dma(SP, b_gfin, gfin[:], gfin_d[:, :], writes=[b_gfin])
    rtb = sb("rtb", [128, 8, 36], BF16); b_rt = B("rt", True)
    T.dma(POOL, b_rt, rtb[:], rt_d.rearrange("(kc p) n -> p kc n", p=128), writes=[b_rt])
    hnT = sb("hnT", [128, 8, G], BF16); b_hnT = B("hnT", True)
    accb = sb("accb", [128, NTG, D]); b_accb = [B("accb", True) for _ in range(NTG)]
    NW = 2
    wg = [sb("wg_%d" % i, [128, 8, 256], BF16) for i in range(NW)]; b_wg = [B("wg", True) for _ in range(NW)]
    wu = [sb("wu_%d" % i, [128, 8, 256], BF16) for i in range(NW)]; b_wu = [B("wu", True) for _ in range(NW)]
    wd = [sb("wd_%d" % i, [128, 2, D], BF16) for i in range(NW)]; b_wd = [B("wd", True) for _ in range(NW)]
    hE = [sb("hE_%d" % i, [128, 2, G], BF16) for i in range(2)]
    b_hE = [[[B("hE") for _ in range(G // 512)] for _ in range(2)] for _ in range(2)]
    sgl = [sb("sgl_%d" % i, [128, 512], BF16) for i in range(2)]; b_sgl = [B("sgl") for _ in range(2)]
    call = sb("call", [128, NTG, 32]); b_call = [B("call") for _ in range(NTG)]
    lg = sb("lg", [128, 36]); b_lg = B("lg")
    lem = sb("lem", [128, 32]); b_lem = B("lem")
    oh = sb("oh", [128, 32]); b_oh = B("oh")
    m8 = sb("m8", [128, 8]); b_m8 = B("m8")
    sm = sb("sm", [128, 16]); b_sm = B("sm")
    junk = sb("junkb", [128, D], BF16); b_junk = B("junk")
    ssb = sb("ssb", [128, 1]); b_ssb = B("ssb")
    rsb = sb("rsb", [128, 1]); b_rsb = B("rsb")
    ob = [sb("ob_%d" % i, [128, D]) for i in range(2)]; b_ob = [B("ob", True) for _ in range(2)]
    pgu = [ps("pgu_%d" % i, [128, 512]) for i in range(4)]; b_pgu = [B("pgu", psum=True) for _ in range(4)]
    pdn = [ps("pdn_%d" % i, [128, 512]) for i in range(4)]; b_pdn = [B("pdn", psum=True) for _ in range(4)]
    ctr = {"gu": 0, "dn": 0, "sgl": 0, "ob": 0}

    def nxt(k, n):
        v = ctr[k] % n
        ctr[k] += 1
        return v

    def load_w(e, slot):
        T.dma(POOL, b_wg[slot], wg[slot][:], eg_d[e].rearrange("(kc p) n -> p kc n", p=128), writes=[b_wg[slot]])
        T.dma(POOL, b_wu[slot], wu[slot][:], eu_d[e].rearrange("(kc p) n -> p kc n", p=128), writes=[b_wu[slot]])
        T.dma(POOL, b_wd[slot], wd[slot][:], ed_d[e].rearrange("(kc p) n -> p kc n", p=128), writes=[b_wd[slot]])

    for g in range(NT // G):
        t0 = g * G
        load_w(0, 0)
        T.dma(SP, b_hnT, hnT[:], hnT_d[:, :, t0:t0 + G], writes=[b_hnT])
        for j in range(NTG):
            T.dma(SP, b_accb[j], accb[:, j, :], x2_d[t0 + j * 128:t0 + (j + 1) * 128, :], writes=[b_accb[j]])
        for j in range(NTG):
            p = nxt("gu", 4)
            for kc in range(8):
                mm(pgu[p][:, 0:36], hnT[:, kc, j * 128:(j + 1) * 128], rtb[:, kc, :], kc == 0, kc == 7, [b_hnT, b_rt], [b_pgu[p]])
            V = nc.vector
            T.op(DVE, lambda: V.tensor_copy(out=lg[:], in_=pgu[p][:, 0:36]), [b_pgu[p]], [b_lg])
            T.op(DVE, lambda: V.reduce_max(out=sm[:, 0:1], in_=lg[:, 0:4], axis=mybir.AxisListType.X), [b_lg], [b_sm])
            T.op(DVE, lambda: V.tensor_scalar(out=sm[:, 1:2], in0=sm[:, 0:1], scalar1=-1.0, scalar2=None, op0=ALU.mult), [b_sm], [b_sm])
            T.op(ACT, lambda: nc.scalar.activation(out=sm[:, 12:16], in_=lg[:, 0:4], func=AF.Exp, bias=sm[:, 1:2], accum_out=sm[:, 2:3]),
                 [b_lg, b_sm], [b_sm])
            T.op(DVE, lambda: V.reciprocal(out=sm[:, 3:4], in_=sm[:, 2:3]), [b_sm], [b_sm])
            T.op(DVE, lambda: V.tensor_scalar(out=sm[:, 8:12], in0=lg[:, 0:4], scalar1=sm[:, 0:1], scalar2=None, op0=ALU.is_equal),
                 [b_lg, b_sm], [b_sm])
            T.op(DVE, lambda: V.tensor_scalar(out=sm[:, 8:12], in0=sm[:, 8:12], scalar1=BIG, scalar2=-BIG, op0=ALU.mult, op1=ALU.add),
                 [b_sm], [b_sm])
            for gi in range(4):
                T.op(DVE, lambda: V.tensor_scalar(out=lem[:, gi * 8:(gi + 1) * 8], in0=lg[:, 4 + gi * 8:12 + gi * 8],
                                                 scalar1=sm[:, 8 + gi:9 + gi], scalar2=None, op0=ALU.add),
                     [b_lg, b_sm], [b_lem])
            T.op(DVE, lambda: V.max(out=m8[:], in_=lem[:]), [b_lem], [b_m8])
            T.op(DVE, lambda: V.tensor_tensor(out=sm[:, 4:5], in0=m8[:, 1:2], in1=m8[:, 0:1], op=ALU.subtract), [b_m8, b_sm], [b_sm])
            T.op(ACT, lambda: nc.scalar.activation(out=sm[:, 5:6], in_=sm[:, 4:5], func=AF.Exp), [b_sm], [b_sm])
            T.op(DVE, lambda: V.tensor_scalar(out=sm[:, 5:6], in0=sm[:, 5:6], scalar1=1.0, scalar2=None, op0=ALU.add), [b_sm], [b_sm])
            T.op(DVE, lambda: V.reciprocal(out=sm[:, 5:6], in_=sm[:, 5:6]), [b_sm], [b_sm])
            T.op(DVE, lambda: V.tensor_tensor(out=sm[:, 6:7], in0=sm[:, 5:6], in1=sm[:, 3:4], op=ALU.mult), [b_sm], [b_sm])
            T.op(DVE, lambda: V.tensor_tensor(out=sm[:, 7:8], in0=sm[:, 3:4], in1=sm[:, 6:7], op=ALU.subtract), [b_sm], [b_sm])
            T.op(DVE, lambda: V.tensor_scalar(out=oh[:], in0=lem[:], scalar1=m8[:, 0:1], scalar2=sm[:, 6:7], op0=ALU.is_equal, op1=ALU.mult),
                 [b_lem, b_m8, b_sm], [b_oh])
            T.op(DVE, lambda: V.tensor_scalar(out=call[:, j, :], in0=lem[:], scalar1=m8[:, 1:2], scalar2=sm[:, 7:8], op0=ALU.is_equal,
                                             op1=ALU.mult),
                 [b_lem, b_m8, b_sm], [b_call[j]])
            T.op(DVE, lambda: V.tensor_tensor(out=call[:, j, :], in0=call[:, j, :], in1=oh[:], op=ALU.add), [b_call[j], b_oh], [b_call[j]])
        for e in range(32):
            sl = e % NW
            if e + 1 < 32:
                load_w(e + 1, (e + 1) % NW)
            hs = e % 2
            for dc in range(2):
                for tb in range(G // 512):
                    pg = nxt("gu", 4)
                    for kc in range(8):
                        mm(pgu[pg][:], wg[sl][:, kc, dc * 128:(dc + 1) * 128], hnT[:, kc, tb * 512:(tb + 1) * 512], kc == 0, kc == 7,
                           [b_wg[sl], b_hnT], [b_pgu[pg]])
                    pu = nxt("gu", 4)
                    for kc in range(8):
                        mm(pgu[pu][:], wu[sl][:, kc, dc * 128:(dc + 1) * 128], hnT[:, kc, tb * 512:(tb + 1) * 512], kc == 0, kc == 7,
                           [b_wu[sl], b_hnT], [b_pgu[pu]])
                    si = nxt("sgl", 2)
                    T.op(ACT, lambda: nc.scalar.activation(out=sgl[si][:], in_=pgu[pg][:], func=AF.Silu), [b_pgu[pg]], [b_sgl[si]])
                    T.op(DVE, lambda: nc.vector.tensor_tensor(out=hE[hs][:, dc, tb * 512:(tb + 1) * 512], in0=pgu[pu][:], in1=sgl[si][:],
                                                             op=ALU.mult),
                         [b_pgu[pu], b_sgl[si]], [b_hE[hs][dc][tb]])
            for j in range(NTG):
                for hf in range(2):
                    pd = nxt("dn", 4)
                    for dc in range(2):
                        mm(pdn[pd][:], hE[hs][:, dc, j * 128:(j + 1) * 128], wd[sl][:, dc, hf * 512:(hf + 1) * 512], dc == 0, dc == 1,
                           [b_hE[hs][dc][j // 4], b_wd[sl]], [b_pdn[pd]])
                    T.op(DVE, lambda: nc.vector.scalar_tensor_tensor(out=accb[:, j, hf * 512:(hf + 1) * 512], in0=pdn[pd][:],
                                                                    scalar=call[:, j, e:e + 1],
                                                                    in1=accb[:, j, hf * 512:(hf + 1) * 512], op0=ALU.mult, op1=ALU.add),
                         [b_pdn[pd], b_call[j], b_accb[j]], [b_accb[j]])
        for j in range(NTG):
            T.op(ACT, lambda: nc.scalar.activation(out=junk[:], in_=accb[:, j, :], func=AF.Square, accum_out=ssb[:]),
                 [b_accb[j]], [b_junk, b_ssb])
            rstd_from_ss(ssb[:], rsb[:], b_ssb, b_rsb)
            oi = nxt("ob", 2)
            T.op(DVE, lambda: nc.vector.scalar_tensor_tensor(out=ob[oi][:], in0=accb[:, j, :], scalar=rsb[:], in1=gfin[:],
                                                            op0=ALU.mult, op1=ALU.mult),
                 [b_accb[j], b_rsb, b_gfin], [b_ob[oi]])
            T.dma(SP, b_ob[oi], out_d[t0 + j * 128:t0 + (j + 1) * 128, :], ob[oi][:], reads=[b_ob[oi]])
    fin = [list(b.r.values()) for b in b_ob]
    for l in fin:
        T.wait(SP, l)
    barrier()


def stage_b_sparse(nc, T, es, engines, B, mm, barrier, NT, NBLK, x2_d, hn_d, xs_d, ys_d, out_d, gfin_d, eg_d, eu_d, ed_d,
                   nh, b_nh, rstd_from_ss, cstb, b_cst, c2b, b_c2b, iota_e, iota_b, thr, pidx, b_c2f,
                   eall, posall, wall, b_rall, runb, b_runb, b_xsd):
    PE, ACT, DVE, POOL, SP = engines
    I32 = mybir.dt.int32
    NTILE = NT // 128
    V = nc.vector
    identb = cstb[:, 0:128]
    onesF = c2b[:, 128:256]

    def sb(name, shape, dt=F32):
        return es.enter_context(nc.sbuf_tensor(name, shape, dt))

    def ps(name, shape, dt=F32):
        return es.enter_context(nc.psum_tensor(name, shape, dt))

    IOff = bass.IndirectOffsetOnAxis
    gfin = sb("gfin", [128, D]); b_gfin = B("gfin", True)
    T.dma(SP, b_gfin, gfin[:], gfin_d[:, :], writes=[b_gfin])
    cmpb = sb("cmpb", [128, 32], BF16); nblk = sb("nblk", [128, 32]); incl = sb("incl", [128, 32]); pstart = sb("pstart", [128, 32])
    one32 = sb("one32", [128, 32]); ebf = sb("ebf", [128, 160]); idxw = sb("idxw", [128, 160], I32)
    destf = sb("destf", [128, 2 * NTILE]); desti = sb("desti", [128, 2 * NTILE], I32)
    oht = sb("oht", [128, 32]); jk = sb("jkb", [128, 32])
    b_t = B("btab")
    ptab = ps("ptab", [128, 512]); b_ptab = B("ptab", psum=True)
    TB = [b_t]
    T.op(DVE, lambda: V.tensor_scalar(out=cmpb[:], in0=runb[:], scalar1=thr, scalar2=None, op0=ALU.is_gt), [b_runb, b_c2f], TB)
    mm(ptab[:, 0:32], onesF, cmpb[:], True, True, [b_c2b, b_t], [b_ptab])
    T.op(DVE, lambda: V.tensor_copy(out=nblk[:], in_=ptab[:, 0:32]), [b_ptab], TB)
    T.op(POOL, lambda: nc.gpsimd.memset(one32[:], 1.0), (), TB)
    T.op(DVE, lambda: V.tensor_tensor_scan(out=incl[:], data0=one32[:], data1=nblk[:], initial=0.0, op0=ALU.mult, op1=ALU.add), TB, TB)
    T.op(DVE, lambda: V.tensor_tensor(out=pstart[:], in0=incl[:], in1=nblk[:], op=ALU.subtract), TB, TB)
    T.op(DVE, lambda: V.tensor_scalar(out=pstart[:], in0=pstart[:], scalar1=128.0, scalar2=None, op0=ALU.mult), TB, TB)
    T.op(POOL, lambda: nc.gpsimd.memset(ebf[:], 0.0), (), TB)
    for e in range(32):
        T.op(DVE, lambda: V.scalar_tensor_tensor(out=ebf[:], in0=iota_b, scalar=incl[:, e:e + 1], in1=ebf[:], op0=ALU.is_ge, op1=ALU.add),
             [b_c2f, b_t], TB)
    T.op(DVE, lambda: V.tensor_scalar(out=ebf[:], in0=ebf[:], scalar1=31.0, scalar2=None, op0=ALU.min), TB, TB)
    eqf = sb("eqf", [128, 160])
    T.op(POOL, lambda: nc.gpsimd.memset(eqf[:], 0.0), (), TB)
    T.op(DVE, lambda: V.tensor_tensor(out=eqf[:, 2:160], in0=ebf[:, 2:160], in1=ebf[:, 0:158], op=ALU.is_equal), TB, TB)
    T.op(DVE, lambda: V.tensor_scalar(out=ebf[:], in0=ebf[:], scalar1=128.0, scalar2=pidx, op0=ALU.mult, op1=ALU.add), [b_t, b_c2f], TB)
    T.op(DVE, lambda: V.scalar_tensor_tensor(out=ebf[:], in0=eqf[:], scalar=8192.0, in1=ebf[:], op0=ALU.mult, op1=ALU.add), TB, TB)
    T.op(DVE, lambda: V.tensor_copy(out=idxw[:], in_=ebf[:]), TB, TB)
    NH = 3
    hnb = [sb("hnb_%d" % i, [128, D], BF16) for i in range(NH)]; b_hnb = [B("hnb", True) for _ in range(NH)]
    b_dst = [B("dst") for _ in range(NTILE)]
    for j in range(NTILE):
        hs = j % NH
        T.dma(SP, b_hnb[hs], hnb[hs][:], hn_d[j * 128:(j + 1) * 128, :], writes=[b_hnb[hs]])
        for k in (2 * j, 2 * j + 1):
            T.op(DVE, lambda: V.tensor_scalar(out=oht[:], in0=iota_e, scalar1=eall[:, k:k + 1], scalar2=None, op0=ALU.is_equal),
                 [b_c2f, b_rall], TB)
            T.op(DVE, lambda: V.scalar_tensor_tensor(out=jk[:], in0=oht[:], scalar=1.0, in1=pstart[:], op0=ALU.mult, op1=ALU.mult,
                                                    accum_out=destf[:, k:k + 1]), TB, TB)
        T.op(DVE, lambda: V.tensor_tensor(out=destf[:, 2 * j:2 * j + 2], in0=destf[:, 2 * j:2 * j + 2], in1=posall[:, 2 * j:2 * j + 2],
                                         op=ALU.add), [b_t, b_rall], TB)
        T.op(DVE, lambda: V.tensor_copy(out=desti[:, 2 * j:2 * j + 2], in_=destf[:, 2 * j:2 * j + 2]), TB, [b_dst[j]])
        for a_ in range(2):
            idma(T, POOL, b_hnb[hs], xs_d[:, :], IOff(ap=desti[:, 2 * j + a_:2 * j + a_ + 1], axis=0), hnb[hs][:], None,
                 reads=[b_hnb[hs], b_dst[j], b_xsd], writes=[])
    T.wait(SP, [d_ for b in b_hnb for d_ in b.r.values()])
    NW = 2
    wgu = [[sb("wgu_%d_%d" % (i, q), [128, 8, 256], BF16) for q in range(2)] for i in range(NW)]
    b_wgu = [[B("wgu", True) for _ in range(2)] for _ in range(NW)]
    wd = [sb("wd_%d" % i, [128, 2, D], BF16) for i in range(NW)]; b_wd = [B("wd", True) for _ in range(NW)]
    NX = 4
    xb = [sb("xb_%d" % i, [128, D], BF16) for i in range(NX)]; b_xb = [B("xb", True) for _ in range(NX)]
    sgl = [sb("sgl_%d" % i, [128, 256]) for i in range(2)]; b_sgl = [B("sgl") for _ in range(2)]
    hTok = [sb("hTok_%d" % i, [128, 256], BF16) for i in range(2)]; b_hTok = [B("hTok") for _ in range(2)]
    hE = [sb("hE_%d" % i, [128, 2, 128], BF16) for i in range(2)]; b_hE = [B("hE") for _ in range(2)]
    yb = [sb("yb_%d" % i, [128, D], BF16) for i in range(2)]; b_yb = [B("yb", True) for _ in range(2)]
    tpb = ps("tpb", [128, 8, 128], BF16); b_tpb = B("tpb", psum=True)
    tp2 = ptab[:, 0:128].bitcast(BF16).rearrange("p (a b) -> p a b", b=128)
    gu = [ps("gu_%d" % i, [128, 512]) for i in range(2)]; b_gu = [B("gu", psum=True) for _ in range(2)]
    yp = [[ps("yp_%d_%d" % (i, h), [128, 512]) for h in range(2)] for i in range(2)]
    b_yp = [[B("yp", psum=True) for h in range(2)] for i in range(2)]

    bc_reg = nc.gpsimd.to_reg(32 * 128 - 1)
    kw = dict(bounds_check=bc_reg, oob_is_err=False)
    eg3 = eg_d.rearrange("r (k n) -> r k n", n=256)
    eu3 = eu_d.rearrange("r (k n) -> r k n", n=256)

    def load_gu(b):
        if b >= NBLK:
            return
        sl = b % NW
        io = IOff(ap=idxw[:, b:b + 1], axis=0)
        idma(T, POOL, b_wgu[sl][0], wgu[sl][0][:].rearrange("p a b -> p (a b)"), None, eg_d[:, :], io, reads=[b_t],
             writes=[b_wgu[sl][0]], **kw)
        idma(T, POOL, b_wgu[sl][1], wgu[sl][1][:].rearrange("p a b -> p (a b)"), None, eu_d[:, :], io, reads=[b_t],
             writes=[b_wgu[sl][1]], **kw)

    def load_d(b):
        if b >= NBLK:
            return
        sl = b % NW
        io = IOff(ap=idxw[:, b:b + 1], axis=0)
        idma(T, POOL, b_wd[sl], wd[sl][:].rearrange("p a b -> p (a b)"), None, ed_d[:, :], io, reads=[b_t], writes=[b_wd[sl]], **kw)

    def load_x(b):
        if b >= NBLK:
            return
        xi = b % NX
        T.dma(SP, b_xb[xi], xb[xi][:], xs_d[b * 128:(b + 1) * 128, :], writes=[b_xb[xi]])

    NXT = 3
    xT = [sb("xTp_%d" % i, [128, 8, 128], BF16) for i in range(NXT)]; b_xT = [B("xT") for _ in range(NXT)]

    def st_T(k):
        xi = k % NX; ti = k % NXT
        for kc in range(8):
            T.op(PE, lambda: nc.tensor.transpose(out=tpb[:, kc, :], in_=xb[xi][:, kc * 128:(kc + 1) * 128], identity=identb),
                 [b_xb[xi], b_cst], [b_tpb])
        T.op(ACT, lambda: nc.scalar.copy(out=xT[ti][:], in_=tpb[:, :, :]), [b_tpb], [b_xT[ti]])

    def st_GU(k):
        tx = k % NXT; ti = k % 2; sl = k % NW
        for q in range(2):
            for kc in range(8):
                mm(gu[ti][:, q * 256:(q + 1) * 256], xT[tx][:, kc, :], wgu[sl][q][:, kc, :], kc == 0, kc == 7,
                   [b_xT[tx], b_wgu[sl][q]], [b_gu[ti]])
        T.op(ACT, lambda: nc.scalar.activation(out=sgl[ti][:], in_=gu[ti][:, 0:256], func=AF.Silu), [b_gu[ti]], [b_sgl[ti]])
        T.op(DVE, lambda: V.tensor_tensor(out=hTok[ti][:], in0=gu[ti][:, 256:512], in1=sgl[ti][:], op=ALU.mult),
             [b_gu[ti], b_sgl[ti]], [b_hTok[ti]])

    def st_T2(k):
        ti = k % 2
        for dc in range(2):
            T.op(PE, lambda: nc.tensor.transpose(out=tp2[:, dc, :], in_=hTok[ti][:, dc * 128:(dc + 1) * 128], identity=identb),
                 [b_hTok[ti], b_cst], [b_ptab])
        T.op(DVE, lambda: V.tensor_copy(out=hE[ti][:], in_=tp2), [b_ptab], [b_hE[ti]])

    def st_DN(k):
        ti = k % 2; sl = k % NW
        for hf in range(2):
            for dc in range(2):
                mm(yp[ti][hf][:], hE[ti][:, dc, :], wd[sl][:, dc, hf * 512:(hf + 1) * 512], dc == 0, dc == 1,
                   [b_hE[ti], b_wd[sl]], [b_yp[ti][hf]])
        T.op(ACT, lambda: nc.scalar.copy(out=yb[ti][:, 0:512], in_=yp[ti][0][:]), [b_yp[ti][0]], [b_yb[ti]])
        T.op(DVE, lambda: V.tensor_copy(out=yb[ti][:, 512:1024], in_=yp[ti][1][:]), [b_yp[ti][1], b_yb[ti]], [b_yb[ti]])
        T.dma(SP, b_yb[ti], ys_d[k * 128:(k + 1) * 128, :], yb[ti][:], reads=[b_yb[ti]])

    for b0 in range(2):
        load_gu(b0); load_d(b0)
    for b0 in range(3):
        load_x(b0)
    for it in range(NBLK + 3):
        load_x(it + 3)
        if it < NBLK:
            st_T(it)
        if 0 <= it - 1 < NBLK:
            st_GU(it - 1)
            load_gu(it + 1)
        if 0 <= it - 2 < NBLK:
            st_T2(it - 2)
        if 0 <= it - 3 < NBLK:
            st_DN(it - 3)
            load_d(it - 1)
    T.wait(POOL, [d_ for b in b_yb for d_ in b.r.values()])
    NC_ = 6
    xa = [sb("xa_%d" % i, [128, D]) for i in range(NC_)]; b_xa = [B("xa", True) for _ in range(NC_)]
    y1 = [sb("y1_%d" % i, [128, D], BF16) for i in range(NC_)]; b_y1 = [B("y1", True) for _ in range(NC_)]
    y2 = [sb("y2_%d" % i, [128, D], BF16) for i in range(NC_)]; b_y2 = [B("y2", True) for _ in range(NC_)]
    ob = [sb("ob_%d" % i, [128, D]) for i in range(NC_)]; b_ob = [B("ob", True) for _ in range(NC_)]
    junk = sb("junkb", [128, D], BF16); b_junk = B("junk")
    ssb = [sb("ssb_%d" % i, [128, 1]) for i in range(NC_)]; b_ssb = [B("ssb") for _ in range(NC_)]
    rsb = [sb("rsb_%d" % i, [128, 1]) for i in range(NC_)]; b_rsb = [B("rsb") for _ in range(NC_)]
    for j in range(NTILE):
        c = j % NC_
        T.dma(SP, b_xa[c], xa[c][:], x2_d[j * 128:(j + 1) * 128, :], writes=[b_xa[c]])
        idma(T, POOL, b_y1[c], y1[c][:], None, ys_d[:, :], IOff(ap=desti[:, 2 * j:2 * j + 1], axis=0), reads=[b_dst[j]], writes=[b_y1[c]])
        idma(T, POOL, b_y2[c], y2[c][:], None, ys_d[:, :], IOff(ap=desti[:, 2 * j + 1:2 * j + 2], axis=0), reads=[b_dst[j]], writes=[b_y2[c]])
        T.op(DVE, lambda: V.scalar_tensor_tensor(out=xa[c][:], in0=y1[c][:], scalar=wall[:, 2 * j:2 * j + 1], in1=xa[c][:],
                                                op0=ALU.mult, op1=ALU.add), [b_y1[c], b_rall, b_xa[c]], [b_xa[c]])
        T.op(DVE, lambda: V.scalar_tensor_tensor(out=xa[c][:], in0=y2[c][:], scalar=wall[:, 2 * j + 1:2 * j + 2], in1=xa[c][:],
                                                op0=ALU.mult, op1=ALU.add), [b_y2[c], b_rall, b_xa[c]], [b_xa[c]])
        T.op(ACT, lambda: nc.scalar.activation(out=junk[:], in_=xa[c][:], func=AF.Square, accum_out=ssb[c][:]),
             [b_xa[c]], [b_junk, b_ssb[c]])
        rstd_from_ss(ssb[c][:], rsb[c][:], b_ssb[c], b_rsb[c])
        T.op(DVE, lambda: V.scalar_tensor_tensor(out=ob[c][:], in0=xa[c][:], scalar=rsb[c][:], in1=gfin[:], op0=ALU.mult, op1=ALU.mult),
             [b_xa[c], b_rsb[c], b_gfin], [b_ob[c]])
        T.dma(ACT, b_ob[c], out_d[j * 128:(j + 1) * 128, :], ob[c][:], reads=[b_ob[c]])
    for b in b_ob:
        T.wait(SP, list(b.r.values()))
        T.wait(ACT, list(b.r.values()))
    barrier()


def make_consts():
    c = np.zeros((128, 768), np.float32)
    c[:, 0:128] = np.eye(128, dtype=np.float32)
    c[:, 128:192] = 1.0
    c[:, 256 + 64:384] = 1.0
    c[:, 384:512] = 1.0 / 512.0
    kj = np.arange(128)[:, None]
    qi = np.arange(128)[None, :]
    c[:, 512:640] = (kj >= qi)
    c[:, 640:768] = (kj <= qi)
    return c


def make_consts2():
    c = np.zeros((128, 450), np.float32)
    tp_ = np.arange(128)[:, None]
    t_ = np.arange(128)[None, :]
    c[:, 0:128] = (tp_ < t_)
    c[:, 128:256] = 1.0
    c[:, 256:288] = np.arange(32)[None, :]
    c[:, 288:448] = np.arange(160)[None, :]
    c[:, 448] = 128.0 * np.arange(128)
    c[:, 449] = np.arange(128)
    return c


def host_layout(inp, n_seq=SEQ_PER_CORE, sparse=True):
    f = lambda a: np.ascontiguousarray(np.asarray(a, dtype=np.float32))
    bc = lambda v: f(np.broadcast_to(np.asarray(v, np.float32).reshape(1, D), (128, D)))
    cwt = np.asarray(inp["conv_dw_w"], np.float32)[0]
    conv_w = f(cwt.T.reshape(4, 128, 31).transpose(1, 0, 2).reshape(128, 124))
    pc = lambda v: np.asarray(v, np.float32).reshape(4, 128).T
    conv_v = f(np.concatenate([pc(inp["conv_dw_b"]), pc(inp["conv_ln_g"]), pc(inp["conv_ln_b"])], axis=1))
    router = f(np.concatenate([np.asarray(inp["router_group"], np.float32)[0],
                               np.asarray(inp["router_expert"], np.float32)[0].reshape(D, 32)], axis=1))
    shared = {
        "w_in": f(inp["w_in"][0]), "w_out": f(inp["w_out"][0]), "cst": make_consts(),
        "gmix_b": bc(inp["norm_mix_g"]), "gffn_b": bc(inp["norm_ffn_g"]), "gfin_b": bc(inp["norm_final_g"]),
        "conv_w": conv_w, "conv_v": conv_v, "router": router,
        "cst2": make_consts2(),
    }
    if sparse:
        lay = lambda w, kcn: f(np.asarray(w, np.float32)[0].reshape(32, kcn, 128, -1).transpose(0, 2, 1, 3).reshape(32 * 128, 2048))
        shared.update({"e_gate": lay(inp["expert_w_gate"], 8), "e_up": lay(inp["expert_w_up"], 8), "e_down": lay(inp["expert_w_down"], 2)})
    else:
        shared.update({"e_gate": f(inp["expert_w_gate"][0]), "e_up": f(inp["expert_w_up"][0]), "e_down": f(inp["expert_w_down"][0])})
    return shared


def kernel(x, norm_mix_g, w_in, conv_dw_w, conv_dw_b, conv_ln_g, conv_ln_b, w_out, norm_ffn_g, router_group,
           router_expert, expert_w_gate, expert_w_up, expert_w_down, norm_final_g):
    inp = dict(norm_mix_g=norm_mix_g, w_in=w_in, conv_dw_w=conv_dw_w, conv_dw_b=conv_dw_b, conv_ln_g=conv_ln_g,
               conv_ln_b=conv_ln_b, w_out=w_out, norm_ffn_g=norm_ffn_g, router_group=router_group,
               router_expert=router_expert, expert_w_gate=expert_w_gate, expert_w_up=expert_w_up,
               expert_w_down=expert_w_down, norm_final_g=norm_final_g)
    shared = host_layout(inp)
    xf = np.asarray(x, dtype=np.float32)
    nc = build()
    in_maps = []
    for c in range(NCORE):
        m = dict(shared)
        m["x"] = np.ascontiguousarray(xf[c * SEQ_PER_CORE:(c + 1) * SEQ_PER_CORE].reshape(SEQ_PER_CORE * S, D))
        in_maps.append(m)
    res = run_bass_kernel_spmd(nc, in_maps, core_ids=list(range(NCORE)))
    out = np.concatenate([np.asarray(r["out"], dtype=np.float32).reshape(SEQ_PER_CORE, S, D) for r in res.results], axis=0)
    return out
```

```python
import numpy as np
from contextlib import ExitStack
import concourse.bass as bass
import concourse.mybir as mybir
from concourse.bass_utils import run_bass_kernel_spmd

F32 = mybir.dt.float32
BF16 = mybir.dt.bfloat16
AF = mybir.ActivationFunctionType
ALU = mybir.AluOpType

S = 2048
D = 1024
NCORE = 8
SEQ_PER_CORE = 4
import os as _os
PATTERN_D = tuple(int(v) for v in _os.environ.get("KPD", "1,4,16").split(","))
BIG = 30000.0


class Eng:
    def __init__(self, name, h, sem, selfsync=True):
        self.name = name; self.h = h; self.sem = sem; self.count = 0
        self.seen = {}; self.selfsync = selfsync


class Buf:
    def __init__(self, name, mk=None, psum=False):
        self.name = name; self.w = None; self.r = {}
        self.mk = mk; self._d = None; self._p = None
        self.dcount = 0; self.psum = psum; self.pcount = 0

    @property
    def dsem(self):
        if self._d is None:
            self._d = self.mk("d_" + self.name)
        return self._d

    @property
    def psem(self):
        if self._p is None:
            self._p = self.mk("p_" + self.name)
        return self._p


class Trk:
    def wait(self, eng, deps):
        for d in deps:
            if d is None:
                continue
            key, sem, val, src = d
            if src is eng and not eng.selfsync:
                continue
            if eng.seen.get(key, 0) >= val:
                continue
            eng.h.wait_ge(sem, val)
            eng.seen[key] = val

    def deps(self, reads, writes):
        deps = []
        for b in reads:
            deps.append(b.w)
            if b.psum:
                deps.extend(b.r.values())
        for b in writes:
            deps.append(b.w)
            deps.extend(b.r.values())
        return deps

    def mark(self, d, reads, writes):
        for b in reads:
            o = b.r.get(d[0])
            if o is None or o[2] < d[2]:
                b.r[d[0]] = d
        for b in writes:
            b.w = d; b.r = {}

    skip = False

    def op(self, eng, fn, reads=(), writes=()):
        if self.skip:
            return
        self.wait(eng, self.deps(reads, writes))
        inst = fn()
        eng.count += 1
        inst.then_inc(eng.sem, 1)
        self.mark((eng.name, eng.sem, eng.count, eng), reads, writes)

    def dma(self, eng, sb, out, in_, reads=(), writes=(), **kw):
        if self.skip:
            return
        self.wait(eng, self.deps(reads, writes))
        inst = eng.h.dma_start(out=out, in_=in_, **kw)
        if eng.name == "pool":
            sb.pcount += 16
            inst.then_inc(sb.psem, 16)
            self.mark(("p" + sb.name, sb.psem, sb.pcount, None), reads, writes)
        else:
            sb.dcount += 16
            inst.then_inc(sb.dsem, 16)
            self.mark(("d" + sb.name, sb.dsem, sb.dcount, None), reads, writes)


def idma(T, eng, sb, out, out_off, in_, in_off, reads=(), writes=(), **kw):
    if T.skip:
        return
    T.wait(eng, T.deps(reads, writes))
    inst = eng.h.indirect_dma_start(out=out, out_offset=out_off, in_=in_, in_offset=in_off, **kw)
    sb.pcount += 16
    inst.then_inc(sb.psem, 16)
    T.mark(("p" + sb.name, sb.psem, sb.pcount, None), reads, writes)


def tile_base(d, tau):
    if d == 1:
        return 128 * tau, tau % 16
    if d == 4:
        r, n = tau // 4, tau % 4
        return r + 512 * n, n
    return tau, 0


def cols(d, tau):
    b, _ = tile_base(d, tau)
    return slice(b, b + 127 * d + 1, d)


def build(n_seq=SEQ_PER_CORE, moe=True, debug=False, G=2048, phases=("A1", "A2", "A3", "A4", "A6"), sparse=True):
    nc = bass.Bass("TRN2", target_bir_lowering=False)
    NT = n_seq * S
    dt_in = lambda name, shape: nc.dram_tensor(name, shape, F32, kind="ExternalInput").ap()
    x_d = dt_in("x", [NT, D])
    win_d = dt_in("w_in", [D, 2560])
    wout_d = dt_in("w_out", [D, D])
    cst_d = dt_in("cst", [128, 768])
    gmix_d = dt_in("gmix_b", [128, D])
    gffn_d = dt_in("gffn_b", [128, D])
    gfin_d = dt_in("gfin_b", [128, D])
    cw_d = dt_in("conv_w", [128, 4 * 31])
    cv_d = dt_in("conv_v", [128, 12])
    rt_d = dt_in("router", [D, 36])
    if sparse:
        eg_d = dt_in("e_gate", [32 * 128, 2048])
        eu_d = dt_in("e_up", [32 * 128, 2048])
        ed_d = dt_in("e_down", [32 * 128, 2048])
    else:
        eg_d = dt_in("e_gate", [32, D, 256])
        eu_d = dt_in("e_up", [32, D, 256])
        ed_d = dt_in("e_down", [32, 256, D])
    cst2_d = dt_in("cst2", [128, 450])
    NBLK = (2 * NT) // 128 + 32
    hn_d = nc.dram_tensor("hns", [NT, D], BF16, kind="Internal").ap()
    xs_d = nc.dram_tensor("xsort", [NBLK * 128, D], BF16, kind="Internal").ap()
    ys_d = nc.dram_tensor("ysort", [NBLK * 128, D], BF16, kind="Internal").ap()
    out_d = nc.dram_tensor("out", [NT, D], F32, kind="ExternalOutput").ap()
    skind = "ExternalOutput" if debug else "Internal"
    x2_d = nc.dram_tensor("x2s", [NT, D], F32, kind=skind).ap()
    hnT_d = nc.dram_tensor("hnTs", [128, 8, NT], BF16, kind=skind).ap()

    with ExitStack() as es:
        def sb(name, shape, dt=F32):
            return es.enter_context(nc.sbuf_tensor(name, shape, dt))

        def sem(name):
            return es.enter_context(nc.semaphore(name))

        PE = Eng("pe", nc.tensor, sem("s_pe"), selfsync=False)
        ACT = Eng("act", nc.scalar, sem("s_act"))
        DVE = Eng("dve", nc.vector, sem("s_dve"))
        POOL = Eng("pool", nc.gpsimd, sem("s_pool"))
        SP = Eng("sp", nc.sync, sem("s_sp"))
        engines = [PE, ACT, DVE, POOL, SP]
        T = Trk()
        nbuf = [0]

        def B(name, dma=False, psum=False):
            nbuf[0] += 1
            nm = "%s_%d" % (name, nbuf[0])
            return Buf(nm, sem if dma else None, psum)

        def mm(out, lhsT, rhs, start, stop, reads, writes):
            T.op(PE, lambda: nc.tensor.matmul(out, lhsT=lhsT, rhs=rhs, start=start, stop=stop), reads, writes)

        def barrier():
            deps = [(e.name, e.sem, e.count, None) for e in engines if e.count > 0]
            for e in engines:
                T.wait(e, deps)

        cstb = sb("cstb", [128, 768], BF16); b_cst = B("cst", True)
        identb = cstb[:, 0:128]
        onesH = [cstb[:, 128:256], cstb[:, 256:384]]
        onesM = cstb[:, 384:512]
        m2 = cstb[:, 512:768]
        gmix = sb("gmix", [128, D]); b_gmix = B("gmix", True)
        gffn = sb("gffn", [128, D]); b_gffn = B("gffn", True)
        cw = sb("cw", [128, 4, 31]); b_cw = B("cw", True)
        cv = sb("cv", [128, 12]); b_cv = B("cv", True)
        nh = sb("nh", [128, 1]); b_nh = B("nh")
        epsT = sb("epsT", [128, 1]); b_eps = B("eps")

        T.dma(POOL, b_cst, cstb[:], cst_d[:, :], writes=[b_cst])
        NTILE = NT // 128
        c2b = sb("c2b", [128, 256], BF16); b_c2b = B("c2b", True)
        c2f = sb("c2f", [128, 194]); b_c2f = B("c2f", True)
        ltri = c2b[:, 0:128]; onesF = c2b[:, 128:256]
        iota_e = c2f[:, 0:32]; iota_b = c2f[:, 32:192]; thr = c2f[:, 192:193]; pidx = c2f[:, 193:194]
        T.dma(POOL, b_c2b, c2b[:], cst2_d[:, 0:256], writes=[b_c2b])
        T.dma(SP, b_c2f, c2f[:], cst2_d[:, 256:450], writes=[b_c2f])
        zb = sb("zb", [128, D], BF16); b_zb = B("zb", True); b_xsd = B("xsd")
        T.op(POOL, lambda: nc.gpsimd.memset(zb[:], 0.0), (), [b_zb])
        zero_done = [0]

        def zero_fill(upto):
            for bz in range(zero_done[0], min(upto, NBLK)):
                T.dma(SP, b_zb, xs_d[bz * 128:(bz + 1) * 128, :], zb[:], reads=[b_zb], writes=[b_xsd])
            zero_done[0] = max(zero_done[0], min(upto, NBLK))
        rtb = sb("rtb", [128, 8, 36], BF16); b_rt = B("rt", True)
        T.dma(POOL, b_rt, rtb[:], rt_d.rearrange("(kc p) n -> p kc n", p=128), writes=[b_rt])
        eall = sb("eall", [128, 2 * NTILE]); posall = sb("posall", [128, 2 * NTILE]); wall = sb("wall", [128, 2 * NTILE])
        b_rall = B("rall")
        runb = sb("runb", [128, 32]); b_runb = B("runb")
        T.op(POOL, lambda: nc.gpsimd.memset(runb[:], 0.0), (), [b_runb])
        RS = [(sb("lg%d" % r_, [128, 36]), sb("lem%d" % r_, [128, 32]), sb("m8%d" % r_, [128, 8]), sb("sm%d" % r_, [128, 16]),
               sb("ohb%d" % r_, [128, 2, 32], BF16), sb("ohs%d" % r_, [128, 32], BF16), sb("posm%d" % r_, [128, 32]),
               sb("jk32%d" % r_, [128, 32]), sb("ppos%d" % r_, [128, 64]), sb("idx8%d" % r_, [128, 8], mybir.dt.uint32)) for r_ in range(4)]
        b_rsl = [B("rscr") for _ in range(4)]
        T.dma(SP, b_gmix, gmix[:], gmix_d[:, :], writes=[b_gmix])
        T.dma(SP, b_gffn, gffn[:], gffn_d[:, :], writes=[b_gffn])
        T.dma(SP, b_cw, cw[:].rearrange("p a b -> p (a b)"), cw_d[:, :], writes=[b_cw])
        T.dma(SP, b_cv, cv[:], cv_d[:, :], writes=[b_cv])
        T.op(POOL, lambda: nc.gpsimd.memset(nh[:], -0.5), (), [b_nh])
        T.op(POOL, lambda: nc.gpsimd.memset(epsT[:], 1e-6), (), [b_eps])

        def rstd_from_ss(ss_ap, rstd_ap, b_ss, b_rstd):
            T.op(DVE, lambda: nc.vector.tensor_scalar(out=ss_ap, in0=ss_ap, scalar1=1.0 / D, scalar2=1e-6,
                                                     op0=ALU.mult, op1=ALU.add), [b_ss], [b_ss])
            T.op(POOL, lambda: nc.gpsimd.tensor_tensor(out=rstd_ap, in0=ss_ap, in1=nh[:], op=ALU.pow),
                 [b_ss, b_nh], [b_rstd])

        es_a = ExitStack()
        es.enter_context(es_a)

        def sba(name, shape, dt=F32):
            return es_a.enter_context(nc.sbuf_tensor(name, shape, dt))

        def psa(name, shape, dt=F32):
            return es_a.enter_context(nc.psum_tensor(name, shape, dt))

        winb = sba("winb", [128, 8, 2560], BF16); b_win = B("win", True)
        b_wout = B("wout", True)
        for kc in range(8):
            for hf in range(2):
                T.dma(POOL, b_win, winb[:, kc, hf * 1280:(hf + 1) * 1280],
                      win_d[kc * 128:(kc + 1) * 128, hf * 1280:(hf + 1) * 1280], writes=[b_win])

        b_wbf = B("wbf", True)
        PRECAST = False
        if sparse and moe and PRECAST:
            for wi, srcw in enumerate((eg_d, eu_d, ed_d)):
                for r4 in range(8):
                    T.dma(POOL, b_wbf, wbf_d[wi][r4 * 512:(r4 + 1) * 512, :], srcw[r4 * 512:(r4 + 1) * 512, :], writes=[b_wbf])

        NXS = 3
        x32 = [sba("x32_%d" % i, [128, D]) for i in range(NXS)]; b_x32 = [B("x32", True) for _ in range(NXS)]
        xn = [sba("xn_%d" % i, [128, D], BF16) for i in range(NXS)]; b_xn = [B("xn", True) for _ in range(NXS)]
        ss = [sba("ss_%d" % i, [128, 1]) for i in range(NXS)]; b_ss = [B("ss") for _ in range(NXS)]
        rstd = [sba("rstd_%d" % i, [128, 1]) for i in range(NXS)]; b_rstd = [B("rstd") for _ in range(NXS)]
        b_hnTs = []
        NHS = 6
        hnTr = [sba("hnTr_%d" % i, [128, 8, 128], BF16) for i in range(NHS)]; b_hnTr = [B("hnTr", True) for _ in range(NHS)]
        hT = sba("hT", [128, 8, S], BF16); b_hT = [B("hT") for _ in range(16)]
        woutb = hT[:, 0:4, :].rearrange("p a (b c) -> p (a b) c", c=D)
        catT = sba("catT", [128, 8, S], BF16); b_cat = [[B("cat") for _ in range(4)] for _ in range(8)]
        R1 = sba("R1", [128, 8192])
        R2 = sba("R2", [128, 3072])
        qTe = R2[:, 0:1024].bitcast(BF16); qTo = R2[:, 1024:2048].bitcast(BF16); kT = R2[:, 2048:3072].bitcast(BF16)
        b_qe = [B("qe") for _ in range(4)]; b_qo = [B("qo") for _ in range(4)]; b_k = [B("k") for _ in range(4)]
        vT = sba("vT", [128, S], BF16); b_vT = [B("vT") for _ in range(4)]
        NVS = 2
        Vz = [R1[:, 4096 + i * 2048:4096 + (i + 1) * 2048].bitcast(BF16).rearrange("p (t h c) -> p t h c", h=2, c=128)
              for i in range(NVS)]
        b_Vz = [[[B("Vz") for _ in range(2)] for _ in range(4)] for _ in range(NVS)]
        acc = R1[:, 0:4096].rearrange("p (a s) -> p a s", s=S); b_acc = B("acc")
        NPT = 8
        pt = [sba("pt_%d" % i, [128, 256], BF16) for i in range(NPT)]; b_pt = [B("pt") for _ in range(NPT)]
        u = R2[:, 0:1040].bitcast(BF16); b_u = [B("u") for _ in range(4)]; b_u0 = B("u0")
        Dg = R2[:, 1040:1040 + 1984].bitcast(BF16).rearrange("p (j c) -> p j c", c=128); b_Dg = B("Dg")
        ybf = R1[:, 0:4096].bitcast(BF16).rearrange("p (c s) -> p c s", s=S); b_y = [[B("y") for _ in range(4)] for _ in range(4)]
        sg = [R1[:, 4096 + i * 512:4096 + (i + 1) * 512] for i in range(2)]; b_sg = [B("sg") for _ in range(2)]
        mean = R1[:, 5120:5632]; b_mean = B("mean")
        var = R1[:, 5632:6144]; b_var = B("var")
        tt = [R1[:, 6144 + i * 512:6144 + (i + 1) * 512] for i in range(2)]; b_tt = [B("tt") for _ in range(2)]
        ysq = [R1[:, 7168 + i * 256:7168 + (i + 1) * 256].bitcast(BF16) for i in range(2)]; b_ysq = [B("ysq") for _ in range(2)]
        attn_bufs = [b_acc] + [b for s_ in b_Vz for g4 in s_ for b in g4] + b_qe + b_qo + b_k
        conv_bufs = [b for r_ in b_y for b in r_] + b_sg + b_ysq + [b_mean, b_var] + b_tt + b_u + [b_u0, b_Dg]

        def seed(dst, srcb):
            deps = []
            for b in srcb:
                if b.w is not None:
                    deps.append(b.w)
                deps.extend(b.r.values())
            for b in dst:
                for d_ in deps:
                    o = b.r.get(d_[0])
                    if o is None or o[2] < d_[2]:
                        b.r[d_[0]] = d_
        tp = psa("tp", [128, 8, 128], BF16); b_tp = B("tp", psum=True)
        NPJ = 3
        NSPS = 2
        pj = [psa("pj_%d" % i, [128, 512]) for i in range(NPJ)]; b_pj = [B("pj", psum=True) for _ in range(NPJ)]
        sps = [psa("sps_%d" % i, [128, 512]) for i in range(NSPS)]; b_sps = [B("sps", psum=True) for _ in range(NSPS)]
        aps = [psa("aps_%d" % i, [128, 512]) for i in range(2)]; b_aps = [B("aps", psum=True) for _ in range(2)]
        ctr = {"hs": 0, "pj": 0, "sps": 0, "aps": 0, "pt": 0, "x": 0, "sg": 0, "ysq": 0, "tt": 0, "vz": 0}

        def nxt(k, n):
            v = ctr[k] % n
            ctr[k] += 1
            return v

        def transpose_tile(src, b_src, dst, b_dst_list):
            for kc in range(8):
                T.op(PE, lambda: nc.tensor.transpose(out=tp[:, kc, :], in_=src[:, kc * 128:(kc + 1) * 128], identity=identb),
                     [b_src, b_cst], [b_tp])
            T.op(ACT, lambda: nc.scalar.copy(out=dst, in_=tp[:, :, :]), [b_tp], b_dst_list)

        def proj(fcol, tb, ps_ap, b_ps):
            for kc in range(8):
                mm(ps_ap, winb[:, kc, fcol:fcol + 128], hT[:, kc, tb * 512:(tb + 1) * 512], kc == 0, kc == 7,
                   [b_win] + b_hT[tb * 4:(tb + 1) * 4], [b_ps])

        for s in range(n_seq):
            r0 = s * S
            T.skip = "A1" not in phases
            seed(b_hT, [b_wout])
            for i in range(16):
                xs = nxt("x", NXS)
                T.dma(SP, b_x32[xs], x32[xs][:], x_d[r0 + i * 128:r0 + (i + 1) * 128, :], writes=[b_x32[xs]])
                T.op(ACT, lambda: nc.scalar.activation(out=xn[xs][:], in_=x32[xs][:], func=AF.Square, accum_out=ss[xs][:]),
                     [b_x32[xs]], [b_xn[xs], b_ss[xs]])
                rstd_from_ss(ss[xs][:], rstd[xs][:], b_ss[xs], b_rstd[xs])
                T.op(DVE, lambda: nc.vector.scalar_tensor_tensor(out=xn[xs][:], in0=x32[xs][:], scalar=rstd[xs][:], in1=gmix[:],
                                                                op0=ALU.mult, op1=ALU.mult),
                     [b_x32[xs], b_rstd[xs], b_gmix], [b_xn[xs]])
                transpose_tile(xn[xs], b_xn[xs], hT[:, :, i * 128:(i + 1) * 128], [b_hT[i]])

            T.skip = "A2" not in phases
            seed(attn_bufs, conv_bufs)
            zero_fill((NBLK * (s + 1) + n_seq - 1) // n_seq)
            for i in range(NVS):
                bl = [b for g4 in b_Vz[i] for b in g4]
                T.op(POOL, lambda: nc.gpsimd.memset(Vz[i], 0.0), (), bl)
            T.op(POOL, lambda: nc.gpsimd.memset(qTe[64:128, :], 0.0), (), b_qe)
            T.op(POOL, lambda: nc.gpsimd.memset(qTo[0:64, :], 0.0), (), b_qo)
            for hp in range(4):
                for tb in range(4):
                    p = nxt("pj", NPJ)
                    proj(1024 + hp * 128, tb, pj[p][:], b_pj[p])
                    T.op(ACT, lambda: nc.scalar.mul(out=qTe[0:64, tb * 512:(tb + 1) * 512], in_=pj[p][0:64, :], mul=0.125),
                         [b_pj[p]], [b_qe[tb]])
                    T.op(DVE, lambda: nc.vector.tensor_scalar(out=qTo[64:128, tb * 512:(tb + 1) * 512], in0=pj[p][64:128, :],
                                                             scalar1=0.125, scalar2=None, op0=ALU.mult),
                         [b_pj[p]], [b_qo[tb]])
                for tb in range(4):
                    p = nxt("pj", NPJ)
                    proj(1536 + hp * 128, tb, pj[p][:], b_pj[p])
                    T.op(DVE, lambda: nc.vector.tensor_copy(out=kT[:, tb * 512:(tb + 1) * 512], in_=pj[p][:]),
                         [b_pj[p]], [b_k[tb]])
                for tb in range(4):
                    p = nxt("pj", NPJ)
                    proj(2048 + hp * 128, tb, pj[p][:], b_pj[p])
                    T.op(ACT, lambda: nc.scalar.copy(out=vT[:, tb * 512:(tb + 1) * 512], in_=pj[p][:]), [b_pj[p]], [b_vT[tb]])
                for pi, d in enumerate(PATTERN_D):
                    vs = nxt("vz", NVS)
                    vz = Vz[vs]
                    T.skip = ("A2" not in phases) or ("noV" in phases)
                    for tg8 in range(2):
                        for t8 in range(8):
                            tau = tg8 * 8 + t8
                            T.op(PE, lambda: nc.tensor.transpose(out=tp[:, t8, :], in_=vT[:, cols(d, tau)], identity=identb),
                                 b_vT + [b_cst], [b_tp])
                        T.op(ACT, lambda: nc.scalar.copy(out=vz[:, tg8 * 8:(tg8 + 1) * 8, 0, 0:64], in_=tp[:, :, 0:64]),
                             [b_tp], [b_Vz[vs][2 * tg8][0], b_Vz[vs][2 * tg8 + 1][0]])
                        T.op(DVE, lambda: nc.vector.tensor_copy(out=vz[:, tg8 * 8:(tg8 + 1) * 8, 1, 64:128], in_=tp[:, :, 64:128]),
                             [b_tp], [b_Vz[vs][2 * tg8][1], b_Vz[vs][2 * tg8 + 1][1]])
                    T.skip = ("A2" not in phases) or ("noS" in phases)
                    items = [(tau, h) for tau in range(16) for h in range(2)]
                    st = {}
                    LOOK = 3

                    def stage_s(tau, h):
                        _, n = tile_base(d, tau)
                        has_prev = n > 0
                        cq = cols(d, tau)
                        qs, bq = (qTe, b_qe) if h == 0 else (qTo, b_qo)
                        sp_ = nxt("sps", NSPS)
                        lo = 0 if has_prev else 128
                        if has_prev:
                            mm(sps[sp_][:, 0:128], kT[:, cols(d, tau - 1)], qs[:, cq], True, True, b_k + bq, [b_sps[sp_]])
                        mm(sps[sp_][:, 128:256], kT[:, cq], qs[:, cq], True, True, b_k + bq, [b_sps[sp_]])
                        pp = nxt("pt", NPT)
                        T.op(ACT, lambda: nc.scalar.activation(out=pt[pp][:, lo:256], in_=sps[sp_][:, lo:256], func=AF.Exp),
                             [b_sps[sp_]], [b_pt[pp]])
                        T.op(POOL, lambda: nc.gpsimd.tensor_tensor(out=pt[pp][:, lo:256], in0=pt[pp][:, lo:256], in1=m2[:, lo:256],
                                                                  op=ALU.mult),
                             [b_pt[pp], b_cst], [b_pt[pp]])
                        st[(tau, h)] = (pp, has_prev)

                    def stage_pv(tau, h):
                        pp, has_prev = st.pop((tau, h))
                        if h == 0:
                            st["a"] = nxt("aps", 2)
                        a = st["a"]
                        cq = cols(d, tau)
                        kbs = ([(tau - 1, 0)] if has_prev else []) + [(tau, 1)]
                        first = (h == 0)
                        for (tk, kb) in kbs:
                            last = (h == 1 and kb == 1)
                            mm(aps[a][:, 0:128], vz[:, tk, h, :], pt[pp][:, kb * 128:(kb + 1) * 128], first, False,
                               [b_Vz[vs][tk // 4][h], b_pt[pp]], [b_aps[a]])
                            first = False
                            mm(aps[a][:, 128:256], onesH[h], pt[pp][:, kb * 128:(kb + 1) * 128], False, last,
                               [b_cst, b_pt[pp]], [b_aps[a]])
                        if h == 1:
                            av = aps[a][:, 0:256].rearrange("p (a b) -> p a b", b=128)
                            if pi == 0:
                                T.op(DVE, lambda: nc.vector.tensor_copy(out=acc[:, :, cq], in_=av), [b_aps[a]], [b_acc])
                            else:
                                T.op(DVE, lambda: nc.vector.tensor_tensor(out=acc[:, :, cq], in0=av, in1=acc[:, :, cq], op=ALU.add),
                                     [b_aps[a], b_acc], [b_acc])

                    for k in range(len(items) + LOOK):
                        if k < len(items):
                            stage_s(*items[k])
                        if k >= LOOK:
                            stage_pv(*items[k - LOOK])
                T.skip = "A2" not in phases
                T.op(DVE, lambda: nc.vector.reciprocal(out=acc[:, 1, :], in_=acc[:, 1, :]), [b_acc], [b_acc])
                T.op(DVE, lambda: nc.vector.tensor_tensor(out=catT[:, 4 + hp, :], in0=acc[:, 0, :], in1=acc[:, 1, :], op=ALU.mult),
                     [b_acc], b_cat[4 + hp])

            T.skip = "A3" not in phases
            seed(conv_bufs, attn_bufs)
            T.op(POOL, lambda: nc.gpsimd.memset(u[:, 0:30], 0.0), (), [b_u0])
            for cc in range(4):
                for j in range(31):
                    T.op(DVE, lambda: nc.vector.tensor_scalar(out=Dg[:, j, :], in0=identb, scalar1=cw[:, cc, j:j + 1], scalar2=None,
                                                             op0=ALU.mult),
                         [b_cst, b_cw], [b_Dg])
                for tb in range(4):
                    pa = nxt("pj", NPJ)
                    proj(cc * 128, tb, pj[pa][:], b_pj[pa])
                    pg = nxt("pj", NPJ)
                    proj(512 + cc * 128, tb, pj[pg][:], b_pj[pg])
                    sgi = nxt("sg", 2)
                    T.op(ACT, lambda: nc.scalar.activation(out=sg[sgi], in_=pj[pg][:], func=AF.Sigmoid), [b_pj[pg]], [b_sg[sgi]])
                    T.op(DVE, lambda: nc.vector.tensor_tensor(out=u[:, 30 + tb * 512:30 + (tb + 1) * 512], in0=pj[pa][:], in1=sg[sgi],
                                                             op=ALU.mult),
                         [b_pj[pa], b_sg[sgi]], [b_u[tb]])
                for tb in range(4):
                    pc = nxt("pj", NPJ)
                    rd = [b_Dg, b_u0] + ([b_u[tb - 1]] if tb > 0 else []) + [b_u[tb]]
                    for j in range(31):
                        mm(pj[pc][:], Dg[:, j, :], u[:, tb * 512 + j:tb * 512 + j + 512], j == 0, j == 30, rd, [b_pj[pc]])
                    T.op(ACT, lambda: nc.scalar.activation(out=ybf[:, cc, tb * 512:(tb + 1) * 512], in_=pj[pc][:], func=AF.Identity,
                                                           bias=cv[:, cc:cc + 1]),
                         [b_pj[pc], b_cv], [b_y[cc][tb]])
            T.skip = "A4" not in phases
            for tb in range(4):
                p1 = nxt("pj", NPJ)
                for cc in range(4):
                    mm(pj[p1][:], onesM, ybf[:, cc, tb * 512:(tb + 1) * 512], cc == 0, cc == 3, [b_cst, b_y[cc][tb]], [b_pj[p1]])
                p2 = nxt("pj", NPJ)
                for cc in range(4):
                    yi = nxt("ysq", 2)
                    T.op(ACT, lambda: nc.scalar.activation(out=ysq[yi], in_=ybf[:, cc, tb * 512:(tb + 1) * 512], func=AF.Square),
                         [b_y[cc][tb]], [b_ysq[yi]])
                    mm(pj[p2][:], onesM, ysq[yi], cc == 0, cc == 3, [b_cst, b_ysq[yi]], [b_pj[p2]])
                T.op(DVE, lambda: nc.vector.tensor_copy(out=mean, in_=pj[p1][:]), [b_pj[p1]], [b_mean])
                T.op(DVE, lambda: nc.vector.tensor_tensor(out=var, in0=mean, in1=mean, op=ALU.mult), [b_mean], [b_var])
                T.op(DVE, lambda: nc.vector.tensor_tensor(out=var, in0=pj[p2][:], in1=var, op=ALU.subtract),
                     [b_pj[p2], b_var], [b_var])
                T.op(ACT, lambda: nc.scalar.activation(out=var, in_=var, func=AF.Ln, bias=epsT[:]), [b_var, b_eps], [b_var])
                T.op(ACT, lambda: nc.scalar.activation(out=var, in_=var, func=AF.Exp, scale=-0.5), [b_var], [b_var])
                for cc in range(4):
                    ti = nxt("tt", 2)
                    T.op(DVE, lambda: nc.vector.tensor_tensor(out=tt[ti], in0=ybf[:, cc, tb * 512:(tb + 1) * 512], in1=mean,
                                                             op=ALU.subtract),
                         [b_y[cc][tb], b_mean], [b_tt[ti]])
                    T.op(DVE, lambda: nc.vector.tensor_tensor(out=tt[ti], in0=tt[ti], in1=var, op=ALU.mult),
                         [b_tt[ti], b_var], [b_tt[ti]])
                    T.op(ACT, lambda: nc.scalar.activation(out=catT[:, cc, tb * 512:(tb + 1) * 512], in_=tt[ti], func=AF.Silu,
                                                           scale=cv[:, 4 + cc:5 + cc], bias=cv[:, 8 + cc:9 + cc]),
                         [b_tt[ti], b_cv], [b_cat[cc][tb]])
            T.skip = "A6" not in phases
            seed([b_wout], b_hT)
            T.dma(POOL, b_wout, woutb, wout_d.rearrange("(kc p) n -> p kc n", p=128), writes=[b_wout])
            xslot = {}

            def a6_load(i):
                if i < 16 and i not in xslot:
                    xs_ = nxt("x", NXS)
                    xslot[i] = xs_
                    T.dma(SP, b_x32[xs_], x32[xs_][:], x_d[r0 + i * 128:r0 + (i + 1) * 128, :], writes=[b_x32[xs_]])

            def a6_main(i):
                a6_load(i)
                xs = xslot[i]
                hsl = nxt("hs", NHS)
                for hf in range(2):
                    p = nxt("pj", NPJ)
                    for kc in range(8):
                        mm(pj[p][:], catT[:, kc, i * 128:(i + 1) * 128], woutb[:, kc, hf * 512:(hf + 1) * 512], kc == 0, kc == 7,
                           [b_wout, b_cat[kc][i // 4]], [b_pj[p]])
                    T.op(DVE, lambda: nc.vector.tensor_tensor(out=x32[xs][:, hf * 512:(hf + 1) * 512], in0=pj[p][:],
                                                             in1=x32[xs][:, hf * 512:(hf + 1) * 512], op=ALU.add),
                         [b_pj[p], b_x32[xs]], [b_x32[xs]])
                a6_load(i + 1)
                T.dma(SP, b_x32[xs], x2_d[r0 + i * 128:r0 + (i + 1) * 128, :], x32[xs][:], reads=[b_x32[xs]])
                T.op(ACT, lambda: nc.scalar.activation(out=xn[xs][:], in_=x32[xs][:], func=AF.Square, accum_out=ss[xs][:]),
                     [b_x32[xs]], [b_xn[xs], b_ss[xs]])
                rstd_from_ss(ss[xs][:], rstd[xs][:], b_ss[xs], b_rstd[xs])
                T.op(DVE, lambda: nc.vector.scalar_tensor_tensor(out=xn[xs][:], in0=x32[xs][:], scalar=rstd[xs][:], in1=gffn[:],
                                                                op0=ALU.mult, op1=ALU.mult),
                     [b_x32[xs], b_rstd[xs], b_gffn], [b_xn[xs]])
                transpose_tile(xn[xs], b_xn[xs], hnTr[hsl][:], [b_hnTr[hsl]])
                if not sparse:
                    T.dma(SP, b_hnTr[hsl], hnT_d[:, :, r0 + i * 128:r0 + (i + 1) * 128], hnTr[hsl][:], reads=[b_hnTr[hsl]])
                else:
                    T.dma(SP, b_xn[xs], hn_d[r0 + i * 128:r0 + (i + 1) * 128, :], xn[xs][:], reads=[b_xn[xs]])
                return hsl

            def route(i, hsl, ri):
                jg = s * 16 + i
                V = nc.vector
                lg, lem, m8, sm, ohb, ohs, posm, jk32, ppos, idx8 = RS[ri]
                R_ = [b_rsl[ri]]
                RA = [b_rsl[ri], b_rall]
                p = nxt("pj", NPJ)
                for kc in range(8):
                    mm(pj[p][:, 0:36], hnTr[hsl][:, kc, :], rtb[:, kc, :], kc == 0, kc == 7, [b_hnTr[hsl], b_rt], [b_pj[p]])
                T.op(DVE, lambda: V.tensor_copy(out=lg[:], in_=pj[p][:, 0:36]), [b_pj[p]], R_)
                yield
                ops = [
                    (DVE, lambda: V.reduce_max(out=sm[:, 0:1], in_=lg[:, 0:4], axis=mybir.AxisListType.X), R_, R_),
                    (DVE, lambda: V.tensor_scalar(out=sm[:, 1:2], in0=sm[:, 0:1], scalar1=-1.0, scalar2=None, op0=ALU.mult), R_, R_),
                    (ACT, lambda: nc.scalar.activation(out=sm[:, 12:16], in_=lg[:, 0:4], func=AF.Exp, bias=sm[:, 1:2], accum_out=sm[:, 2:3]),
                     R_, R_),
                    (DVE, lambda: V.reciprocal(out=sm[:, 3:4], in_=sm[:, 2:3]), R_, R_),
                    (DVE, lambda: V.tensor_scalar(out=sm[:, 8:12], in0=lg[:, 0:4], scalar1=sm[:, 0:1], scalar2=None, op0=ALU.is_equal), R_, R_),
                    (DVE, lambda: V.tensor_scalar(out=sm[:, 8:12], in0=sm[:, 8:12], scalar1=BIG, scalar2=-BIG, op0=ALU.mult, op1=ALU.add),
                     R_, R_),
                ]
                for gi in range(4):
                    ops.append((DVE, lambda gi=gi: V.tensor_scalar(out=lem[:, gi * 8:(gi + 1) * 8], in0=lg[:, 4 + gi * 8:12 + gi * 8],
                                                                  scalar1=sm[:, 8 + gi:9 + gi], scalar2=None, op0=ALU.add), R_, R_))
                ops += [
                    (DVE, lambda: V.max(out=m8[:], in_=lem[:]), R_, R_),
                    (DVE, lambda: V.tensor_tensor(out=sm[:, 4:5], in0=m8[:, 1:2], in1=m8[:, 0:1], op=ALU.subtract), R_, R_),
                    (ACT, lambda: nc.scalar.activation(out=sm[:, 5:6], in_=sm[:, 4:5], func=AF.Exp), R_, R_),
                    (DVE, lambda: V.tensor_scalar(out=sm[:, 5:6], in0=sm[:, 5:6], scalar1=1.0, scalar2=None, op0=ALU.add), R_, R_),
                    (DVE, lambda: V.reciprocal(out=sm[:, 5:6], in_=sm[:, 5:6]), R_, R_),
                    (DVE, lambda: V.tensor_tensor(out=wall[:, 2 * jg:2 * jg + 1], in0=sm[:, 5:6], in1=sm[:, 3:4], op=ALU.mult), R_, RA),
                    (DVE, lambda: V.tensor_tensor(out=wall[:, 2 * jg + 1:2 * jg + 2], in0=sm[:, 3:4], in1=wall[:, 2 * jg:2 * jg + 1],
                                                  op=ALU.subtract), RA, RA),
                ]
                ops.append((DVE, lambda: V.max_index(out=idx8[:], in_max=m8[:], in_values=lem[:]), R_, R_))
                ops.append((DVE, lambda: V.tensor_copy(out=eall[:, 2 * jg:2 * jg + 2], in_=idx8[:, 0:2]), R_, RA))
                for a_ in range(2):
                    ops.append((DVE, lambda a_=a_: V.tensor_scalar(out=ohb[:, a_, :], in0=iota_e, scalar1=eall[:, 2 * jg + a_:2 * jg + a_ + 1],
                                                                  scalar2=None, op0=ALU.is_equal), [b_rsl[ri], b_c2f, b_rall], R_))
                ops.append((DVE, lambda: V.tensor_tensor(out=ohs[:], in0=ohb[:, 0, :], in1=ohb[:, 1, :], op=ALU.add), R_, R_))
                for (e_, f_, rd_, wr_) in ops:
                    T.op(e_, f_, rd_, wr_)
                    yield
                p2 = nxt("pj", NPJ)
                mm(pj[p2][:, 0:32], ltri, ohs[:], True, True, [b_c2b, b_rsl[ri]], [b_pj[p2]])
                mm(pj[p2][:, 32:64], onesF, ohs[:], True, True, [b_c2b, b_rsl[ri]], [b_pj[p2]])
                T.op(DVE, lambda: V.tensor_copy(out=ppos[:], in_=pj[p2][:, 0:64]), [b_pj[p2]], R_)
                tails[i] = (p2, ri, jg)

            def route_tail_a(i):
                p2, ri, jg = tails[i]
                posm = RS[ri][6]
                ppos = RS[ri][8]
                T.op(DVE, lambda: nc.vector.tensor_tensor(out=posm[:], in0=ppos[:, 0:32], in1=runb[:], op=ALU.add),
                     [b_rsl[ri], b_runb], [b_rsl[ri]])
                T.op(DVE, lambda: nc.vector.tensor_tensor(out=runb[:], in0=ppos[:, 32:64], in1=runb[:], op=ALU.add),
                     [b_rsl[ri], b_runb], [b_runb])

            def route_tail_b(i, a_):
                p2, ri, jg = tails[i]
                ohb, posm, jk32 = RS[ri][4], RS[ri][6], RS[ri][7]
                T.op(DVE, lambda: nc.vector.scalar_tensor_tensor(out=jk32[:], in0=ohb[:, a_, :], scalar=1.0, in1=posm[:], op0=ALU.mult,
                                                                op1=ALU.mult, accum_out=posall[:, 2 * jg + a_:2 * jg + a_ + 1]),
                     [b_rsl[ri]], [b_rsl[ri], b_rall])

            tails = {}
            GRP = int(_os.environ.get("GRP", "4"))
            for i0 in range(0, 16, GRP):
                hsl_ = [a6_main(i) for i in range(i0, i0 + GRP)]
                if not sparse:
                    continue
                gens = [route(i0 + k_, hsl_[k_], k_) for k_ in range(GRP)]
                alive = list(gens)
                while alive:
                    nxt_alive = []
                    for g_ in alive:
                        try:
                            next(g_)
                            nxt_alive.append(g_)
                        except StopIteration:
                            pass
                    alive = nxt_alive
                for k_ in range(GRP):
                    route_tail_a(i0 + k_)
                for a_ in range(2):
                    for k_ in range(GRP):
                        route_tail_b(i0 + k_, a_)

        T.skip = False
        zero_fill(NBLK)
        last_store = [list(b.r.values()) for b in b_x32 + b_hnTr + b_xn]
        for e in engines:
            for l in last_store:
                T.wait(e, l)
        barrier()
        es_a.close()

        if moe and sparse:
            stage_b_sparse(nc, T, es, engines, B, mm, barrier, NT, NBLK, x2_d, hn_d, xs_d, ys_d, out_d, gfin_d, eg_d, eu_d, ed_d,
                           nh, b_nh, rstd_from_ss, cstb, b_cst, c2b, b_c2b, iota_e, iota_b, thr, pidx, b_c2f,
                           eall, posall, wall, b_rall, runb, b_runb, b_xsd)
        elif moe:
            stage_b(nc, T, es, engines, B, mm, barrier, n_seq, G, x2_d, hnT_d, out_d, gfin_d, rt_d, eg_d, eu_d, ed_d,
                    nh, b_nh, rstd_from_ss)
        else:
            T.wait(SP, [])
    return nc


def stage_b(nc, T, es, engines, B, mm, barrier, n_seq, G, x2_d, hnT_d, out_d, gfin_d, rt_d, eg_d, eu_d, ed_d,
            nh, b_nh, rstd_from_ss):
    PE, ACT, DVE, POOL, SP = engines
    NT = n_seq * S
    NTG = G // 128

    def sb(name, shape, dt=F32):
        return es.enter_context(nc.sbuf_tensor(name, shape, dt))

    def ps(name, shape, dt=F32):
        return es.enter_context(nc.psum_tensor(name, shape, dt))

    gfin = sb("gfin", [128, D]); b_gfin = B("gfin", True)
    T.dma(SP, b_gfin, gfin[:], gfin_d[:, :], writes=[b_gfin])
    rtb = sb("rtb", [128, 8, 36], BF16); b_rt = B("rt", True)
    T.dma(POOL, b_rt, rtb[:], rt_d.rearrange("(kc p) n -> p kc n", p=128), writes=[b_rt])
    hnT = sb("hnT", [128, 8, G], BF16); b_hnT = B("hnT", True)
    accb = sb("accb", [128, NTG, D]); b_accb = [B("accb", True) for _ in range(NTG)]
    NW = 2
    wg = [sb("wg_%d" % i, [128, 8, 256], BF16) for i in range(NW)]; b_wg = [B("wg", True) for _ in range(NW)]
    wu = [sb("wu_%d" % i, [128, 8, 256], BF16) for i in range(NW)]; b_wu = [B("wu", True) for _ in range(NW)]
    wd = [sb("wd_%d" % i, [128, 2, D], BF16) for i in range(NW)]; b_wd = [B("wd", True) for _ in range(NW)]
    hE = [sb("hE_%d" % i, [128, 2, G], BF16) for i in range(2)]
    b_hE = [[[B("hE") for _ in range(G // 512)] for _ in range(2)] for _ in range(2)]
    sgl = [sb("sgl_%d" % i, [128, 512], BF16) for i in range(2)]; b_sgl = [B("sgl") for _ in range(2)]
    call = sb("call", [128, NTG, 32]); b_call = [B("call") for _ in range(NTG)]
    lg = sb("lg", [128, 36]); b_lg = B("lg")
    lem = sb("lem", [128, 32]); b_lem = B("lem")
    oh = sb("oh", [128, 32]); b_oh = B("oh")
    m8 = sb("m8", [128, 8]); b_m8 = B("m8")
    sm = sb("sm", [128, 16]); b_sm = B("sm")
    junk = sb("junkb", [128, D], BF16); b_junk = B("junk")
    ssb = sb("ssb", [128, 1]); b_ssb = B("ssb")
    rsb = sb("rsb", [128, 1]); b_rsb = B("rsb")
    ob = [sb("ob_%d" % i, [128, D]) for i in range(2)]; b_ob = [B("ob", True) for _ in range(2)]
    pgu = [ps("pgu_%d" % i, [128, 512]) for i in range(4)]; b_pgu = [B("pgu", psum=True) for _ in range(4)]
    pdn = [ps("pdn_%d" % i, [128, 512]) for i in range(4)]; b_pdn = [B("pdn", psum=True) for _ in range(4)]
    ctr = {"gu": 0, "dn": 0, "sgl": 0, "ob": 0}

    def nxt(k, n):
        v = ctr[k] % n
        ctr[k] += 1
        return v

    def load_w(e, slot):
        T.dma(POOL, b_wg[slot], wg[slot][:], eg_d[e].rearrange("(kc p) n -> p kc n", p=128), writes=[b_wg[slot]])
        T.dma(POOL, b_wu[slot], wu[slot][:], eu_d[e].rearrange("(kc p) n -> p kc n", p=128), writes=[b_wu[slot]])
        T.dma(POOL, b_wd[slot], wd[slot][:], ed_d[e].rearrange("(kc p) n -> p kc n", p=128), writes=[b_wd[slot]])

    for g in range(NT // G):
        t0 = g * G
        load_w(0, 0)
        T.dma(SP, b_hnT, hnT[:], hnT_d[:, :, t0:t0 + G], writes=[b_hnT])
        for j in range(NTG):
            T.dma(SP, b_accb[j], accb[:, j, :], x2_d[t0 + j * 128:t0 + (j + 1) * 128, :], writes=[b_accb[j]])
        for j in range(NTG):
            p = nxt("gu", 4)
            for kc in range(8):
                mm(pgu[p][:, 0:36], hnT[:, kc, j * 128:(j + 1) * 128], rtb[:, kc, :], kc == 0, kc == 7, [b_hnT, b_rt], [b_pgu[p]])
            V = nc.vector
            T.op(DVE, lambda: V.tensor_copy(out=lg[:], in_=pgu[p][:, 0:36]), [b_pgu[p]], [b_lg])
            T.op(DVE, lambda: V.reduce_max(out=sm[:, 0:1], in_=lg[:, 0:4], axis=mybir.AxisListType.X), [b_lg], [b_sm])
            T.op(DVE, lambda: V.tensor_scalar(out=sm[:, 1:2], in0=sm[:, 0:1], scalar1=-1.0, scalar2=None, op0=ALU.mult), [b_sm], [b_sm])
            T.op(ACT, lambda: nc.scalar.activation(out=sm[:, 12:16], in_=lg[:, 0:4], func=AF.Exp, bias=sm[:, 1:2], accum_out=sm[:, 2:3]),
                 [b_lg, b_sm], [b_sm])
            T.op(DVE, lambda: V.reciprocal(out=sm[:, 3:4], in_=sm[:, 2:3]), [b_sm], [b_sm])
            T.op(DVE, lambda: V.tensor_scalar(out=sm[:, 8:12], in0=lg[:, 0:4], scalar1=sm[:, 0:1], scalar2=None, op0=ALU.is_equal),
                 [b_lg, b_sm], [b_sm])
            T.op(DVE, lambda: V.tensor_scalar(out=sm[:, 8:12], in0=sm[:, 8:12], scalar1=BIG, scalar2=-BIG, op0=ALU.mult, op1=ALU.add),
                 [b_sm], [b_sm])
            for gi in range(4):
                T.op(DVE, lambda: V.tensor_scalar(out=lem[:, gi * 8:(gi + 1) * 8], in0=lg[:, 4 + gi * 8:12 + gi * 8],
                                                 scalar1=sm[:, 8 + gi:9 + gi], scalar2=None, op0=ALU.add),
                     [b_lg, b_sm], [b_lem])
            T.op(DVE, lambda: V.max(out=m8[:], in_=lem[:]), [b_lem], [b_m8])
            T.op(DVE, lambda: V.tensor_tensor(out=sm[:, 4:5], in0=m8[:, 1:2], in1=m8[:, 0:1], op=ALU.subtract), [b_m8, b_sm], [b_sm])
            T.op(ACT, lambda: nc.scalar.activation(out=sm[:, 5:6], in_=sm[:, 4:5], func=AF.Exp), [b_sm], [b_sm])
            T.op(DVE, lambda: V.tensor_scalar(out=sm[:, 5:6], in0=sm[:, 5:6], scalar1=1.0, scalar2=None, op0=ALU.add), [b_sm], [b_sm])
            T.op(DVE, lambda: V.reciprocal(out=sm[:, 5:6], in_=sm[:, 5:6]), [b_sm], [b_sm])
            T.op(DVE, lambda: V.tensor_tensor(out=sm[:, 6:7], in0=sm[:, 5:6], in1=sm[:, 3:4], op=ALU.mult), [b_sm], [b_sm])
            T.op(DVE, lambda: V.tensor_tensor(out=sm[:, 7:8], in0=sm[:, 3:4], in1=sm[:, 6:7], op=ALU.subtract), [b_sm], [b_sm])
            T.op(DVE, lambda: V.tensor_scalar(out=oh[:], in0=lem[:], scalar1=m8[:, 0:1], scalar2=sm[:, 6:7], op0=ALU.is_equal, op1=ALU.mult),
                 [b_lem, b_m8, b_sm], [b_oh])
            T.op(DVE, lambda: V.tensor_scalar(out=call[:, j, :], in0=lem[:], scalar1=m8[:, 1:2], scalar2=sm[:, 7:8], op0=ALU.is_equal,
                                             op1=ALU.mult),
                 [b_lem, b_m8, b_sm], [b_call[j]])
            T.op(DVE, lambda: V.tensor_tensor(out=call[:, j, :], in0=call[:, j, :], in1=oh[:], op=ALU.add), [b_call[j], b_oh], [b_call[j]])
        for e in range(32):
            sl = e % NW
            if e + 1 < 32:
                load_w(e + 1, (e + 1) % NW)
            hs = e % 2
            for dc in range(2):
                for tb in range(G // 512):
                    pg = nxt("gu", 4)
                    for kc in range(8):
                        mm(pgu[pg][:], wg[sl][:, kc, dc * 128:(dc + 1) * 128], hnT[:, kc, tb * 512:(tb + 1) * 512], kc == 0, kc == 7,
                           [b_wg[sl], b_hnT], [b_pgu[pg]])
                    pu = nxt("gu", 4)
                    for kc in range(8):
                        mm(pgu[pu][:], wu[sl][:, kc, dc * 128:(dc + 1) * 128], hnT[:, kc, tb * 512:(tb + 1) * 512], kc == 0, kc == 7,
                           [b_wu[sl], b_hnT], [b_pgu[pu]])
                    si = nxt("sgl", 2)
                    T.op(ACT, lambda: nc.scalar.activation(out=sgl[si][:], in_=pgu[pg][:], func=AF.Silu), [b_pgu[pg]], [b_sgl[si]])
                    T.op(DVE, lambda: nc.vector.tensor_tensor(out=hE[hs][:, dc, tb * 512:(tb + 1) * 512], in0=pgu[pu][:], in1=sgl[si][:],
                                                             op=ALU.mult),
                         [b_pgu[pu], b_sgl[si]], [b_hE[hs][dc][tb]])
            for j in range(NTG):
                for hf in range(2):
                    pd = nxt("dn", 4)
                    for dc in range(2):
                        mm(pdn[pd][:], hE[hs][:, dc, j * 128:(j + 1) * 128], wd[sl][:, dc, hf * 512:(hf + 1) * 512], dc == 0, dc == 1,
                           [b_hE[hs][dc][j // 4], b_wd[sl]], [b_pdn[pd]])
                    T.op(DVE, lambda: nc.vector.scalar_tensor_tensor(out=accb[:, j, hf * 512:(hf + 1) * 512], in0=pdn[pd][:],
                                                                    scalar=call[:, j, e:e + 1],
                                                                    in1=accb[:, j, hf * 512:(hf + 1) * 512], op0=ALU.mult, op1=ALU.add),
                         [b_pdn[pd], b_call[j], b_accb[j]], [b_accb[j]])
        for j in range(NTG):
            T.op(ACT, lambda: nc.scalar.activation(out=junk[:], in_=accb[:, j, :], func=AF.Square, accum_out=ssb[:]),
                 [b_accb[j]], [b_junk, b_ssb])
            rstd_from_ss(ssb[:], rsb[:], b_ssb, b_rsb)
            oi = nxt("ob", 2)
            T.op(DVE, lambda: nc.vector.scalar_tensor_tensor(out=ob[oi][:], in0=accb[:, j, :], scalar=rsb[:], in1=gfin[:],
                                                            op0=ALU.mult, op1=ALU.mult),
                 [b_accb[j], b_rsb, b_gfin], [b_ob[oi]])
            T.dma(SP, b_ob[oi], out_d[t0 + j * 128:t0 + (j + 1) * 128, :], ob[oi][:], reads=[b_ob[oi]])
    fin = [list(b.r.values()) for b in b_ob]
    for l in fin:
        T.wait(SP, l)
    barrier()


def stage_b_sparse(nc, T, es, engines, B, mm, barrier, NT, NBLK, x2_d, hn_d, xs_d, ys_d, out_d, gfin_d, eg_d, eu_d, ed_d,
                   nh, b_nh, rstd_from_ss, cstb, b_cst, c2b, b_c2b, iota_e, iota_b, thr, pidx, b_c2f,
                   eall, posall, wall, b_rall, runb, b_runb, b_xsd):
    PE, ACT, DVE, POOL, SP = engines
    I32 = mybir.dt.int32
    NTILE = NT // 128
    V = nc.vector
    identb = cstb[:, 0:128]
    onesF = c2b[:, 128:256]

    def sb(name, shape, dt=F32):
        return es.enter_context(nc.sbuf_tensor(name, shape, dt))

    def ps(name, shape, dt=F32):
        return es.enter_context(nc.psum_tensor(name, shape, dt))

    IOff = bass.IndirectOffsetOnAxis
    gfin = sb("gfin", [128, D]); b_gfin = B("gfin", True)
    T.dma(SP, b_gfin, gfin[:], gfin_d[:, :], writes=[b_gfin])
    cmpb = sb("cmpb", [128, 32], BF16); nblk = sb("nblk", [128, 32]); incl = sb("incl", [128, 32]); pstart = sb("pstart", [128, 32])
    one32 = sb("one32", [128, 32]); ebf = sb("ebf", [128, 160]); idxw = sb("idxw", [128, 160], I32)
    destf = sb("destf", [128, 2 * NTILE]); desti = sb("desti", [128, 2 * NTILE], I32)
    oht = sb("oht", [128, 32]); jk = sb("jkb", [128, 32])
    b_t = B("btab")
    ptab = ps("ptab", [128, 512]); b_ptab = B("ptab", psum=True)
    TB = [b_t]
    T.op(DVE, lambda: V.tensor_scalar(out=cmpb[:], in0=runb[:], scalar1=thr, scalar2=None, op0=ALU.is_gt), [b_runb, b_c2f], TB)
    mm(ptab[:, 0:32], onesF, cmpb[:], True, True, [b_c2b, b_t], [b_ptab])
    T.op(DVE, lambda: V.tensor_copy(out=nblk[:], in_=ptab[:, 0:32]), [b_ptab], TB)
    T.op(POOL, lambda: nc.gpsimd.memset(one32[:], 1.0), (), TB)
    T.op(DVE, lambda: V.tensor_tensor_scan(out=incl[:], data0=one32[:], data1=nblk[:], initial=0.0, op0=ALU.mult, op1=ALU.add), TB, TB)
    T.op(DVE, lambda: V.tensor_tensor(out=pstart[:], in0=incl[:], in1=nblk[:], op=ALU.subtract), TB, TB)
    T.op(DVE, lambda: V.tensor_scalar(out=pstart[:], in0=pstart[:], scalar1=128.0, scalar2=None, op0=ALU.mult), TB, TB)
    T.op(POOL, lambda: nc.gpsimd.memset(ebf[:], 0.0), (), TB)
    for e in range(32):
        T.op(DVE, lambda: V.scalar_tensor_tensor(out=ebf[:], in0=iota_b, scalar=incl[:, e:e + 1], in1=ebf[:], op0=ALU.is_ge, op1=ALU.add),
             [b_c2f, b_t], TB)
    T.op(DVE, lambda: V.tensor_scalar(out=ebf[:], in0=ebf[:], scalar1=31.0, scalar2=None, op0=ALU.min), TB, TB)
    eqf = sb("eqf", [128, 160])
    T.op(POOL, lambda: nc.gpsimd.memset(eqf[:], 0.0), (), TB)
    T.op(DVE, lambda: V.tensor_tensor(out=eqf[:, 2:160], in0=ebf[:, 2:160], in1=ebf[:, 0:158], op=ALU.is_equal), TB, TB)
    T.op(DVE, lambda: V.tensor_scalar(out=ebf[:], in0=ebf[:], scalar1=128.0, scalar2=pidx, op0=ALU.mult, op1=ALU.add), [b_t, b_c2f], TB)
    T.op(DVE, lambda: V.scalar_tensor_tensor(out=ebf[:], in0=eqf[:], scalar=8192.0, in1=ebf[:], op0=ALU.mult, op1=ALU.add), TB, TB)
    T.op(DVE, lambda: V.tensor_copy(out=idxw[:], in_=ebf[:]), TB, TB)
    NH = 3
    hnb = [sb("hnb_%d" % i, [128, D], BF16) for i in range(NH)]; b_hnb = [B("hnb", True) for _ in range(NH)]
    b_dst = [B("dst") for _ in range(NTILE)]
    for j in range(NTILE):
        hs = j % NH
        T.dma(SP, b_hnb[hs], hnb[hs][:], hn_d[j * 128:(j + 1) * 128, :], writes=[b_hnb[hs]])
        for k in (2 * j, 2 * j + 1):
            T.op(DVE, lambda: V.tensor_scalar(out=oht[:], in0=iota_e, scalar1=eall[:, k:k + 1], scalar2=None, op0=ALU.is_equal),
                 [b_c2f, b_rall], TB)
            T.op(DVE, lambda: V.scalar_tensor_tensor(out=jk[:], in0=oht[:], scalar=1.0, in1=pstart[:], op0=ALU.mult, op1=ALU.mult,
                                                    accum_out=destf[:, k:k + 1]), TB, TB)
        T.op(DVE, lambda: V.tensor_tensor(out=destf[:, 2 * j:2 * j + 2], in0=destf[:, 2 * j:2 * j + 2], in1=posall[:, 2 * j:2 * j + 2],
                                         op=ALU.add), [b_t, b_rall], TB)
        T.op(DVE, lambda: V.tensor_copy(out=desti[:, 2 * j:2 * j + 2], in_=destf[:, 2 * j:2 * j + 2]), TB, [b_dst[j]])
        for a_ in range(2):
            idma(T, POOL, b_hnb[hs], xs_d[:, :], IOff(ap=desti[:, 2 * j + a_:2 * j + a_ + 1], axis=0), hnb[hs][:], None,
                 reads=[b_hnb[hs], b_dst[j], b_xsd], writes=[])
    T.wait(SP, [d_ for b in b_hnb for d_ in b.r.values()])
    NW = 2
    wgu = [[sb("wgu_%d_%d" % (i, q), [128, 8, 256], BF16) for q in range(2)] for i in range(NW)]
    b_wgu = [[B("wgu", True) for _ in range(2)] for _ in range(NW)]
    wd = [sb("wd_%d" % i, [128, 2, D], BF16) for i in range(NW)]; b_wd = [B("wd", True) for _ in range(NW)]
    NX = 4
    xb = [sb("xb_%d" % i, [128, D], BF16) for i in range(NX)]; b_xb = [B("xb", True) for _ in range(NX)]
    sgl = [sb("sgl_%d" % i, [128, 256]) for i in range(2)]; b_sgl = [B("sgl") for _ in range(2)]
    hTok = [sb("hTok_%d" % i, [128, 256], BF16) for i in range(2)]; b_hTok = [B("hTok") for _ in range(2)]
    hE = [sb("hE_%d" % i, [128, 2, 128], BF16) for i in range(2)]; b_hE = [B("hE") for _ in range(2)]
    yb = [sb("yb_%d" % i, [128, D], BF16) for i in range(2)]; b_yb = [B("yb", True) for _ in range(2)]
    tpb = ps("tpb", [128, 8, 128], BF16); b_tpb = B("tpb", psum=True)
    tp2 = ptab[:, 0:128].bitcast(BF16).rearrange("p (a b) -> p a b", b=128)
    gu = [ps("gu_%d" % i, [128, 512]) for i in range(2)]; b_gu = [B("gu", psum=True) for _ in range(2)]
    yp = [[ps("yp_%d_%d" % (i, h), [128, 512]) for h in range(2)] for i in range(2)]
    b_yp = [[B("yp", psum=True) for h in range(2)] for i in range(2)]

    bc_reg = nc.gpsimd.to_reg(32 * 128 - 1)
    kw = dict(bounds_check=bc_reg, oob_is_err=False)
    eg3 = eg_d.rearrange("r (k n) -> r k n", n=256)
    eu3 = eu_d.rearrange("r (k n) -> r k n", n=256)

    def load_gu(b):
        if b >= NBLK:
            return
        sl = b % NW
        io = IOff(ap=idxw[:, b:b + 1], axis=0)
        idma(T, POOL, b_wgu[sl][0], wgu[sl][0][:].rearrange("p a b -> p (a b)"), None, eg_d[:, :], io, reads=[b_t],
             writes=[b_wgu[sl][0]], **kw)
        idma(T, POOL, b_wgu[sl][1], wgu[sl][1][:].rearrange("p a b -> p (a b)"), None, eu_d[:, :], io, reads=[b_t],
             writes=[b_wgu[sl][1]], **kw)

    def load_d(b):
        if b >= NBLK:
            return
        sl = b % NW
        io = IOff(ap=idxw[:, b:b + 1], axis=0)
        idma(T, POOL, b_wd[sl], wd[sl][:].rearrange("p a b -> p (a b)"), None, ed_d[:, :], io, reads=[b_t], writes=[b_wd[sl]], **kw)

    def load_x(b):
        if b >= NBLK:
            return
        xi = b % NX
        T.dma(SP, b_xb[xi], xb[xi][:], xs_d[b * 128:(b + 1) * 128, :], writes=[b_xb[xi]])

    NXT = 3
    xT = [sb("xTp_%d" % i, [128, 8, 128], BF16) for i in range(NXT)]; b_xT = [B("xT") for _ in range(NXT)]

    def st_T(k):
        xi = k % NX; ti = k % NXT
        for kc in range(8):
            T.op(PE, lambda: nc.tensor.transpose(out=tpb[:, kc, :], in_=xb[xi][:, kc * 128:(kc + 1) * 128], identity=identb),
                 [b_xb[xi], b_cst], [b_tpb])
        T.op(ACT, lambda: nc.scalar.copy(out=xT[ti][:], in_=tpb[:, :, :]), [b_tpb], [b_xT[ti]])

    def st_GU(k):
        tx = k % NXT; ti = k % 2; sl = k % NW
        for q in range(2):
            for kc in range(8):
                mm(gu[ti][:, q * 256:(q + 1) * 256], xT[tx][:, kc, :], wgu[sl][q][:, kc, :], kc == 0, kc == 7,
                   [b_xT[tx], b_wgu[sl][q]], [b_gu[ti]])
        T.op(ACT, lambda: nc.scalar.activation(out=sgl[ti][:], in_=gu[ti][:, 0:256], func=AF.Silu), [b_gu[ti]], [b_sgl[ti]])
        T.op(DVE, lambda: V.tensor_tensor(out=hTok[ti][:], in0=gu[ti][:, 256:512], in1=sgl[ti][:], op=ALU.mult),
             [b_gu[ti], b_sgl[ti]], [b_hTok[ti]])

    def st_T2(k):
        ti = k % 2
        for dc in range(2):
            T.op(PE, lambda: nc.tensor.transpose(out=tp2[:, dc, :], in_=hTok[ti][:, dc * 128:(dc + 1) * 128], identity=identb),
                 [b_hTok[ti], b_cst], [b_ptab])
        T.op(DVE, lambda: V.tensor_copy(out=hE[ti][:], in_=tp2), [b_ptab], [b_hE[ti]])

    def st_DN(k):
        ti = k % 2; sl = k % NW
        for hf in range(2):
            for dc in range(2):
                mm(yp[ti][hf][:], hE[ti][:, dc, :], wd[sl][:, dc, hf * 512:(hf + 1) * 512], dc == 0, dc == 1,
                   [b_hE[ti], b_wd[sl]], [b_yp[ti][hf]])
        T.op(ACT, lambda: nc.scalar.copy(out=yb[ti][:, 0:512], in_=yp[ti][0][:]), [b_yp[ti][0]], [b_yb[ti]])
        T.op(DVE, lambda: V.tensor_copy(out=yb[ti][:, 512:1024], in_=yp[ti][1][:]), [b_yp[ti][1], b_yb[ti]], [b_yb[ti]])
        T.dma(SP, b_yb[ti], ys_d[k * 128:(k + 1) * 128, :], yb[ti][:], reads=[b_yb[ti]])

    for b0 in range(2):
        load_gu(b0); load_d(b0)
    for b0 in range(3):
        load_x(b0)
    for it in range(NBLK + 3):
        load_x(it + 3)
        if it < NBLK:
            st_T(it)
        if 0 <= it - 1 < NBLK:
            st_GU(it - 1)
            load_gu(it + 1)
        if 0 <= it - 2 < NBLK:
            st_T2(it - 2)
        if 0 <= it - 3 < NBLK:
            st_DN(it - 3)
            load_d(it - 1)
    T.wait(POOL, [d_ for b in b_yb for d_ in b.r.values()])
    NC_ = 6
    xa = [sb("xa_%d" % i, [128, D]) for i in range(NC_)]; b_xa = [B("xa", True) for _ in range(NC_)]
    y1 = [sb("y1_%d" % i, [128, D], BF16) for i in range(NC_)]; b_y1 = [B("y1", True) for _ in range(NC_)]
    y2 = [sb("y2_%d" % i, [128, D], BF16) for i in range(NC_)]; b_y2 = [B("y2", True) for _ in range(NC_)]
    ob = [sb("ob_%d" % i, [128, D]) for i in range(NC_)]; b_ob = [B("ob", True) for _ in range(NC_)]
    junk = sb("junkb", [128, D], BF16); b_junk = B("junk")
    ssb = [sb("ssb_%d" % i, [128, 1]) for i in range(NC_)]; b_ssb = [B("ssb") for _ in range(NC_)]
    rsb = [sb("rsb_%d" % i, [128, 1]) for i in range(NC_)]; b_rsb = [B("rsb") for _ in range(NC_)]
    for j in range(NTILE):
        c = j % NC_
        T.dma(SP, b_xa[c], xa[c][:], x2_d[j * 128:(j + 1) * 128, :], writes=[b_xa[c]])
        idma(T, POOL, b_y1[c], y1[c][:], None, ys_d[:, :], IOff(ap=desti[:, 2 * j:2 * j + 1], axis=0), reads=[b_dst[j]], writes=[b_y1[c]])
        idma(T, POOL, b_y2[c], y2[c][:], None, ys_d[:, :], IOff(ap=desti[:, 2 * j + 1:2 * j + 2], axis=0), reads=[b_dst[j]], writes=[b_y2[c]])
        T.op(DVE, lambda: V.scalar_tensor_tensor(out=xa[c][:], in0=y1[c][:], scalar=wall[:, 2 * j:2 * j + 1], in1=xa[c][:],
                                                op0=ALU.mult, op1=ALU.add), [b_y1[c], b_rall, b_xa[c]], [b_xa[c]])
        T.op(DVE, lambda: V.scalar_tensor_tensor(out=xa[c][:], in0=y2[c][:], scalar=wall[:, 2 * j + 1:2 * j + 2], in1=xa[c][:],
                                                op0=ALU.mult, op1=ALU.add), [b_y2[c], b_rall, b_xa[c]], [b_xa[c]])
        T.op(ACT, lambda: nc.scalar.activation(out=junk[:], in_=xa[c][:], func=AF.Square, accum_out=ssb[c][:]),
             [b_xa[c]], [b_junk, b_ssb[c]])
        rstd_from_ss(ssb[c][:], rsb[c][:], b_ssb[c], b_rsb[c])
        T.op(DVE, lambda: V.scalar_tensor_tensor(out=ob[c][:], in0=xa[c][:], scalar=rsb[c][:], in1=gfin[:], op0=ALU.mult, op1=ALU.mult),
             [b_xa[c], b_rsb[c], b_gfin], [b_ob[c]])
        T.dma(ACT, b_ob[c], out_d[j * 128:(j + 1) * 128, :], ob[c][:], reads=[b_ob[c]])
    for b in b_ob:
        T.wait(SP, list(b.r.values()))
        T.wait(ACT, list(b.r.values()))
    barrier()


def make_consts():
    c = np.zeros((128, 768), np.float32)
    c[:, 0:128] = np.eye(128, dtype=np.float32)
    c[:, 128:192] = 1.0
    c[:, 256 + 64:384] = 1.0
    c[:, 384:512] = 1.0 / 512.0
    kj = np.arange(128)[:, None]
    qi = np.arange(128)[None, :]
    c[:, 512:640] = (kj >= qi)
    c[:, 640:768] = (kj <= qi)
    return c


def make_consts2():
    c = np.zeros((128, 450), np.float32)
    tp_ = np.arange(128)[:, None]
    t_ = np.arange(128)[None, :]
    c[:, 0:128] = (tp_ < t_)
    c[:, 128:256] = 1.0
    c[:, 256:288] = np.arange(32)[None, :]
    c[:, 288:448] = np.arange(160)[None, :]
    c[:, 448] = 128.0 * np.arange(128)
    c[:, 449] = np.arange(128)
    return c


def host_layout(inp, n_seq=SEQ_PER_CORE, sparse=True):
    f = lambda a: np.ascontiguousarray(np.asarray(a, dtype=np.float32))
    bc = lambda v: f(np.broadcast_to(np.asarray(v, np.float32).reshape(1, D), (128, D)))
    cwt = np.asarray(inp["conv_dw_w"], np.float32)[0]
    conv_w = f(cwt.T.reshape(4, 128, 31).transpose(1, 0, 2).reshape(128, 124))
    pc = lambda v: np.asarray(v, np.float32).reshape(4, 128).T
    conv_v = f(np.concatenate([pc(inp["conv_dw_b"]), pc(inp["conv_ln_g"]), pc(inp["conv_ln_b"])], axis=1))
    router = f(np.concatenate([np.asarray(inp["router_group"], np.float32)[0],
                               np.asarray(inp["router_expert"], np.float32)[0].reshape(D, 32)], axis=1))
    shared = {
        "w_in": f(inp["w_in"][0]), "w_out": f(inp["w_out"][0]), "cst": make_consts(),
        "gmix_b": bc(inp["norm_mix_g"]), "gffn_b": bc(inp["norm_ffn_g"]), "gfin_b": bc(inp["norm_final_g"]),
        "conv_w": conv_w, "conv_v": conv_v, "router": router,
        "cst2": make_consts2(),
    }
    if sparse:
        lay = lambda w, kcn: f(np.asarray(w, np.float32)[0].reshape(32, kcn, 128, -1).transpose(0, 2, 1, 3).reshape(32 * 128, 2048))
        shared.update({"e_gate": lay(inp["expert_w_gate"], 8), "e_up": lay(inp["expert_w_up"], 8), "e_down": lay(inp["expert_w_down"], 2)})
    else:
        shared.update({"e_gate": f(inp["expert_w_gate"][0]), "e_up": f(inp["expert_w_up"][0]), "e_down": f(inp["expert_w_down"][0])})
    return shared


def kernel(x, norm_mix_g, w_in, conv_dw_w, conv_dw_b, conv_ln_g, conv_ln_b, w_out, norm_ffn_g, router_group,
           router_expert, expert_w_gate, expert_w_up, expert_w_down, norm_final_g):
    inp = dict(norm_mix_g=norm_mix_g, w_in=w_in, conv_dw_w=conv_dw_w, conv_dw_b=conv_dw_b, conv_ln_g=conv_ln_g,
               conv_ln_b=conv_ln_b, w_out=w_out, norm_ffn_g=norm_ffn_g, router_group=router_group,
               router_expert=router_expert, expert_w_gate=expert_w_gate, expert_w_up=expert_w_up,
               expert_w_down=expert_w_down, norm_final_g=norm_final_g)
    shared = host_layout(inp)
    xf = np.asarray(x, dtype=np.float32)
    nc = build()
    in_maps = []
    for c in range(NCORE):
        m = dict(shared)
        m["x"] = np.ascontiguousarray(xf[c * SEQ_PER_CORE:(c + 1) * SEQ_PER_CORE].reshape(SEQ_PER_CORE * S, D))
        in_maps.append(m)
    res = run_bass_kernel_spmd(nc, in_maps, core_ids=list(range(NCORE)))
    out = np.concatenate([np.asarray(r["out"], dtype=np.float32).reshape(SEQ_PER_CORE, S, D) for r in res.results], axis=0)
    return out
```
